# Optimizing a Trainium2 kernel written in Bass

```python
import jax, jax.numpy as jnp
from jax import lax
import numpy as np

D_MODEL = 2048
BATCH = 4
SEQ = 2048
DEPTH = 1

HEAD_DIM = 128
N_HEADS_TOTAL = D_MODEL // HEAD_DIM
N_HEADS_MOBA = N_HEADS_TOTAL // 2
N_HEADS_SB = N_HEADS_TOTAL - N_HEADS_MOBA
W_MOBA = N_HEADS_MOBA * HEAD_DIM
W_SB = N_HEADS_SB * HEAD_DIM
D_MIX = W_MOBA + W_SB
D_FF = -(-8 * D_MODEL // (3 * 256)) * 256
MOBA_BLOCK = 256
MOBA_TOPK = 3
MOBA_Q_CHUNK = 32
SB_Q_BLOCK = 128
ROPE_THETA = 500000.0
ROT_DIM = HEAD_DIM // 4
RMS_EPS = 1e-6
FFN_RES_SCALE = 0.5

kernel_name = "hybrid_moba_stickbreaking_macaron_block"


def rmsnorm(x, g):
    xf = x.astype(jnp.float32)
    y = xf * lax.rsqrt(jnp.mean(xf * xf, axis=-1, keepdims=True) + RMS_EPS) * g.astype(jnp.float32)
    return y.astype(x.dtype)


def swiglu(x, w_gate, w_up, w_down):
    return (jax.nn.silu(x @ w_gate) * (x @ w_up)) @ w_down


def partial_rope(x):
    S = x.shape[2]
    half = ROT_DIM // 2
    inv_freq = ROPE_THETA ** (-jnp.arange(0, ROT_DIM, 2, dtype=jnp.float32) / ROT_DIM)
    ang = jnp.arange(S, dtype=jnp.float32)[:, None] * inv_freq[None, :]
    cos, sin = jnp.cos(ang), jnp.sin(ang)
    xr = x[..., :ROT_DIM].astype(jnp.float32)
    x1, x2 = xr[..., :half], xr[..., half:]
    rot = jnp.concatenate([x1 * cos - x2 * sin, x2 * cos + x1 * sin], axis=-1).astype(x.dtype)
    return jnp.concatenate([rot, x[..., ROT_DIM:]], axis=-1)


def moba_attention(q, k, v):
    B, H, S, D = q.shape
    nb = -(-S // MOBA_BLOCK)
    pad = nb * MOBA_BLOCK - S
    kp = jnp.pad(k, ((0, 0), (0, 0), (0, pad), (0, 0)))
    vp = jnp.pad(v, ((0, 0), (0, 0), (0, pad), (0, 0)))
    k_blocks = kp.reshape(B, H, nb, MOBA_BLOCK, D)
    v_blocks = vp.reshape(B, H, nb, MOBA_BLOCK, D)
    k_mean = jnp.mean(k_blocks.astype(jnp.float32), axis=3)
    topk = min(MOBA_TOPK, nb - 1)
    scale = D ** -0.5
    n_chunks = S // MOBA_Q_CHUNK
    q_chunks = q.reshape(B, H, n_chunks, MOBA_Q_CHUNK, D).transpose(2, 0, 1, 3, 4)
    b_idx = jnp.arange(B)[:, None, None, None]
    h_idx = jnp.arange(H)[None, :, None, None]
    block_ids = jnp.arange(nb)
    offs = jnp.arange(MOBA_BLOCK)

    def chunk_fn(args):
        c, qc = args
        t0 = c * MOBA_Q_CHUNK
        q_pos = t0 + jnp.arange(MOBA_Q_CHUNK)
        own = t0 // MOBA_BLOCK
        qf = qc.astype(jnp.float32)
        k_own = lax.dynamic_index_in_dim(k_blocks, own, axis=2, keepdims=False)
        v_own = lax.dynamic_index_in_dim(v_blocks, own, axis=2, keepdims=False)
        s_own = jnp.einsum('bhqd,bhkd->bhqk', qf, k_own.astype(jnp.float32)) * scale
        own_pos = own * MOBA_BLOCK + offs
        s_own = jnp.where(own_pos[None, :] <= q_pos[:, None], s_own, -jnp.inf)
        if topk > 0:
            gate = jnp.einsum('bhqd,bhnd->bhqn', qf, k_mean)
            gate = jnp.where(block_ids < own, gate, -jnp.inf)
            _, sel = lax.top_k(gate, topk)
            sel_valid = sel < own
            k_sel = k_blocks[b_idx, h_idx, sel]
            v_sel = v_blocks[b_idx, h_idx, sel]
            s_sel = jnp.einsum('bhqd,bhqnkd->bhqnk', qf, k_sel.astype(jnp.float32)) * scale
            s_sel = jnp.where(sel_valid[..., None], s_sel, -jnp.inf)
            s_sel = s_sel.reshape(B, H, MOBA_Q_CHUNK, topk * MOBA_BLOCK)
            p = jax.nn.softmax(jnp.concatenate([s_sel, s_own], axis=-1), axis=-1)
            p_sel = p[..., :topk * MOBA_BLOCK].reshape(B, H, MOBA_Q_CHUNK, topk, MOBA_BLOCK)
            p_own = p[..., topk * MOBA_BLOCK:]
            out = (jnp.einsum('bhqnk,bhqnkd->bhqd', p_sel.astype(v.dtype), v_sel)
                   + jnp.einsum('bhqk,bhkd->bhqd', p_own.astype(v.dtype), v_own))
        else:
            p_own = jax.nn.softmax(s_own, axis=-1)
            out = jnp.einsum('bhqk,bhkd->bhqd', p_own.astype(v.dtype), v_own)
        return out

    outs = lax.map(chunk_fn, (jnp.arange(n_chunks), q_chunks))
    return outs.transpose(1, 2, 0, 3, 4).reshape(B, H, S, D)


def stick_breaking_attention(q, k, v):
    B, H, S, D = q.shape
    scale = D ** -0.5
    nqb = S // SB_Q_BLOCK
    kf = k.astype(jnp.float32)
    k_pos = jnp.arange(S)
    q_blocks = q.reshape(B, H, nqb, SB_Q_BLOCK, D).transpose(2, 0, 1, 3, 4)

    def block_fn(args):
        i, qb = args
        q_pos = i * SB_Q_BLOCK + jnp.arange(SB_Q_BLOCK)
        z = jnp.einsum('bhqd,bhkd->bhqk', qb.astype(jnp.float32), kf) * scale
        causal = k_pos[None, :] < q_pos[:, None]
        log_beta = jax.nn.log_sigmoid(z)
        log_one_minus = jnp.where(causal, jax.nn.log_sigmoid(-z), 0.0)
        log_stick = lax.cumsum(log_one_minus, axis=3, reverse=True) - log_one_minus
        a = jnp.where(causal, jnp.exp(log_beta + log_stick), 0.0)
        return jnp.einsum('bhqk,bhkd->bhqd', a.astype(v.dtype), v)

    outs = lax.map(block_fn, (jnp.arange(nqb), q_blocks))
    return outs.transpose(1, 2, 0, 3, 4).reshape(B, H, S, D)


def split_heads(t, n_heads):
    B, S, _ = t.shape
    return t.reshape(B, S, n_heads, HEAD_DIM).transpose(0, 2, 1, 3)


def merge_heads(t):
    B, H, S, D = t.shape
    return t.transpose(0, 2, 1, 3).reshape(B, S, H * D)


def setup_inputs(seed: int = 0) -> dict:
    key = jax.random.key(seed)
    ks = jax.random.split(key, 20)

    def w(k, shape, fan_in):
        return jax.random.normal(k, shape, jnp.float32) * (fan_in ** -0.5)

    def gain(k, n):
        return 1.0 + 0.02 * jax.random.normal(k, (DEPTH, n), jnp.float32)

    return {
        "x": jax.random.normal(ks[0], (BATCH, SEQ, D_MODEL), jnp.float32),
        "ffn1_pre_g": gain(ks[1], D_MODEL),
        "ffn1_w_gate": w(ks[2], (DEPTH, D_MODEL, D_FF), D_MODEL),
        "ffn1_w_up": w(ks[3], (DEPTH, D_MODEL, D_FF), D_MODEL),
        "ffn1_w_down": w(ks[4], (DEPTH, D_FF, D_MODEL), D_FF),
        "ffn1_post_g": gain(ks[5], D_MODEL),
        "mix_pre_g": gain(ks[6], D_MODEL),
        "w_in": w(ks[7], (DEPTH, D_MODEL, 3 * D_MIX), D_MODEL),
        "moba_out_g": gain(ks[8], W_MOBA),
        "sb_out_g": gain(ks[9], W_SB),
        "w_out": w(ks[10], (DEPTH, D_MIX, D_MODEL), D_MIX),
        "mix_post_g": gain(ks[11], D_MODEL),
        "ffn2_pre_g": gain(ks[12], D_MODEL),
        "ffn2_w_gate": w(ks[13], (DEPTH, D_MODEL, D_FF), D_MODEL),
        "ffn2_w_up": w(ks[14], (DEPTH, D_MODEL, D_FF), D_MODEL),
        "ffn2_w_down": w(ks[15], (DEPTH, D_FF, D_MODEL), D_FF),
        "ffn2_post_g": gain(ks[16], D_MODEL),
    }


def reference(x, ffn1_pre_g, ffn1_w_gate, ffn1_w_up, ffn1_w_down, ffn1_post_g,
              mix_pre_g, w_in, moba_out_g, sb_out_g, w_out, mix_post_g,
              ffn2_pre_g, ffn2_w_gate, ffn2_w_up, ffn2_w_down, ffn2_post_g):
    for l in range(DEPTH):
        f = swiglu(rmsnorm(x, ffn1_pre_g[l]), ffn1_w_gate[l], ffn1_w_up[l], ffn1_w_down[l])
        x = x + FFN_RES_SCALE * rmsnorm(f, ffn1_post_g[l])

        h = rmsnorm(x, mix_pre_g[l])
        proj = h @ w_in[l]
        q_a, k_a, v_a, q_b, k_b, v_b = jnp.split(
            proj, [W_MOBA, 2 * W_MOBA, 3 * W_MOBA, 3 * W_MOBA + W_SB, 3 * W_MOBA + 2 * W_SB], axis=-1)
        q_a = partial_rope(split_heads(q_a, N_HEADS_MOBA))
        k_a = partial_rope(split_heads(k_a, N_HEADS_MOBA))
        o_a = moba_attention(q_a, k_a, split_heads(v_a, N_HEADS_MOBA))
        o_b = stick_breaking_attention(split_heads(q_b, N_HEADS_SB), split_heads(k_b, N_HEADS_SB),
                                       split_heads(v_b, N_HEADS_SB))
        o = jnp.concatenate([rmsnorm(merge_heads(o_a), moba_out_g[l]),
                             rmsnorm(merge_heads(o_b), sb_out_g[l])], axis=-1)
        x = x + rmsnorm(o @ w_out[l], mix_post_g[l])

        f = swiglu(rmsnorm(x, ffn2_pre_g[l]), ffn2_w_gate[l], ffn2_w_up[l], ffn2_w_down[l])
        x = x + FFN_RES_SCALE * rmsnorm(f, ffn2_post_g[l])
    return x
```

```python
import numpy as np
import concourse.bass as bass
import concourse.mybir as mybir
from concourse.bass_utils import run_bass_kernel_spmd

F32 = mybir.dt.float32
BF16 = mybir.dt.bfloat16
AF = mybir.ActivationFunctionType
ALU = mybir.AluOpType
AX = mybir.AxisListType

D = 2048
DFF = 5632
NCH = DFF // 128
NJ = 4
CPJ = NCH // NJ
T = 8
KC = D // 128
NH = 16
EPS = 1e-6
SCALE = 128 ** -0.5
NEG = -30000.0
KB = 1024
SB_BASE = 16512

SAME_ENGINE_SYNC = {"vector": True, "scalar": True, "gpsimd": True, "tensor": False, "sync": False}


class Sched:
    def __init__(self, nc):
        self.nc = nc
        self.engines = ["sync", "scalar", "vector", "gpsimd", "tensor"]
        self.lists = {e: [] for e in self.engines}
        self.cnt = {}
        self.semh = {}
        self.seen = {e: {} for e in self.engines}
        self.lastw = {}
        self.readers = {}
        self._stack = []

    def add_sem(self, key):
        cm = self.nc.semaphore("s_" + key)
        h = cm.__enter__()
        self._stack.append(cm)
        self.semh[key] = h
        self.cnt[key] = 0

    def close(self):
        for cm in reversed(self._stack):
            cm.__exit__(None, None, None)

    def _waits(self, engine, deps):
        need = {}
        for (sk, v) in deps:
            if v > need.get(sk, 0):
                need[sk] = v
        out = []
        for sk, v in need.items():
            if sk == engine and not SAME_ENGINE_SYNC[engine]:
                continue
            if self.seen[engine].get(sk, 0) >= v:
                continue
            self.seen[engine][sk] = v
            out.append((sk, v))
        return out

    def op(self, engine, fn, reads=(), writes=(), dma=None):
        deps = []
        for r in reads:
            t = self.lastw.get(r)
            if t:
                deps.append(t)
        for w in writes:
            t = self.lastw.get(w)
            if t:
                deps.append(t)
            deps.extend(self.readers.get(w, ()))
        waits = self._waits(engine, deps)
        if dma is None:
            self.cnt[engine] += 1
            tok = (engine, self.cnt[engine])
            inc = (engine, 1)
        else:
            self.cnt[dma] += 16
            tok = (dma, self.cnt[dma])
            inc = (dma, 16)
        self.lists[engine].append((waits, fn, inc))
        for r in reads:
            self.readers.setdefault(r, []).append(tok)
        for w in writes:
            self.lastw[w] = tok
            self.readers[w] = []
        return tok

    def barrier(self):
        for e in self.engines:
            deps = [(k, v) for k, v in self.cnt.items() if v > 0]
            waits = self._waits(e, deps)
            if waits:
                self.lists[e].append((waits, None, None))
        self.lastw = {}
        self.readers = {}

    def emit(self, block):
        nc = self.nc

        def run(eng, items):
            for waits, fn, inc in items:
                for sk, v in waits:
                    eng.wait_ge(self.semh[sk], v)
                if fn is not None:
                    ins = fn(eng)
                    ins.then_inc(self.semh[inc[0]], inc[1])

        @block.sync
        def _(e):
            run(e, self.lists["sync"])

        @block.scalar
        def _(e):
            run(e, self.lists["scalar"])

        @block.vector
        def _(e):
            run(e, self.lists["vector"])

        @block.gpsimd
        def _(e):
            run(e, self.lists["gpsimd"])

        @block.tensor
        def _(e):
            run(e, self.lists["tensor"])


import os as _os
_DBG = set(_os.environ.get("KDBG", "").split(","))


def build_nc(debug=False, stage=99):
    nc = bass.Bass("TRN2", target_bir_lowering=False)
    S = Sched(nc)

    def din(name, shape, dt=F32):
        if stage <= 3 and name in ("wgu2", "wd2", "win", "wout", "rope"):
            return None
        if stage <= 5 and name in ("wgu2", "wd2", "wout"):
            return None
        return nc.dram_tensor(name, list(shape), dt, kind="ExternalInput").ap()

    x_own = din("x_own", [T * 128, D])
    x_par = din("x_par", [T * 128, D])
    wgu = [din("wgu1", [NCH, 128, 2, KC, 128]), din("wgu2", [NCH, 128, 2, KC, 128])]
    wd = [din("wd1", [NJ, 4, 128, CPJ, 512]), din("wd2", [NJ, 4, 128, CPJ, 512])]
    win = din("win", [12, 128, KC, 512])
    wout = din("wout", [4, 128, KC, 512])
    gT_d = din("gT", [128, 64])
    gpost_d = din("gpost", [3, 128, D])
    rope_d = din("rope", [2, 128, T * 2 * 4 * 32])
    ident_d = din("ident", [128, 128])
    masks_d = din("masks", [128, 2 * 128])
    pbias_d = din("pbias", [128, 1])
    out_d = nc.dram_tensor("out", [T * 128, D], F32, kind="ExternalOutput").ap()

    dk = "ExternalOutput" if debug else "Internal"
    x1_own = nc.dram_tensor("x1_own", [T * 128, D], F32, kind=dk).ap()
    x1_par = nc.dram_tensor("x1_par", [T * 128, D], F32, kind=dk).ap()
    x2_own = nc.dram_tensor("x2_own", [T * 128, D], F32, kind=dk).ap()
    kT_dram = nc.dram_tensor("kT_dram", [NH, 128, 2048], BF16, kind="Internal").ap()
    v_dram = nc.dram_tensor("v_dram", [NH, 128, 16, 128], BF16, kind="Internal").ap()
    o_dbg = nc.dram_tensor("o_dbg", [T * 128, D], F32, kind=dk).ap() if debug else None

    def sb(name, shape, dt, off):
        return nc.alloc_sbuf_tensor_at(name, list(shape), dt, offset=SB_BASE + off)

    o = 0
    ident_f = sb("ident_f", [128, 128], F32, o); o += 512
    ident_b = sb("ident_b", [128, 128], BF16, o); o += 256
    masks_b = sb("masks_b", [128, 256], BF16, o); o += 512
    gT = sb("gT", [128, 64], F32, o); o += 256
    pbias = sb("pbias", [128, 1], F32, o); o += 32
    small = sb("small", [128, 96], F32, o); o += 384
    ones_b = sb("ones_b", [128, 2048], BF16, o); o += 4096
    small2 = sb("small2", [128, 64], F32, o); o += 256
    CONST_END = 6 * KB + 512
    assert o <= CONST_END
    A0 = CONST_END
    B0 = A0 + 64 * KB
    C0 = B0 + 32 * KB
    R0 = C0 + 32 * KB
    big = sb("big", [128, T, D], F32, A0)
    hT = sb("hT", [128, KC, 1024], BF16, B0)
    qT = sb("qT", [128, NH, 1024], BF16, C0)
    aT = sb("aT", [128, CPJ, 1024], BF16, C0)
    sg = [sb("sg%d" % i, [128, 1024], F32, C0 + 22 * KB + i * 4 * KB) for i in range(2)]
    r = R0
    wgu_sb = [sb("wgu_sb%d" % i, [128, 2, KC, 128], BF16, r + i * 8 * KB) for i in range(3)]
    r += 24 * KB
    wd_sb = [sb("wd_sb%d" % i, [128, CPJ, 512], BF16, r + i * 11 * KB) for i in range(2)]
    r += 22 * KB
    xt = sb("xt", [128, D], F32, r); r += 8 * KB
    xn = sb("xn", [128, D], F32, r); r += 8 * KB
    gpost = sb("gpost", [128, D], F32, r); r += 8 * KB
    FFN_END = r
    r = R0
    win_sb = [sb("win_sb%d" % i, [128, KC, 512], BF16, r + i * 16 * KB) for i in range(2)]
    r += 32 * KB
    rope_sb = sb("rope_sb", [128, 2, T * 2 * 4 * 32], F32, r); r += 16 * KB
    xt2 = sb("xt2", [128, D], F32, r); r += 8 * KB
    xn2 = sb("xn2", [128, D], F32, r); r += 8 * KB
    gpost2 = sb("gpost2", [128, D], F32, R0 + 32 * KB)
    qk_sb = [sb("qk_sb%d" % i, [128, 512], BF16, r + i * KB) for i in range(2)]; r += 2 * KB
    kst = [sb("kst%d" % i, [128, 4, 128], BF16, r + i * KB) for i in range(2)]; r += 2 * KB
    rt1 = sb("rt1", [128, 4, 32], F32, r); r += 512
    rt2 = sb("rt2", [128, 4, 32], F32, r); r += 512
    QKV_END = r
    r = R0
    kT_h = [sb("kT_h%d" % i, [128, 2048], BF16, r + i * 4 * KB) for i in range(2)]; r += 8 * KB
    v_h = [sb("v_h%d" % i, [128, 16, 128], BF16, r + i * 4 * KB) for i in range(2)]; r += 8 * KB
    EL = [sb("EL%d" % i, [128, 2048], F32, r + i * 8 * KB) for i in range(2)]; r += 16 * KB
    Pbm = [sb("Pbm%d" % i, [128, 2048], BF16, r - 16 * KB + i * 4 * KB) for i in range(4)]
    zb = [sb("zb%d" % i, [128, 2048], F32, r + i * 8 * KB) for i in range(2)]; r += 16 * KB
    Cb1 = sb("Cb1", [128, 2048], F32, r); r += 8 * KB
    Pb = [sb("Pb%d" % i, [128, 2048], BF16, r + i * 4 * KB) for i in range(2)]; r += 8 * KB
    PT = [sb("PT%d" % i, [128, 2048], BF16, r + i * 4 * KB) for i in range(2)]; r += 8 * KB
    km_f = sb("km_f", [128, 8], F32, r); r += 32
    km_b = [sb("km_b%d" % i, [128, 8], BF16, r + 32 * i) for i in range(2)]; r += 64
    ATT_END = r
    LIMIT = 229376 - SB_BASE
    assert max(FFN_END, QKV_END, ATT_END) <= LIMIT, (FFN_END, QKV_END, ATT_END)

    ps = nc.alloc_psum_tensor("ps", [128, 4096], F32)
    psb = ps.bitcast(BF16)

    def bank(b, w=512, off=0):
        return ps[:, b * 512 + off: b * 512 + off + w]

    for k in ["sync", "scalar", "vector", "gpsimd", "tensor"]:
        S.add_sem(k)
    for k in ["d_const", "d_const_sw", "d_xt0", "d_xt1", "d_xt2", "d_out0", "d_out1", "d_gpost", "d_out", "d_wgu0", "d_wgu1", "d_wgu2", "d_wd0", "d_wd1",
              "d_win0", "d_win1", "d_rope", "d_kst0", "d_kst1", "d_vst0", "d_vst1",
              "d_kT0", "d_kT1", "d_v0", "d_v1", "d_dbg"]:
        S.add_sem(k)

    ss = small[:, 0:16]
    rs = small[:, 16:32]
    rstd = small[:, 32:48]
    g8 = small[:, 48:56]
    m8 = small[:, 56:64]
    selb = small[:, 64:72]
    rsum = small[:, 72:80]
    misc = small[:, 80:96]
    g8s = [small2[:, 0:8], small2[:, 8:16]]
    m8s = [small2[:, 16:24], small2[:, 24:32]]
    sels = [small2[:, 32:40], small2[:, 40:48]]
    rsums = [small[:, 64:72], small[:, 72:80]]
    rtot = small2[:, 48:50]
    rinv4 = small2[:, 50:54]
    negT = small2[:, 54:56]
    negTp = small2[:, 56:58]

    S.op("sync", lambda e: e.dma_start(out=ident_f[:], in_=ident_d[:, :]), writes=["ident_f"], dma="d_const")
    S.op("sync", lambda e: e.dma_start(out=gT[:], in_=gT_d[:, :]), writes=["gT"], dma="d_const")
    S.op("sync", lambda e: e.dma_start(out=pbias[:], in_=pbias_d[:, :]), writes=["pbias"], dma="d_const")
    S.op("gpsimd", lambda e: e.dma_start(out=ident_b[:], in_=ident_d[:, :]), writes=["ident_b"], dma="d_const_sw")
    S.op("gpsimd", lambda e: e.dma_start(out=masks_b[:], in_=masks_d[:, :]), writes=["masks_b"], dma="d_const_sw")
    S.op("vector", lambda e: e.memset(ones_b[:], 1.0), writes=["ones_b"])
    S.barrier()

    nA_xt = [sb("nA_xt%d" % i, [128, D], F32, A0 + i * 8 * KB) for i in range(3)]
    nA_xn = [sb("nA_xn%d" % i, [128, D], F32, A0 + 24 * KB + i * 8 * KB) for i in range(2)]
    nA_junk = sb("nA_junk", [128, D], BF16, A0 + 40 * KB)
    nA_gbc = sb("nA_gbc", [128, KC, 128], F32, A0 + 44 * KB)
    nR_xn = [sb("nR_xn%d" % i, [128, D], F32, R0 + 32 * KB + i * 8 * KB) for i in range(2)]
    nR_junk = sb("nR_junk", [128, D], BF16, R0 + 48 * KB)
    nR_gbc = sb("nR_gbc", [128, KC, 128], F32, R0 + 52 * KB)
    pB_xt = [sb("pB_xt%d" % i, [128, D], F32, B0 + i * 8 * KB) for i in range(2)]
    pB_out = [sb("pB_out%d" % i, [128, D], F32, B0 + 16 * KB + i * 8 * KB) for i in range(2)]
    pC_junk = sb("pC_junk", [128, D], BF16, C0)

    def norm_T(get_src, dst_T, gcol0, ngroups, xn_ring, junk, gbc, tag):
        gw = D // ngroups
        for kc in range(KC):
            S.op("vector", lambda e, kc=kc: e.tensor_scalar(
                out=gbc[:, kc, :], in0=ones_b[:, 0:128], scalar1=gT[:, gcol0 + kc:gcol0 + kc + 1], scalar2=None,
                op0=ALU.mult), writes=["gbc"])
        srcs = {}

        def S1(t):
            src_ap, src_res = get_src(t)
            k = t % len(xn_ring)
            xn_ = xn_ring[k]
            for g in range(ngroups):
                S.op("scalar", lambda e, g=g: e.activation(
                    out=junk[:, 0:gw], in_=src_ap[:, g * gw:(g + 1) * gw], func=AF.Square,
                    accum_out=ss[:, 2 * t + g: 2 * t + g + 1]), reads=[src_res], writes=["junk", ("ss", t)])
            S.op("scalar", lambda e: e.activation(
                out=rs[:, 2 * t:2 * t + ngroups], in_=ss[:, 2 * t:2 * t + ngroups], func=AF.Sqrt,
                scale=1.0 / gw, bias=eps_col[:, 0:1]), reads=[("ss", t)], writes=[("rs", t)])
            S.op("vector", lambda e: e.reciprocal(out=rstd[:, 2 * t:2 * t + ngroups], in_=rs[:, 2 * t:2 * t + ngroups]),
                 reads=[("rs", t)], writes=[("rstd", t)])
            for g in range(ngroups):
                S.op("scalar", lambda e, g=g: e.activation(
                    out=xn_[:, g * gw:(g + 1) * gw], in_=src_ap[:, g * gw:(g + 1) * gw], func=AF.Copy,
                    scale=rstd[:, 2 * t + g:2 * t + g + 1]), reads=[src_res, ("rstd", t)], writes=[("xn", k)])

        def S2(t):
            k = t % len(xn_ring)
            xn_ = xn_ring[k]
            b0 = 4 * (t % 2)
            for kc in range(KC):
                b = b0 + kc // 4
                S.op("tensor", lambda e, kc=kc, b=b: e.transpose(
                    out=bank(b, 128, (kc % 4) * 128), in_=xn_[:, kc * 128:(kc + 1) * 128], identity=ident_f[:]),
                    reads=[("xn", k)], writes=[("ps", b)])

        def S3(t):
            b0 = 4 * (t % 2)
            for q in range(4):
                b = b0 + q
                S.op("vector", lambda e, q=q, b=b: e.tensor_tensor(
                    out=dst_T[:, 4 * q:4 * q + 4, t * 128:(t + 1) * 128],
                    in0=bank(b).rearrange("p (k j) -> p k j", k=4), in1=gbc[:, 4 * q:4 * q + 4, :], op=ALU.mult),
                    reads=[("ps", b), "gbc"], writes=[(tag, t)])

        for p_ in range(-1, T):
            if p_ + 1 < T:
                S1(p_ + 1)
            if p_ >= 0:
                S2(p_)
                S3(p_)

    eps_col = misc[:, 0:1]
    S.op("vector", lambda e: e.memset(eps_col, EPS), writes=["eps"])
    S.barrier()

    def dram_src(src, ring):
        def get(t):
            k = t % len(ring)
            S.op("sync", lambda e: e.dma_start(out=ring[k][:], in_=src[t * 128:(t + 1) * 128, :]),
                 writes=[("xt", k)], dma="d_xt%d" % k)
            return ring[k], ("xt", k)
        return get

    def postnorm_residual(fsb, src, dst, gidx, res_scale, gp_):
        S.op("sync", lambda e: e.dma_start(out=gp_[:], in_=gpost_d[gidx, :, :]), writes=["gpost"], dma="d_gpost")

        alias_B = [("hT", tt) for tt in range(T)]
        alias_C = [("aT", cc) for cc in range(CPJ)] + [("sg", 0), ("sg", 1)]

        def P1(t):
            k = t % 2
            S.op("sync", lambda e: e.dma_start(out=pB_xt[k][:], in_=src[t * 128:(t + 1) * 128, :]),
                 writes=[("xt", k)] + (alias_B if t < 2 else []), dma="d_xt%d" % k)
            S.op("scalar", lambda e: e.activation(
                out=pC_junk[:], in_=fsb[:, t, :], func=AF.Square, accum_out=ss[:, t:t + 1]),
                reads=[("f", t)], writes=["junk", ("ss", t)] + (alias_C if t == 0 else []))
            S.op("scalar", lambda e: e.activation(
                out=rs[:, t:t + 1], in_=ss[:, t:t + 1], func=AF.Sqrt, scale=1.0 / D, bias=eps_col[:, 0:1]),
                reads=[("ss", t)], writes=[("rs", t)])
            S.op("vector", lambda e: e.reciprocal(out=rstd[:, t:t + 1], in_=rs[:, t:t + 1]),
                 reads=[("rs", t)], writes=[("rstd", t)])
            S.op("vector", lambda e: e.tensor_scalar(
                out=rstd[:, 8 + t:9 + t], in0=rstd[:, t:t + 1], scalar1=float(res_scale), scalar2=None, op0=ALU.mult),
                reads=[("rstd", t)], writes=[("rstds", t)])

        def P2(t):
            k = t % 2
            S.op("vector", lambda e: e.scalar_tensor_tensor(
                out=pB_out[k][:], in0=fsb[:, t, :], scalar=rstd[:, 8 + t:9 + t], in1=gp_[:], op0=ALU.mult, op1=ALU.mult),
                reads=[("f", t), ("rstds", t), "gpost"], writes=[("po", k), ("po2", k)] + (alias_B if t < 2 else []))
            S.op("gpsimd", lambda e: e.tensor_tensor(
                out=pB_out[k][:, 0:1024], in0=pB_out[k][:, 0:1024], in1=pB_xt[k][:, 0:1024], op=ALU.add),
                reads=[("po", k), ("xt", k)], writes=[("po", k)])
            S.op("vector", lambda e: e.tensor_tensor(
                out=pB_out[k][:, 1024:2048], in0=pB_out[k][:, 1024:2048], in1=pB_xt[k][:, 1024:2048], op=ALU.add),
                reads=[("po2", k), ("xt", k)], writes=[("po2", k)])
            S.op("sync", lambda e: e.dma_start(out=dst[t * 128:(t + 1) * 128, :], in_=pB_out[k][:]),
                 reads=[("po", k), ("po2", k)], writes=[("dst", t)], dma="d_out%d" % k)

        for p_ in range(-1, T):
            if p_ + 1 < T:
                P1(p_ + 1)
            if p_ >= 0:
                P2(p_)


    def tok_proj(lhsT_fn, lhs_res_fn, nk, w_slots, w_sems, w_src_fn, ngrp, epilogue, bank_fn, w_tag):
        for n in range(ngrp):
            sl = n % len(w_slots)
            S.op("gpsimd", lambda e, n=n, sl=sl: e.dma_start(out=w_slots[sl][:], in_=w_src_fn(n)),
                 writes=[(w_tag, sl)], dma=w_sems[sl])
            for t in range(T):
                b = bank_fn(n, t)
                for k in range(nk):
                    S.op("tensor", lambda e, k=k, t=t, b=b, sl=sl: e.matmul(
                        out=bank(b), lhsT=lhsT_fn(k, t), rhs=w_slots[sl][:, k, :], start=(k == 0), stop=(k == nk - 1)),
                        reads=[(w_tag, sl), lhs_res_fn(k, t)], writes=[("ps", b)])
                epilogue(n, t, b)

    def ffn(idx, src, dst, gcol0, gidx, sub=99):
        Wgu, Wd = wgu[idx], wd[idx]
        norm_T(dram_src(src, nA_xt), hT, gcol0, 1, nA_xn, nA_junk, nA_gbc, "hT")
        if sub < 1:
            S.barrier()
            return
        for j in range(NJ):
            for cc in range(CPJ):
                c = j * CPJ + cc
                sl = c % 3
                par = c % 2
                S.op("gpsimd", lambda e, c=c, sl=sl: e.dma_start(out=wgu_sb[sl][:], in_=Wgu[c]),
                     writes=[("wgu", sl)], dma="d_wgu%d" % sl)
                for gu in range(2):
                    for kc in range(KC):
                        for half in range(2):
                            b = par * 4 + gu * 2 + half
                            S.op("tensor", lambda e, gu=gu, kc=kc, half=half, b=b, sl=sl: e.matmul(
                                out=bank(b), lhsT=wgu_sb[sl][:, gu, kc, :], rhs=hT[:, kc, half * 512:(half + 1) * 512],
                                start=(kc == 0), stop=(kc == KC - 1)),
                                reads=[("wgu", sl)] + [("hT", tt) for tt in range(half * 4, half * 4 + 4)],
                                writes=[("ps", b)])
                bg = par * 4
                S.op("scalar", lambda e, bg=bg, par=par: e.activation(
                    out=sg[par][:], in_=ps[:, bg * 512: bg * 512 + 1024], func=AF.Silu),
                    reads=[("ps", bg), ("ps", bg + 1)], writes=[("sg", par)])
                S.op("vector", lambda e, bg=bg, par=par, cc=cc: e.tensor_tensor(
                    out=aT[:, cc, :], in0=sg[par][:], in1=ps[:, (bg + 2) * 512:(bg + 2) * 512 + 1024], op=ALU.mult),
                    reads=[("sg", par), ("ps", bg + 2), ("ps", bg + 3)], writes=[("aT", cc)])
            for n in range(4):
                sl = (j * 4 + n) % 2
                S.op("gpsimd", lambda e, j=j, n=n, sl=sl: e.dma_start(out=wd_sb[sl][:], in_=Wd[j, n]),
                     writes=[("wd", sl)], dma="d_wd%d" % sl)
                for t in range(T):
                    for cc in range(CPJ):
                        S.op("tensor", lambda e, t=t, cc=cc, sl=sl: e.matmul(
                            out=bank(t), lhsT=aT[:, cc, t * 128:(t + 1) * 128], rhs=wd_sb[sl][:, cc, :],
                            start=(cc == 0), stop=(cc == CPJ - 1)),
                            reads=[("wd", sl), ("aT", cc)], writes=[("ps", t)])
                    if j == 0:
                        S.op("scalar", lambda e, t=t, n=n: e.activation(
                            out=big[:, t, n * 512:(n + 1) * 512], in_=bank(t), func=AF.Copy),
                            reads=[("ps", t)], writes=[("f", t)])
                    else:
                        S.op("vector", lambda e, t=t, n=n: e.tensor_tensor(
                            out=big[:, t, n * 512:(n + 1) * 512], in0=big[:, t, n * 512:(n + 1) * 512],
                            in1=bank(t), op=ALU.add),
                            reads=[("ps", t), ("f", t)], writes=[("f", t)])
        postnorm_residual(big, src, dst, gidx, 0.5, gpost)
        S.barrier()

    def finish():
        with nc.Block() as block:
            S.emit(block)
        S.close()
        return nc

    if stage == 0:
        return finish()
    if stage == 1:
        ffn(0, x_par, x1_par, 0, 0, sub=0)
        return finish()
    ffn(0, x_par, x1_par, 0, 0)
    if stage == 2:
        return finish()
    ffn(0, x_own, x1_own, 0, 0)
    if stage == 3:
        return finish()

    S.op("sync", lambda e: e.dma_start(out=rope_sb[:], in_=rope_d.rearrange("w p f -> p w f")),
         writes=["rope"], dma="d_rope")
    rope_v = [rope_sb[:, w, :].rearrange("p (t c h f) -> p t c h f", t=T, c=2, h=4) for w in range(2)]
    cnt = {"pp": 0, "tp": 0, "st": 0}

    def qkv_for(which, src, groups):
        norm_T(dram_src(src, nA_xt), hT, 16, 1, nA_xn, nA_junk, nA_gbc, "hT")

        pending = []

        def epilogue(n_idx, t, b):
            prev = pending.pop() if pending else None
            epilogue_main(n_idx, t, b)
            if prev is not None:
                prev()

        def epilogue_main(n_idx, t, b):
            grp = groups[n_idx]
            typ = grp // 2
            hh0 = (grp % 2) * 4
            h0 = (0 if typ < 3 else 8) + hh0
            kt = t if which == 0 else 8 + t
            st = cnt["st"] % 2
            cnt["st"] += 1
            S.op("scalar", lambda e, b=b, st=st: e.activation(out=qk_sb[st][:], in_=bank(b), func=AF.Copy),
                 reads=[("ps", b)], writes=[("qk", st)])
            if typ in (2, 5):
                if "nov" in _DBG:
                    return
                S.op("sync", lambda e, st=st, h0=h0, kt=kt: e.dma_start(
                    out=v_dram[h0:h0 + 4, :, kt, :].rearrange("h p d -> p h d"),
                    in_=qk_sb[st][:].rearrange("p (h d) -> p h d", h=4)),
                    reads=[("qk", st)], writes=[("vd", h0, kt)], dma="d_vst%d" % st)
                return
            if typ in (0, 1) and "norope" not in _DBG:
                ps4 = bank(b).rearrange("p (h d) -> p h d", h=4)
                cosv = rope_v[which][:, t, 0, :, :]
                sinv = rope_v[which][:, t, 1, :, :]
                S.op("vector", lambda e, ps4=ps4, cosv=cosv: e.tensor_tensor(
                    out=rt1[:], in0=ps4[:, :, 0:32], in1=cosv, op=ALU.mult),
                    reads=[("ps", b), "rope", ("qk", st)], writes=["rt1"])
                if "ropeA" not in _DBG:
                  S.op("vector", lambda e, ps4=ps4, sinv=sinv: e.tensor_tensor(
                    out=rt2[:, :, 0:16], in0=ps4[:, :, 16:32], in1=sinv[:, :, 0:16], op=ALU.mult),
                    reads=[("ps", b), "rope", ("qk", st)], writes=["rt2"])
                if "ropeA" not in _DBG:
                  S.op("vector", lambda e, ps4=ps4, sinv=sinv: e.tensor_tensor(
                    out=rt2[:, :, 16:32], in0=ps4[:, :, 0:16], in1=sinv[:, :, 16:32], op=ALU.mult),
                    reads=[("ps", b), "rope", ("qk", st)], writes=["rt2"])
                if "ropeA" not in _DBG and "ropeB" not in _DBG:
                  S.op("vector", lambda e, st=st: e.tensor_tensor(
                    out=qk_sb[st][:].rearrange("p (h d) -> p h d", h=4)[:, :, 0:32], in0=rt1[:], in1=rt2[:],
                    op=ALU.add), reads=["rt1", "rt2", ("qk", st)], writes=[("qk", st)])
            if "notr" in _DBG:
                return
            pending.append(lambda: epilogue_tail(typ, st, h0, t, kt))

        def epilogue_tail(typ, st, h0, t, kt):
            tb = 4 + cnt["tp"] % 4
            cnt["tp"] += 1
            for jh in range(4):
                S.op("tensor", lambda e, jh=jh, st=st, tb=tb: e.transpose(
                    out=psb[:, tb * 1024 + jh * 128: tb * 1024 + (jh + 1) * 128],
                    in_=qk_sb[st][:, jh * 128:(jh + 1) * 128], identity=ident_b[:]),
                    reads=[("qk", st)], writes=[("ps", tb)])
            tsrc = psb[:, tb * 1024: tb * 1024 + 512].rearrange("p (h j) -> p h j", h=4)
            if typ in (0, 3):
                S.op("vector", lambda e, tsrc=tsrc, h0=h0, t=t: e.tensor_copy(
                    out=qT[:, h0:h0 + 4, t * 128:(t + 1) * 128], in_=tsrc),
                    reads=[("ps", tb)], writes=[("qT", h0, t)])
            else:
                S.op("vector", lambda e, tsrc=tsrc, st=st: e.tensor_copy(out=kst[st][:], in_=tsrc),
                     reads=[("ps", tb)], writes=[("kst", st)])
                if "nok" not in _DBG:
                  S.op("sync", lambda e, st=st, h0=h0, kt=kt: e.dma_start(
                    out=kT_dram[h0:h0 + 4, :, kt * 128:(kt + 1) * 128].rearrange("h d j -> d h j"),
                    in_=kst[st][:]), reads=[("kst", st)], writes=[("kd", h0, kt)], dma="d_kst%d" % st)

        def bank_fn(n, t):
            b = cnt["pp"] % 4
            cnt["pp"] += 1
            return b

        tok_proj(lambda k, t: hT[:, k, t * 128:(t + 1) * 128], lambda k, t: ("hT", t), KC,
                 win_sb, ["d_win0", "d_win1"], lambda n: win[groups[n]], len(groups), epilogue, bank_fn, "win")
        if pending:
            pending.pop()()
        S.barrier()

    qkv_for(0, x1_par, [2, 3, 4, 5, 8, 9, 10, 11])
    qkv_for(1, x1_own, list(range(12)))
    if stage == 4:
        return finish()

    o_sb = big
    PTB = 4

    def attention():
        items = [(h, i) for h in range(8) for i in range(T)] + [None, None] + \
                [(h, i) for h in range(8, NH) for i in range(T)]
        N = len(items)

        def I(n):
            h, i = items[n]
            nk = 1024 + 128 * (i + 1)
            return dict(h=h, i=i, w=n % 2, w4=n % 4, hs=h % 2, moba=(h < 8), nk=nk, nkt=nk // 128,
                        npast=4 + i // 2, sb=[("ps", bb) for bb in range((nk + 511) // 512)])

        def st_scores(n):
            c = I(n)
            h, i, hs, nk = c["h"], c["i"], c["hs"], c["nk"]
            if i == 0:
                S.op("sync", lambda e: e.dma_start(out=kT_h[hs][:], in_=kT_dram[h]),
                     writes=[("kT", hs)], dma="d_kT%d" % hs)
                S.op("sync", lambda e: e.dma_start(out=v_h[hs][:], in_=v_dram[h]),
                     writes=[("v", hs)], dma="d_v%d" % hs)
                if c["moba"]:
                    S.op("vector", lambda e: e.tensor_reduce(
                        out=km_f[:], in_=kT_h[hs][:].rearrange("p (n k) -> p n k", k=256), axis=AX.X, op=ALU.add),
                        reads=[("kT", hs)], writes=["km_f"])
                    S.op("vector", lambda e: e.tensor_copy(out=km_b[hs][:], in_=km_f[:]),
                         reads=["km_f"], writes=[("km_b", hs)])
                    for ww in range(2):
                        S.op("vector", lambda e, ww=ww: e.memset(g8s[ww], -1e30), writes=[("g8", ww)])
            qTi = qT[:, h, i * 128:(i + 1) * 128]
            mask = masks_b[:, 0:128] if c["moba"] else masks_b[:, 128:256]
            for c0 in range(0, nk, 512):
                wd_ = min(512, nk - c0)
                last = (c0 + wd_ == nk)
                S.op("tensor", lambda e, c0=c0, wd_=wd_, last=last: e.matmul(
                    out=ps[:, c0:c0 + wd_], lhsT=qTi, rhs=kT_h[hs][:, c0:c0 + wd_], start=True, stop=(not last)),
                    reads=[("kT", hs)], writes=[("ps", c0 // 512)])
            S.op("tensor", lambda e: e.matmul(
                out=ps[:, nk - 128:nk], lhsT=ident_b[:], rhs=mask, start=False, stop=True),
                writes=[("ps", (nk - 128) // 512)])
            if c["moba"]:
                S.op("tensor", lambda e: e.matmul(
                    out=ps[:, 6 * 512:6 * 512 + 8], lhsT=qTi, rhs=km_b[hs][:], start=True, stop=True),
                    reads=[("km_b", hs)], writes=[("ps", 6)])

        def st_pre(n):
            c = I(n)
            w, nk, npast = c["w"], c["nk"], c["npast"]
            if c["moba"]:
                g8w, m8w, selw = g8s[w], m8s[w], sels[w]
                S.op("vector", lambda e: e.tensor_scalar(
                    out=g8w[:, 0:4], in0=ps[:, 6 * 512:6 * 512 + 4], scalar1=pbias[:, 0:1], scalar2=None,
                    op0=ALU.add), reads=[("ps", 6)], writes=[("g8", w)])
                if npast > 4:
                    S.op("vector", lambda e: e.tensor_copy(
                        out=g8w[:, 4:npast], in_=ps[:, 6 * 512 + 4:6 * 512 + npast]),
                        reads=[("ps", 6)], writes=[("g8", w)])
                S.op("vector", lambda e: e.max(out=m8w, in_=g8w), reads=[("g8", w)], writes=[("m8", w)])
                S.op("vector", lambda e: e.tensor_scalar(
                    out=selw, in0=g8w, scalar1=m8w[:, 2:3], scalar2=None, op0=ALU.is_ge),
                    reads=[("g8", w), ("m8", w)], writes=[("sel", w)])
                S.op("vector", lambda e: e.tensor_scalar(
                    out=selw, in0=selw, scalar1=-1.0, scalar2=-NEG, op0=ALU.add, op1=ALU.mult),
                    reads=[("sel", w)], writes=[("sel", w)])
                S.op("vector", lambda e: e.tensor_scalar(
                    out=selw[:, 0:4], in0=selw[:, 0:4], scalar1=pbias[:, 0:1], scalar2=None, op0=ALU.add),
                    reads=[("sel", w)], writes=[("sel", w)])
            else:
                S.op("vector", lambda e: e.tensor_scalar(
                    out=zb[w][:, 0:nk], in0=ps[:, 0:nk], scalar1=SCALE, scalar2=None, op0=ALU.mult),
                    reads=c["sb"], writes=[("zb", w)])

        def st_exp1(n):
            c = I(n)
            w, nk, npast = c["w"], c["nk"], c["npast"]
            if c["moba"]:
                selw, rsw, w4 = sels[w], rsums[w], c["w4"]
                for nb in range(npast):
                    S.op("scalar", lambda e, nb=nb: e.activation(
                        out=Pbm[w4][:, nb * 256:(nb + 1) * 256], in_=ps[:, nb * 256:(nb + 1) * 256], func=AF.Exp,
                        scale=SCALE, bias=selw[:, nb:nb + 1], accum_out=rsw[:, nb:nb + 1]),
                        reads=[("ps", nb // 2), ("sel", w)], writes=[("Pbm", w4), ("rsum", w)])
                S.op("scalar", lambda e: e.activation(
                    out=Pbm[w4][:, npast * 256:nk], in_=ps[:, npast * 256:nk], func=AF.Exp, scale=SCALE,
                    accum_out=rsw[:, npast:npast + 1]),
                    reads=c["sb"], writes=[("Pbm", w4), ("rsum", w)])
            else:
                S.op("scalar", lambda e: e.activation(
                    out=EL[w][:, 0:1024], in_=zb[w][:, 0:1024], func=AF.Exp, bias=pbias[:, 0:1]),
                    reads=[("zb", w)], writes=[("EL", w), ("Pbm", 2 * w), ("Pbm", 2 * w + 1)])
                S.op("scalar", lambda e: e.activation(
                    out=EL[w][:, 1024:nk], in_=zb[w][:, 1024:nk], func=AF.Exp),
                    reads=[("zb", w)], writes=[("EL", w)])
                S.op("scalar", lambda e: e.activation(
                    out=EL[w][:, 0:nk], in_=EL[w][:, 0:nk], func=AF.Ln, bias=one_col[:, 0:1]),
                    reads=[("EL", w)], writes=[("EL", w)])

        def st_B(n):
            c = I(n)
            w, w4, nk, npast = c["w"], c["w4"], c["nk"], c["npast"]
            if c["moba"]:
                S.op("vector", lambda e: e.reduce_sum(
                    out=rtot[:, w:w + 1], in_=rsums[w][:, 0:npast + 1], axis=AX.X),
                    reads=[("rsum", w)], writes=[("rtot", w)])
                S.op("vector", lambda e: e.reciprocal(out=rinv4[:, w4:w4 + 1], in_=rtot[:, w:w + 1]),
                     reads=[("rtot", w)], writes=[("rinv", w4)])
            else:
                S.op("vector", lambda e: e.tensor_tensor_scan(
                    out=Cb1[:, 0:nk], data0=ones_b[:, 0:nk], data1=EL[w][:, 0:nk], initial=0.0,
                    op0=ALU.mult, op1=ALU.add), reads=[("EL", w)], writes=["Cb"])
                S.op("vector", lambda e: e.tensor_scalar(
                    out=negT[:, w:w + 1], in0=Cb1[:, nk - 1:nk], scalar1=-1.0, scalar2=None, op0=ALU.mult),
                    reads=["Cb"], writes=[("negT", w)])
                S.op("vector", lambda e: e.tensor_scalar(
                    out=negTp[:, w:w + 1], in0=negT[:, w:w + 1], scalar1=pbias[:, 0:1], scalar2=None, op0=ALU.add),
                    reads=[("negT", w)], writes=[("negTp", w)])
                S.op("gpsimd", lambda e: e.tensor_tensor(
                    out=zb[w][:, 1:nk], in0=zb[w][:, 1:nk], in1=Cb1[:, 0:nk - 1], op=ALU.add),
                    reads=["Cb", ("zb", w)], writes=[("zb", w)])

        def st_expP(n):
            c = I(n)
            w, nk = c["w"], c["nk"]
            if c["moba"]:
                return
            S.op("scalar", lambda e: e.activation(
                out=Pb[w][:, 0:1024], in_=zb[w][:, 0:1024], func=AF.Exp, bias=negTp[:, w:w + 1]),
                reads=[("zb", w), ("negTp", w)], writes=[("Pb", w)])
            S.op("scalar", lambda e: e.activation(
                out=Pb[w][:, 1024:nk], in_=zb[w][:, 1024:nk], func=AF.Exp, bias=negT[:, w:w + 1]),
                reads=[("zb", w), ("negT", w)], writes=[("Pb", w)])

        def st_T(n):
            c = I(n)
            w = c["w"]
            src, res = (Pbm[c["w4"]], ("Pbm", c["w4"])) if c["moba"] else (Pb[w], ("Pb", w))
            for kt in range(c["nkt"]):
                tb = PTB + kt // 8
                S.op("tensor", lambda e, kt=kt: e.transpose(
                    out=psb[:, PTB * 1024 + kt * 128: PTB * 1024 + (kt + 1) * 128],
                    in_=src[:, kt * 128:(kt + 1) * 128], identity=ident_b[:]),
                    reads=[res], writes=[("ps", tb)])

        def st_PTcopy(n):
            c = I(n)
            w, nk = c["w"], c["nk"]
            S.op("vector", lambda e: e.tensor_copy(
                out=PT[w][:, 0:1024], in_=psb[:, PTB * 1024: PTB * 1024 + 1024]),
                reads=[("ps", PTB)], writes=[("PTa", w)])
            S.op("vector", lambda e: e.tensor_copy(
                out=PT[w][:, 1024:nk], in_=psb[:, PTB * 1024 + 1024: PTB * 1024 + nk]),
                reads=[("ps", PTB + 1)], writes=[("PTb", w)])

        def st_PV(n):
            c = I(n)
            w, hs, nkt = c["w"], c["hs"], c["nkt"]
            for kt in range(nkt):
                S.op("tensor", lambda e, kt=kt: e.matmul(
                    out=ps[:, 7 * 512:7 * 512 + 128], lhsT=PT[w][:, kt * 128:(kt + 1) * 128],
                    rhs=v_h[hs][:, kt, :], start=(kt == 0), stop=(kt == nkt - 1)),
                    reads=[("PTa", w), ("PTb", w), ("v", hs)], writes=[("ps", 7)])

        def st_out(n):
            c = I(n)
            h, i, w4 = c["h"], c["i"], c["w4"]
            if c["moba"]:
                S.op("scalar", lambda e: e.activation(
                    out=o_sb[:, i, h * 128:(h + 1) * 128], in_=ps[:, 7 * 512:7 * 512 + 128], func=AF.Copy,
                    scale=rinv4[:, w4:w4 + 1]), reads=[("ps", 7), ("rinv", w4)], writes=[("o", i)])
            else:
                S.op("scalar", lambda e: e.activation(
                    out=o_sb[:, i, h * 128:(h + 1) * 128], in_=ps[:, 7 * 512:7 * 512 + 128], func=AF.Copy),
                    reads=[("ps", 7)], writes=[("o", i)])

        ok = lambda n: 0 <= n < N and items[n] is not None
        for p in range(-2, N + 2):
            if ok(p + 2): st_scores(p + 2)
            if ok(p): st_expP(p)
            if ok(p + 2): st_pre(p + 2)
            if ok(p + 1): st_B(p + 1)
            if ok(p + 2): st_exp1(p + 2)
            if ok(p - 1): st_T(p - 1)
            if ok(p - 1): st_PTcopy(p - 1)
            if ok(p - 2): st_PV(p - 2)
            if ok(p - 2): st_out(p - 2)

    one_col = misc[:, 8:9]
    S.op("vector", lambda e: e.memset(one_col, 1.0), writes=["one"])
    attention()
    S.barrier()
    if debug:
        for t in range(T):
            S.op("sync", lambda e, t=t: e.dma_start(out=o_dbg[t * 128:(t + 1) * 128, :], in_=o_sb[:, t, :]),
                 dma="d_dbg")
        S.barrier()
    if stage == 5:
        return finish()

    oT = hT
    norm_T(lambda t: (o_sb[:, t, :], ("o", t)), oT, 32, 2, nR_xn, nR_junk, nR_gbc, "hT")
    S.barrier()

    def op_epilogue(n, t, b):
        S.op("scalar", lambda e, t=t, n=n, b=b: e.activation(
            out=big[:, t, n * 512:(n + 1) * 512], in_=bank(b), func=AF.Copy),
            reads=[("ps", b)], writes=[("f", t)])

    cnt["pp"] = 0

    def bank_fn2(n, t):
        b = cnt["pp"] % 8
        cnt["pp"] += 1
        return b

    tok_proj(lambda k, t: oT[:, k, t * 128:(t + 1) * 128], lambda k, t: ("hT", t), KC,
             win_sb, ["d_win0", "d_win1"], lambda n: wout[n], 4, op_epilogue, bank_fn2, "win")
    postnorm_residual(big, x1_own, x2_own, 1, 1.0, gpost2)
    S.barrier()

    ffn(1, x2_own, out_d, 48, 2)

    return finish()


_NC_CACHE = {}


def _rope_tables(pos0):
    half = 16
    inv_freq = (500000.0 ** (-np.arange(0, 32, 2, dtype=np.float32) / 32)).astype(np.float32)
    pos = (pos0 + np.arange(1024, dtype=np.float32))
    ang = pos[:, None] * inv_freq[None, :]
    cos = np.cos(ang).astype(np.float32)
    sin = np.sin(ang).astype(np.float32)
    cos2 = np.concatenate([cos, cos], axis=1)
    sinS = np.concatenate([-sin, sin], axis=1)
    tab = np.stack([cos2, sinS], axis=1)
    tab = np.broadcast_to(tab[:, :, None, :], (1024, 2, 4, 32))
    tab = tab.reshape(T, 128, 2, 4, 32).transpose(1, 0, 2, 3, 4)
    return np.ascontiguousarray(tab).reshape(128, T * 2 * 4 * 32)


def prepare_inputs(inputs):
    f = lambda a: np.ascontiguousarray(np.asarray(a, dtype=np.float32))
    x = f(inputs["x"])

    def gu(wg, wu):
        g = f(wg)[0].reshape(KC, 128, NCH, 128).transpose(2, 1, 0, 3)
        u = f(wu)[0].reshape(KC, 128, NCH, 128).transpose(2, 1, 0, 3)
        return np.ascontiguousarray(np.stack([g, u], axis=2))

    def dn(w):
        return np.ascontiguousarray(f(w)[0].reshape(NJ, CPJ, 128, 4, 512).transpose(0, 3, 2, 1, 4))

    def colgrp(w, ng):
        return np.ascontiguousarray(f(w)[0].reshape(KC, 128, ng, 512).transpose(2, 1, 0, 3))

    def gcol(g):
        return f(g).reshape(KC, 128).T

    shared = {
        "wgu1": gu(inputs["ffn1_w_gate"], inputs["ffn1_w_up"]), "wd1": dn(inputs["ffn1_w_down"]),
        "wgu2": gu(inputs["ffn2_w_gate"], inputs["ffn2_w_up"]), "wd2": dn(inputs["ffn2_w_down"]),
        "win": colgrp(inputs["w_in"], 12), "wout": colgrp(inputs["w_out"], 4),
        "gT": np.ascontiguousarray(np.concatenate([
            gcol(inputs["ffn1_pre_g"]), gcol(inputs["mix_pre_g"]),
            gcol(np.concatenate([f(inputs["moba_out_g"])[0], f(inputs["sb_out_g"])[0]])),
            gcol(inputs["ffn2_pre_g"])], axis=1)),
        "gpost": np.ascontiguousarray(np.stack([
            np.broadcast_to(f(inputs[k])[0][None, :], (128, D)) for k in
            ("ffn1_post_g", "mix_post_g", "ffn2_post_g")])),
        "ident": np.eye(128, dtype=np.float32),
    }
    qi = np.arange(128)[:, None]
    ki = np.arange(128)[None, :]
    m_le = np.where(ki <= qi, 0.0, 2 * NEG).astype(np.float32)
    m_lt = np.where(ki < qi, 0.0, 2 * NEG).astype(np.float32)
    shared["masks"] = np.ascontiguousarray(np.concatenate([m_le, m_lt], axis=1))
    ropes = [_rope_tables(0.0), _rope_tables(1024.0)]
    in_maps = []
    for c in range(8):
        b, r = divmod(c, 2)
        m = dict(shared)
        m["x_own"] = np.ascontiguousarray(x[b, r * 1024:(r + 1) * 1024])
        m["x_par"] = np.ascontiguousarray(x[b, (1 - r) * 1024:(2 - r) * 1024])
        m["rope"] = np.ascontiguousarray(np.stack([ropes[1 - r], ropes[r]]))
        m["pbias"] = np.full((128, 1), NEG if r == 0 else 0.0, dtype=np.float32)
        in_maps.append(m)
    return in_maps


def kernel(**inputs):
    in_maps = prepare_inputs(inputs)
    if "nc" not in _NC_CACHE:
        _NC_CACHE["nc"] = build_nc(False)
    res = run_bass_kernel_spmd(_NC_CACHE["nc"], in_maps, core_ids=list(range(8)))
    out = np.empty((4, 2048, D), dtype=np.float32)
    for c in range(8):
        b, r = divmod(c, 2)
        out[b, r * 1024:(r + 1) * 1024] = res.results[c]["out"]
    return out
```

```python
import numpy as np
import concourse.bass as bass
import concourse.mybir as mybir
from concourse.bass_utils import run_bass_kernel_spmd

F32 = mybir.dt.float32
BF16 = mybir.dt.bfloat16
AF = mybir.ActivationFunctionType
ALU = mybir.AluOpType
AX = mybir.AxisListType

D = 2048
DFF = 5632
NCH = DFF // 128
NJ = 4
CPJ = NCH // NJ
T = 8
KC = D // 128
NH = 16
EPS = 1e-6
SCALE = 128 ** -0.5
NEG = -30000.0
KB = 1024
SB_BASE = 16512

SAME_ENGINE_SYNC = {"vector": True, "scalar": True, "gpsimd": True, "tensor": False, "sync": False}


class Sched:
    def __init__(self, nc):
        self.nc = nc
        self.engines = ["sync", "scalar", "vector", "gpsimd", "tensor"]
        self.lists = {e: [] for e in self.engines}
        self.cnt = {}
        self.semh = {}
        self.seen = {e: {} for e in self.engines}
        self.lastw = {}
        self.readers = {}
        self._stack = []

    def add_sem(self, key):
        cm = self.nc.semaphore("s_" + key)
        h = cm.__enter__()
        self._stack.append(cm)
        self.semh[key] = h
        self.cnt[key] = 0

    def close(self):
        for cm in reversed(self._stack):
            cm.__exit__(None, None, None)

    def _waits(self, engine, deps):
        need = {}
        for (sk, v) in deps:
            if v > need.get(sk, 0):
                need[sk] = v
        out = []
        for sk, v in need.items():
            if sk == engine and not SAME_ENGINE_SYNC[engine]:
                continue
            if self.seen[engine].get(sk, 0) >= v:
                continue
            self.seen[engine][sk] = v
            out.append((sk, v))
        return out

    def op(self, engine, fn, reads=(), writes=(), dma=None):
        deps = []
        for r in reads:
            t = self.lastw.get(r)
            if t:
                deps.append(t)
        for w in writes:
            t = self.lastw.get(w)
            if t:
                deps.append(t)
            deps.extend(self.readers.get(w, ()))
        waits = self._waits(engine, deps)
        if dma is None:
            self.cnt[engine] += 1
            tok = (engine, self.cnt[engine])
            inc = (engine, 1)
        else:
            self.cnt[dma] += 16
            tok = (dma, self.cnt[dma])
            inc = (dma, 16)
        self.lists[engine].append((waits, fn, inc))
        for r in reads:
            self.readers.setdefault(r, []).append(tok)
        for w in writes:
            self.lastw[w] = tok
            self.readers[w] = []
        return tok

    def barrier(self):
        for e in self.engines:
            deps = [(k, v) for k, v in self.cnt.items() if v > 0]
            waits = self._waits(e, deps)
            if waits:
                self.lists[e].append((waits, None, None))
        self.lastw = {}
        self.readers = {}

    def emit(self, block):
        nc = self.nc

        def run(eng, items):
            for waits, fn, inc in items:
                for sk, v in waits:
                    eng.wait_ge(self.semh[sk], v)
                if fn is not None:
                    ins = fn(eng)
                    ins.then_inc(self.semh[inc[0]], inc[1])

        @block.sync
        def _(e):
            run(e, self.lists["sync"])

        @block.scalar
        def _(e):
            run(e, self.lists["scalar"])

        @block.vector
        def _(e):
            run(e, self.lists["vector"])

        @block.gpsimd
        def _(e):
            run(e, self.lists["gpsimd"])

        @block.tensor
        def _(e):
            run(e, self.lists["tensor"])


import os as _os
_DBG = set(_os.environ.get("KDBG", "").split(","))


def build_nc(debug=False, stage=99):
    nc = bass.Bass("TRN2", target_bir_lowering=False)
    S = Sched(nc)

    def din(name, shape, dt=F32):
        if stage <= 3 and name in ("wgu2", "wd2", "win", "wout", "rope"):
            return None
        if stage <= 5 and name in ("wgu2", "wd2", "wout"):
            return None
        return nc.dram_tensor(name, list(shape), dt, kind="ExternalInput").ap()

    x_own = din("x_own", [T * 128, D])
    x_par = din("x_par", [T * 128, D])
    wgu = [din("wgu1", [NCH, 128, 2, KC, 128]), din("wgu2", [NCH, 128, 2, KC, 128])]
    wd = [din("wd1", [NJ, 4, 128, CPJ, 512]), din("wd2", [NJ, 4, 128, CPJ, 512])]
    win = din("win", [12, 128, KC, 512])
    wout = din("wout", [4, 128, KC, 512])
    gT_d = din("gT", [128, 64])
    gpost_d = din("gpost", [3, 128, D])
    rope_d = din("rope", [2, 128, T * 2 * 4 * 32])
    ident_d = din("ident", [128, 128])
    masks_d = din("masks", [128, 2 * 128])
    pbias_d = din("pbias", [128, 1])
    out_d = nc.dram_tensor("out", [T * 128, D], F32, kind="ExternalOutput").ap()

    dk = "ExternalOutput" if debug else "Internal"
    x1_own = nc.dram_tensor("x1_own", [T * 128, D], F32, kind=dk).ap()
    x1_par = nc.dram_tensor("x1_par", [T * 128, D], F32, kind=dk).ap()
    x2_own = nc.dram_tensor("x2_own", [T * 128, D], F32, kind=dk).ap()
    kT_dram = nc.dram_tensor("kT_dram", [NH, 128, 2048], BF16, kind="Internal").ap()
    v_dram = nc.dram_tensor("v_dram", [NH, 128, 16, 128], BF16, kind="Internal").ap()
    o_dbg = nc.dram_tensor("o_dbg", [T * 128, D], F32, kind=dk).ap() if debug else None

    def sb(name, shape, dt, off):
        return nc.alloc_sbuf_tensor_at(name, list(shape), dt, offset=SB_BASE + off)

    o = 0
    ident_f = sb("ident_f", [128, 128], F32, o); o += 512
    ident_b = sb("ident_b", [128, 128], BF16, o); o += 256
    masks_b = sb("masks_b", [128, 256], BF16, o); o += 512
    gT = sb("gT", [128, 64], F32, o); o += 256
    pbias = sb("pbias", [128, 1], F32, o); o += 32
    small = sb("small", [128, 96], F32, o); o += 384
    ones_b = sb("ones_b", [128, 2048], BF16, o); o += 4096
    small2 = sb("small2", [128, 64], F32, o); o += 256
    CONST_END = 6 * KB + 512
    assert o <= CONST_END
    A0 = CONST_END
    B0 = A0 + 64 * KB
    C0 = B0 + 32 * KB
    R0 = C0 + 32 * KB
    big = sb("big", [128, T, D], F32, A0)
    hT = sb("hT", [128, KC, 1024], BF16, B0)
    qT = sb("qT", [128, NH, 1024], BF16, C0)
    aT = sb("aT", [128, CPJ, 1024], BF16, C0)
    sg = [sb("sg%d" % i, [128, 1024], F32, C0 + 22 * KB + i * 4 * KB) for i in range(2)]
    r = R0
    wgu_sb = [sb("wgu_sb%d" % i, [128, 2, KC, 128], BF16, r + i * 8 * KB) for i in range(3)]
    r += 24 * KB
    wd_sb = [sb("wd_sb%d" % i, [128, CPJ, 512], BF16, r + i * 11 * KB) for i in range(2)]
    r += 22 * KB
    xt = sb("xt", [128, D], F32, r); r += 8 * KB
    xn = sb("xn", [128, D], F32, r); r += 8 * KB
    gpost = sb("gpost", [128, D], F32, r); r += 8 * KB
    FFN_END = r
    r = R0
    win_sb = [sb("win_sb%d" % i, [128, KC, 512], BF16, r + i * 16 * KB) for i in range(2)]
    r += 32 * KB
    rope_sb = sb("rope_sb", [128, 2, T * 2 * 4 * 32], F32, r); r += 16 * KB
    xt2 = sb("xt2", [128, D], F32, r); r += 8 * KB
    xn2 = sb("xn2", [128, D], F32, r); r += 8 * KB
    gpost2 = sb("gpost2", [128, D], F32, R0 + 32 * KB)
    qk_sb = [sb("qk_sb%d" % i, [128, 512], BF16, r + i * KB) for i in range(2)]; r += 2 * KB
    kst = [sb("kst%d" % i, [128, 4, 128], BF16, r + i * KB) for i in range(2)]; r += 2 * KB
    rt1 = sb("rt1", [128, 4, 32], F32, r); r += 512
    rt2 = sb("rt2", [128, 4, 32], F32, r); r += 512
    QKV_END = r
    r = R0
    kT_h = [sb("kT_h%d" % i, [128, 2048], BF16, r + i * 4 * KB) for i in range(2)]; r += 8 * KB
    v_h = [sb("v_h%d" % i, [128, 16, 128], BF16, r + i * 4 * KB) for i in range(2)]; r += 8 * KB
    EL = [sb("EL%d" % i, [128, 2048], F32, r + i * 8 * KB) for i in range(2)]; r += 16 * KB
    Pbm = [sb("Pbm%d" % i, [128, 2048], BF16, r - 16 * KB + i * 4 * KB) for i in range(4)]
    zb = [sb("zb%d" % i, [128, 2048], F32, r + i * 8 * KB) for i in range(2)]; r += 16 * KB
    Cb1 = sb("Cb1", [128, 2048], F32, r); r += 8 * KB
    Pb = [sb("Pb%d" % i, [128, 2048], BF16, r + i * 4 * KB) for i in range(2)]; r += 8 * KB
    PT = [sb("PT%d" % i, [128, 2048], BF16, r + i * 4 * KB) for i in range(2)]; r += 8 * KB
    km_f = sb("km_f", [128, 8], F32, r); r += 32
    km_b = [sb("km_b%d" % i, [128, 8], BF16, r + 32 * i) for i in range(2)]; r += 64
    ATT_END = r
    LIMIT = 229376 - SB_BASE
    assert max(FFN_END, QKV_END, ATT_END) <= LIMIT, (FFN_END, QKV_END, ATT_END)

    ps = nc.alloc_psum_tensor("ps", [128, 4096], F32)
    psb = ps.bitcast(BF16)

    def bank(b, w=512, off=0):
        return ps[:, b * 512 + off: b * 512 + off + w]

    for k in ["sync", "scalar", "vector", "gpsimd", "tensor"]:
        S.add_sem(k)
    for k in ["d_const", "d_const_sw", "d_xt0", "d_xt1", "d_xt2", "d_out0", "d_out1", "d_gpost", "d_out", "d_wgu0", "d_wgu1", "d_wgu2", "d_wd0", "d_wd1",
              "d_win0", "d_win1", "d_rope", "d_kst0", "d_kst1", "d_vst0", "d_vst1",
              "d_kT0", "d_kT1", "d_v0", "d_v1", "d_dbg"]:
        S.add_sem(k)

    ss = small[:, 0:16]
    rs = small[:, 16:32]
    rstd = small[:, 32:48]
    g8 = small[:, 48:56]
    m8 = small[:, 56:64]
    selb = small[:, 64:72]
    rsum = small[:, 72:80]
    misc = small[:, 80:96]
    g8s = [small2[:, 0:8], small2[:, 8:16]]
    m8s = [small2[:, 16:24], small2[:, 24:32]]
    sels = [small2[:, 32:40], small2[:, 40:48]]
    rsums = [small[:, 64:72], small[:, 72:80]]
    rtot = small2[:, 48:50]
    rinv4 = small2[:, 50:54]
    negT = small2[:, 54:56]
    negTp = small2[:, 56:58]

    S.op("sync", lambda e: e.dma_start(out=ident_f[:], in_=ident_d[:, :]), writes=["ident_f"], dma="d_const")
    S.op("sync", lambda e: e.dma_start(out=gT[:], in_=gT_d[:, :]), writes=["gT"], dma="d_const")
    S.op("sync", lambda e: e.dma_start(out=pbias[:], in_=pbias_d[:, :]), writes=["pbias"], dma="d_const")
    S.op("gpsimd", lambda e: e.dma_start(out=ident_b[:], in_=ident_d[:, :]), writes=["ident_b"], dma="d_const_sw")
    S.op("gpsimd", lambda e: e.dma_start(out=masks_b[:], in_=masks_d[:, :]), writes=["masks_b"], dma="d_const_sw")
    S.op("vector", lambda e: e.memset(ones_b[:], 1.0), writes=["ones_b"])
    S.barrier()

    nA_xt = [sb("nA_xt%d" % i, [128, D], F32, A0 + i * 8 * KB) for i in range(3)]
    nA_xn = [sb("nA_xn%d" % i, [128, D], F32, A0 + 24 * KB + i * 8 * KB) for i in range(2)]
    nA_junk = sb("nA_junk", [128, D], BF16, A0 + 40 * KB)
    nA_gbc = sb("nA_gbc", [128, KC, 128], F32, A0 + 44 * KB)
    nR_xn = [sb("nR_xn%d" % i, [128, D], F32, R0 + 32 * KB + i * 8 * KB) for i in range(2)]
    nR_junk = sb("nR_junk", [128, D], BF16, R0 + 48 * KB)
    nR_gbc = sb("nR_gbc", [128, KC, 128], F32, R0 + 52 * KB)
    pB_xt = [sb("pB_xt%d" % i, [128, D], F32, B0 + i * 8 * KB) for i in range(2)]
    pB_out = [sb("pB_out%d" % i, [128, D], F32, B0 + 16 * KB + i * 8 * KB) for i in range(2)]
    pC_junk = sb("pC_junk", [128, D], BF16, C0)

    def norm_T(get_src, dst_T, gcol0, ngroups, xn_ring, junk, gbc, tag):
        gw = D // ngroups
        for kc in range(KC):
            S.op("vector", lambda e, kc=kc: e.tensor_scalar(
                out=gbc[:, kc, :], in0=ones_b[:, 0:128], scalar1=gT[:, gcol0 + kc:gcol0 + kc + 1], scalar2=None,
                op0=ALU.mult), writes=["gbc"])
        srcs = {}

        def S1(t):
            src_ap, src_res = get_src(t)
            k = t % len(xn_ring)
            xn_ = xn_ring[k]
            for g in range(ngroups):
                S.op("scalar", lambda e, g=g: e.activation(
                    out=junk[:, 0:gw], in_=src_ap[:, g * gw:(g + 1) * gw], func=AF.Square,
                    accum_out=ss[:, 2 * t + g: 2 * t + g + 1]), reads=[src_res], writes=["junk", ("ss", t)])
            S.op("scalar", lambda e: e.activation(
                out=rs[:, 2 * t:2 * t + ngroups], in_=ss[:, 2 * t:2 * t + ngroups], func=AF.Sqrt,
                scale=1.0 / gw, bias=eps_col[:, 0:1]), reads=[("ss", t)], writes=[("rs", t)])
            S.op("vector", lambda e: e.reciprocal(out=rstd[:, 2 * t:2 * t + ngroups], in_=rs[:, 2 * t:2 * t + ngroups]),
                 reads=[("rs", t)], writes=[("rstd", t)])
            for g in range(ngroups):
                S.op("scalar", lambda e, g=g: e.activation(
                    out=xn_[:, g * gw:(g + 1) * gw], in_=src_ap[:, g * gw:(g + 1) * gw], func=AF.Copy,
                    scale=rstd[:, 2 * t + g:2 * t + g + 1]), reads=[src_res, ("rstd", t)], writes=[("xn", k)])

        def S2(t):
            k = t % len(xn_ring)
            xn_ = xn_ring[k]
            b0 = 4 * (t % 2)
            for kc in range(KC):
                b = b0 + kc // 4
                S.op("tensor", lambda e, kc=kc, b=b: e.transpose(
                    out=bank(b, 128, (kc % 4) * 128), in_=xn_[:, kc * 128:(kc + 1) * 128], identity=ident_f[:]),
                    reads=[("xn", k)], writes=[("ps", b)])

        def S3(t):
            b0 = 4 * (t % 2)
            for q in range(4):
                b = b0 + q
                S.op("vector", lambda e, q=q, b=b: e.tensor_tensor(
                    out=dst_T[:, 4 * q:4 * q + 4, t * 128:(t + 1) * 128],
                    in0=bank(b).rearrange("p (k j) -> p k j", k=4), in1=gbc[:, 4 * q:4 * q + 4, :], op=ALU.mult),
                    reads=[("ps", b), "gbc"], writes=[(tag, t)])

        for p_ in range(-1, T):
            if p_ + 1 < T:
                S1(p_ + 1)
            if p_ >= 0:
                S2(p_)
                S3(p_)

    eps_col = misc[:, 0:1]
    S.op("vector", lambda e: e.memset(eps_col, EPS), writes=["eps"])
    S.barrier()

    def dram_src(src, ring):
        def get(t):
            k = t % len(ring)
            S.op("sync", lambda e: e.dma_start(out=ring[k][:], in_=src[t * 128:(t + 1) * 128, :]),
                 writes=[("xt", k)], dma="d_xt%d" % k)
            return ring[k], ("xt", k)
        return get

    def postnorm_residual(fsb, src, dst, gidx, res_scale, gp_):
        S.op("sync", lambda e: e.dma_start(out=gp_[:], in_=gpost_d[gidx, :, :]), writes=["gpost"], dma="d_gpost")

        alias_B = [("hT", tt) for tt in range(T)]
        alias_C = [("aT", cc) for cc in range(CPJ)] + [("sg", 0), ("sg", 1)]

        def P1(t):
            k = t % 2
            S.op("sync", lambda e: e.dma_start(out=pB_xt[k][:], in_=src[t * 128:(t + 1) * 128, :]),
                 writes=[("xt", k)] + (alias_B if t < 2 else []), dma="d_xt%d" % k)
            S.op("scalar", lambda e: e.activation(
                out=pC_junk[:], in_=fsb[:, t, :], func=AF.Square, accum_out=ss[:, t:t + 1]),
                reads=[("f", t)], writes=["junk", ("ss", t)] + (alias_C if t == 0 else []))
            S.op("scalar", lambda e: e.activation(
                out=rs[:, t:t + 1], in_=ss[:, t:t + 1], func=AF.Sqrt, scale=1.0 / D, bias=eps_col[:, 0:1]),
                reads=[("ss", t)], writes=[("rs", t)])
            S.op("vector", lambda e: e.reciprocal(out=rstd[:, t:t + 1], in_=rs[:, t:t + 1]),
                 reads=[("rs", t)], writes=[("rstd", t)])
            S.op("vector", lambda e: e.tensor_scalar(
                out=rstd[:, 8 + t:9 + t], in0=rstd[:, t:t + 1], scalar1=float(res_scale), scalar2=None, op0=ALU.mult),
                reads=[("rstd", t)], writes=[("rstds", t)])

        def P2(t):
            k = t % 2
            S.op("vector", lambda e: e.scalar_tensor_tensor(
                out=pB_out[k][:], in0=fsb[:, t, :], scalar=rstd[:, 8 + t:9 + t], in1=gp_[:], op0=ALU.mult, op1=ALU.mult),
                reads=[("f", t), ("rstds", t), "gpost"], writes=[("po", k), ("po2", k)] + (alias_B if t < 2 else []))
            S.op("gpsimd", lambda e: e.tensor_tensor(
                out=pB_out[k][:, 0:1024], in0=pB_out[k][:, 0:1024], in1=pB_xt[k][:, 0:1024], op=ALU.add),
                reads=[("po", k), ("xt", k)], writes=[("po", k)])
            S.op("vector", lambda e: e.tensor_tensor(
                out=pB_out[k][:, 1024:2048], in0=pB_out[k][:, 1024:2048], in1=pB_xt[k][:, 1024:2048], op=ALU.add),
                reads=[("po2", k), ("xt", k)], writes=[("po2", k)])
            S.op("sync", lambda e: e.dma_start(out=dst[t * 128:(t + 1) * 128, :], in_=pB_out[k][:]),
                 reads=[("po", k), ("po2", k)], writes=[("dst", t)], dma="d_out%d" % k)

        for p_ in range(-1, T):
            if p_ + 1 < T:
                P1(p_ + 1)
            if p_ >= 0:
                P2(p_)


    def tok_proj(lhsT_fn, lhs_res_fn, nk, w_slots, w_sems, w_src_fn, ngrp, epilogue, bank_fn, w_tag):
        for n in range(ngrp):
            sl = n % len(w_slots)
            S.op("gpsimd", lambda e, n=n, sl=sl: e.dma_start(out=w_slots[sl][:], in_=w_src_fn(n)),
                 writes=[(w_tag, sl)], dma=w_sems[sl])
            for t in range(T):
                b = bank_fn(n, t)
                for k in range(nk):
                    S.op("tensor", lambda e, k=k, t=t, b=b, sl=sl: e.matmul(
                        out=bank(b), lhsT=lhsT_fn(k, t), rhs=w_slots[sl][:, k, :], start=(k == 0), stop=(k == nk - 1)),
                        reads=[(w_tag, sl), lhs_res_fn(k, t)], writes=[("ps", b)])
                epilogue(n, t, b)

    def ffn(idx, src, dst, gcol0, gidx, sub=99):
        Wgu, Wd = wgu[idx], wd[idx]
        norm_T(dram_src(src, nA_xt), hT, gcol0, 1, nA_xn, nA_junk, nA_gbc, "hT")
        if sub < 1:
            S.barrier()
            return
        for j in range(NJ):
            for cc in range(CPJ):
                c = j * CPJ + cc
                sl = c % 3
                par = c % 2
                S.op("gpsimd", lambda e, c=c, sl=sl: e.dma_start(out=wgu_sb[sl][:], in_=Wgu[c]),
                     writes=[("wgu", sl)], dma="d_wgu%d" % sl)
                for gu in range(2):
                    for kc in range(KC):
                        for half in range(2):
                            b = par * 4 + gu * 2 + half
                            S.op("tensor", lambda e, gu=gu, kc=kc, half=half, b=b, sl=sl: e.matmul(
                                out=bank(b), lhsT=wgu_sb[sl][:, gu, kc, :], rhs=hT[:, kc, half * 512:(half + 1) * 512],
                                start=(kc == 0), stop=(kc == KC - 1)),
                                reads=[("wgu", sl)] + [("hT", tt) for tt in range(half * 4, half * 4 + 4)],
                                writes=[("ps", b)])
                bg = par * 4
                S.op("scalar", lambda e, bg=bg, par=par: e.activation(
                    out=sg[par][:], in_=ps[:, bg * 512: bg * 512 + 1024], func=AF.Silu),
                    reads=[("ps", bg), ("ps", bg + 1)], writes=[("sg", par)])
                S.op("vector", lambda e, bg=bg, par=par, cc=cc: e.tensor_tensor(
                    out=aT[:, cc, :], in0=sg[par][:], in1=ps[:, (bg + 2) * 512:(bg + 2) * 512 + 1024], op=ALU.mult),
                    reads=[("sg", par), ("ps", bg + 2), ("ps", bg + 3)], writes=[("aT", cc)])
            for n in range(4):
                sl = (j * 4 + n) % 2
                S.op("gpsimd", lambda e, j=j, n=n, sl=sl: e.dma_start(out=wd_sb[sl][:], in_=Wd[j, n]),
                     writes=[("wd", sl)], dma="d_wd%d" % sl)
                for t in range(T):
                    for cc in range(CPJ):
                        S.op("tensor", lambda e, t=t, cc=cc, sl=sl: e.matmul(
                            out=bank(t), lhsT=aT[:, cc, t * 128:(t + 1) * 128], rhs=wd_sb[sl][:, cc, :],
                            start=(cc == 0), stop=(cc == CPJ - 1)),
                            reads=[("wd", sl), ("aT", cc)], writes=[("ps", t)])
                    if j == 0:
                        S.op("scalar", lambda e, t=t, n=n: e.activation(
                            out=big[:, t, n * 512:(n + 1) * 512], in_=bank(t), func=AF.Copy),
                            reads=[("ps", t)], writes=[("f", t)])
                    else:
                        S.op("vector", lambda e, t=t, n=n: e.tensor_tensor(
                            out=big[:, t, n * 512:(n + 1) * 512], in0=big[:, t, n * 512:(n + 1) * 512],
                            in1=bank(t), op=ALU.add),
                            reads=[("ps", t), ("f", t)], writes=[("f", t)])
        postnorm_residual(big, src, dst, gidx, 0.5, gpost)
        S.barrier()

    def finish():
        with nc.Block() as block:
            S.emit(block)
        S.close()
        return nc

    if stage == 0:
        return finish()
    if stage == 1:
        ffn(0, x_par, x1_par, 0, 0, sub=0)
        return finish()
    ffn(0, x_par, x1_par, 0, 0)
    if stage == 2:
        return finish()
    ffn(0, x_own, x1_own, 0, 0)
    if stage == 3:
        return finish()

    S.op("sync", lambda e: e.dma_start(out=rope_sb[:], in_=rope_d.rearrange("w p f -> p w f")),
         writes=["rope"], dma="d_rope")
    rope_v = [rope_sb[:, w, :].rearrange("p (t c h f) -> p t c h f", t=T, c=2, h=4) for w in range(2)]
    cnt = {"pp": 0, "tp": 0, "st": 0}

    def qkv_for(which, src, groups):
        norm_T(dram_src(src, nA_xt), hT, 16, 1, nA_xn, nA_junk, nA_gbc, "hT")

        pending = []

        def epilogue(n_idx, t, b):
            prev = pending.pop() if pending else None
            epilogue_main(n_idx, t, b)
            if prev is not None:
                prev()

        def epilogue_main(n_idx, t, b):
            grp = groups[n_idx]
            typ = grp // 2
            hh0 = (grp % 2) * 4
            h0 = (0 if typ < 3 else 8) + hh0
            kt = t if which == 0 else 8 + t
            st = cnt["st"] % 2
            cnt["st"] += 1
            S.op("scalar", lambda e, b=b, st=st: e.activation(out=qk_sb[st][:], in_=bank(b), func=AF.Copy),
                 reads=[("ps", b)], writes=[("qk", st)])
            if typ in (2, 5):
                if "nov" in _DBG:
                    return
                S.op("sync", lambda e, st=st, h0=h0, kt=kt: e.dma_start(
                    out=v_dram[h0:h0 + 4, :, kt, :].rearrange("h p d -> p h d"),
                    in_=qk_sb[st][:].rearrange("p (h d) -> p h d", h=4)),
                    reads=[("qk", st)], writes=[("vd", h0, kt)], dma="d_vst%d" % st)
                return
            if typ in (0, 1) and "norope" not in _DBG:
                ps4 = bank(b).rearrange("p (h d) -> p h d", h=4)
                cosv = rope_v[which][:, t, 0, :, :]
                sinv = rope_v[which][:, t, 1, :, :]
                S.op("vector", lambda e, ps4=ps4, cosv=cosv: e.tensor_tensor(
                    out=rt1[:], in0=ps4[:, :, 0:32], in1=cosv, op=ALU.mult),
                    reads=[("ps", b), "rope", ("qk", st)], writes=["rt1"])
                if "ropeA" not in _DBG:
                  S.op("vector", lambda e, ps4=ps4, sinv=sinv: e.tensor_tensor(
                    out=rt2[:, :, 0:16], in0=ps4[:, :, 16:32], in1=sinv[:, :, 0:16], op=ALU.mult),
                    reads=[("ps", b), "rope", ("qk", st)], writes=["rt2"])
                if "ropeA" not in _DBG:
                  S.op("vector", lambda e, ps4=ps4, sinv=sinv: e.tensor_tensor(
                    out=rt2[:, :, 16:32], in0=ps4[:, :, 0:16], in1=sinv[:, :, 16:32], op=ALU.mult),
                    reads=[("ps", b), "rope", ("qk", st)], writes=["rt2"])
                if "ropeA" not in _DBG and "ropeB" not in _DBG:
                  S.op("vector", lambda e, st=st: e.tensor_tensor(
                    out=qk_sb[st][:].rearrange("p (h d) -> p h d", h=4)[:, :, 0:32], in0=rt1[:], in1=rt2[:],
                    op=ALU.add), reads=["rt1", "rt2", ("qk", st)], writes=[("qk", st)])
            if "notr" in _DBG:
                return
            pending.append(lambda: epilogue_tail(typ, st, h0, t, kt))

        def epilogue_tail(typ, st, h0, t, kt):
            tb = 4 + cnt["tp"] % 4
            cnt["tp"] += 1
            for jh in range(4):
                S.op("tensor", lambda e, jh=jh, st=st, tb=tb: e.transpose(
                    out=psb[:, tb * 1024 + jh * 128: tb * 1024 + (jh + 1) * 128],
                    in_=qk_sb[st][:, jh * 128:(jh + 1) * 128], identity=ident_b[:]),
                    reads=[("qk", st)], writes=[("ps", tb)])
            tsrc = psb[:, tb * 1024: tb * 1024 + 512].rearrange("p (h j) -> p h j", h=4)
            if typ in (0, 3):
                S.op("vector", lambda e, tsrc=tsrc, h0=h0, t=t: e.tensor_copy(
                    out=qT[:, h0:h0 + 4, t * 128:(t + 1) * 128], in_=tsrc),
                    reads=[("ps", tb)], writes=[("qT", h0, t)])
            else:
                S.op("vector", lambda e, tsrc=tsrc, st=st: e.tensor_copy(out=kst[st][:], in_=tsrc),
                     reads=[("ps", tb)], writes=[("kst", st)])
                if "nok" not in _DBG:
                  S.op("sync", lambda e, st=st, h0=h0, kt=kt: e.dma_start(
                    out=kT_dram[h0:h0 + 4, :, kt * 128:(kt + 1) * 128].rearrange("h d j -> d h j"),
                    in_=kst[st][:]), reads=[("kst", st)], writes=[("kd", h0, kt)], dma="d_kst%d" % st)

        def bank_fn(n, t):
            b = cnt["pp"] % 4
            cnt["pp"] += 1
            return b

        tok_proj(lambda k, t: hT[:, k, t * 128:(t + 1) * 128], lambda k, t: ("hT", t), KC,
                 win_sb, ["d_win0", "d_win1"], lambda n: win[groups[n]], len(groups), epilogue, bank_fn, "win")
        if pending:
            pending.pop()()
        S.barrier()

    qkv_for(0, x1_par, [2, 3, 4, 5, 8, 9, 10, 11])
    qkv_for(1, x1_own, list(range(12)))
    if stage == 4:
        return finish()

    o_sb = big
    PTB = 4

    def attention():
        items = [(h, i) for h in range(8) for i in range(T)] + [None, None] + \
                [(h, i) for h in range(8, NH) for i in range(T)]
        N = len(items)

        def I(n):
            h, i = items[n]
            nk = 1024 + 128 * (i + 1)
            return dict(h=h, i=i, w=n % 2, w4=n % 4, hs=h % 2, moba=(h < 8), nk=nk, nkt=nk // 128,
                        npast=4 + i // 2, sb=[("ps", bb) for bb in range((nk + 511) // 512)])

        def st_scores(n):
            c = I(n)
            h, i, hs, nk = c["h"], c["i"], c["hs"], c["nk"]
            if i == 0:
                S.op("sync", lambda e: e.dma_start(out=kT_h[hs][:], in_=kT_dram[h]),
                     writes=[("kT", hs)], dma="d_kT%d" % hs)
                S.op("sync", lambda e: e.dma_start(out=v_h[hs][:], in_=v_dram[h]),
                     writes=[("v", hs)], dma="d_v%d" % hs)
                if c["moba"]:
                    S.op("vector", lambda e: e.tensor_reduce(
                        out=km_f[:], in_=kT_h[hs][:].rearrange("p (n k) -> p n k", k=256), axis=AX.X, op=ALU.add),
                        reads=[("kT", hs)], writes=["km_f"])
                    S.op("vector", lambda e: e.tensor_copy(out=km_b[hs][:], in_=km_f[:]),
                         reads=["km_f"], writes=[("km_b", hs)])
                    for ww in range(2):
                        S.op("vector", lambda e, ww=ww: e.memset(g8s[ww], -1e30), writes=[("g8", ww)])
            qTi = qT[:, h, i * 128:(i + 1) * 128]
            mask = masks_b[:, 0:128] if c["moba"] else masks_b[:, 128:256]
            for c0 in range(0, nk, 512):
                wd_ = min(512, nk - c0)
                last = (c0 + wd_ == nk)
                S.op("tensor", lambda e, c0=c0, wd_=wd_, last=last: e.matmul(
                    out=ps[:, c0:c0 + wd_], lhsT=qTi, rhs=kT_h[hs][:, c0:c0 + wd_], start=True, stop=(not last)),
                    reads=[("kT", hs)], writes=[("ps", c0 // 512)])
            S.op("tensor", lambda e: e.matmul(
                out=ps[:, nk - 128:nk], lhsT=ident_b[:], rhs=mask, start=False, stop=True),
                writes=[("ps", (nk - 128) // 512)])
            if c["moba"]:
                S.op("tensor", lambda e: e.matmul(
                    out=ps[:, 6 * 512:6 * 512 + 8], lhsT=qTi, rhs=km_b[hs][:], start=True, stop=True),
                    reads=[("km_b", hs)], writes=[("ps", 6)])

        def st_pre(n):
            c = I(n)
            w, nk, npast = c["w"], c["nk"], c["npast"]
            if c["moba"]:
                g8w, m8w, selw = g8s[w], m8s[w], sels[w]
                S.op("vector", lambda e: e.tensor_scalar(
                    out=g8w[:, 0:4], in0=ps[:, 6 * 512:6 * 512 + 4], scalar1=pbias[:, 0:1], scalar2=None,
                    op0=ALU.add), reads=[("ps", 6)], writes=[("g8", w)])
                if npast > 4:
                    S.op("vector", lambda e: e.tensor_copy(
                        out=g8w[:, 4:npast], in_=ps[:, 6 * 512 + 4:6 * 512 + npast]),
                        reads=[("ps", 6)], writes=[("g8", w)])
                S.op("vector", lambda e: e.max(out=m8w, in_=g8w), reads=[("g8", w)], writes=[("m8", w)])
                S.op("vector", lambda e: e.tensor_scalar(
                    out=selw, in0=g8w, scalar1=m8w[:, 2:3], scalar2=None, op0=ALU.is_ge),
                    reads=[("g8", w), ("m8", w)], writes=[("sel", w)])
                S.op("vector", lambda e: e.tensor_scalar(
                    out=selw, in0=selw, scalar1=-1.0, scalar2=-NEG, op0=ALU.add, op1=ALU.mult),
                    reads=[("sel", w)], writes=[("sel", w)])
                S.op("vector", lambda e: e.tensor_scalar(
                    out=selw[:, 0:4], in0=selw[:, 0:4], scalar1=pbias[:, 0:1], scalar2=None, op0=ALU.add),
                    reads=[("sel", w)], writes=[("sel", w)])
            else:
                S.op("vector", lambda e: e.tensor_scalar(
                    out=zb[w][:, 0:nk], in0=ps[:, 0:nk], scalar1=SCALE, scalar2=None, op0=ALU.mult),
                    reads=c["sb"], writes=[("zb", w), ("zbh", w)])

        def st_exp1(n):
            c = I(n)
            w, nk, npast = c["w"], c["nk"], c["npast"]
            if c["moba"]:
                selw, rsw, w4 = sels[w], rsums[w], c["w4"]
                for nb in range(npast):
                    S.op("scalar", lambda e, nb=nb: e.activation(
                        out=Pbm[w4][:, nb * 256:(nb + 1) * 256], in_=ps[:, nb * 256:(nb + 1) * 256], func=AF.Exp,
                        scale=SCALE, bias=selw[:, nb:nb + 1], accum_out=rsw[:, nb:nb + 1]),
                        reads=[("ps", nb // 2), ("sel", w)], writes=[("Pbm", w4), ("rsum", w)])
                S.op("scalar", lambda e: e.activation(
                    out=Pbm[w4][:, npast * 256:nk], in_=ps[:, npast * 256:nk], func=AF.Exp, scale=SCALE,
                    accum_out=rsw[:, npast:npast + 1]),
                    reads=c["sb"], writes=[("Pbm", w4), ("rsum", w)])
            else:
                S.op("scalar", lambda e: e.activation(
                    out=EL[w][:, 0:1024], in_=zb[w][:, 0:1024], func=AF.Exp, bias=pbias[:, 0:1]),
                    reads=[("zb", w)], writes=[("EL", w), ("Pbm", 2 * w), ("Pbm", 2 * w + 1)])
                S.op("scalar", lambda e: e.activation(
                    out=EL[w][:, 1024:nk], in_=zb[w][:, 1024:nk], func=AF.Exp),
                    reads=[("zb", w)], writes=[("EL", w)])
                S.op("scalar", lambda e: e.activation(
                    out=EL[w][:, 0:nk], in_=EL[w][:, 0:nk], func=AF.Ln, bias=one_col[:, 0:1]),
                    reads=[("EL", w)], writes=[("EL", w)])

        def st_B(n):
            c = I(n)
            w, w4, nk, npast = c["w"], c["w4"], c["nk"], c["npast"]
            if c["moba"]:
                S.op("vector", lambda e: e.reduce_sum(
                    out=rtot[:, w:w + 1], in_=rsums[w][:, 0:npast + 1], axis=AX.X),
                    reads=[("rsum", w)], writes=[("rtot", w)])
                S.op("vector", lambda e: e.reciprocal(out=rinv4[:, w4:w4 + 1], in_=rtot[:, w:w + 1]),
                     reads=[("rtot", w)], writes=[("rinv", w4)])
            else:
                S.op("vector", lambda e: e.tensor_tensor_scan(
                    out=Cb1[:, 0:nk], data0=ones_b[:, 0:nk], data1=EL[w][:, 0:nk], initial=0.0,
                    op0=ALU.mult, op1=ALU.add), reads=[("EL", w)], writes=["Cb"])
                S.op("vector", lambda e: e.tensor_scalar(
                    out=negT[:, w:w + 1], in0=Cb1[:, nk - 1:nk], scalar1=-1.0, scalar2=None, op0=ALU.mult),
                    reads=["Cb"], writes=[("negT", w)])
                S.op("vector", lambda e: e.tensor_scalar(
                    out=negTp[:, w:w + 1], in0=negT[:, w:w + 1], scalar1=pbias[:, 0:1], scalar2=None, op0=ALU.add),
                    reads=[("negT", w)], writes=[("negTp", w)])
                hsp = 1 + (nk - 1) * 5 // 8
                S.op("gpsimd", lambda e: e.tensor_tensor(
                    out=zb[w][:, 1:hsp], in0=zb[w][:, 1:hsp], in1=Cb1[:, 0:hsp - 1], op=ALU.add),
                    reads=["Cb", ("zb", w)], writes=[("zb", w)])
                S.op("vector", lambda e: e.tensor_tensor(
                    out=zb[w][:, hsp:nk], in0=zb[w][:, hsp:nk], in1=Cb1[:, hsp - 1:nk - 1], op=ALU.add),
                    reads=["Cb", ("zbh", w)], writes=[("zbh", w)])

        def st_expP(n):
            c = I(n)
            w, nk = c["w"], c["nk"]
            if c["moba"]:
                return
            S.op("scalar", lambda e: e.activation(
                out=Pb[w][:, 0:1024], in_=zb[w][:, 0:1024], func=AF.Exp, bias=negTp[:, w:w + 1]),
                reads=[("zb", w), ("zbh", w), ("negTp", w)], writes=[("Pb", w)])
            S.op("scalar", lambda e: e.activation(
                out=Pb[w][:, 1024:nk], in_=zb[w][:, 1024:nk], func=AF.Exp, bias=negT[:, w:w + 1]),
                reads=[("zb", w), ("zbh", w), ("negT", w)], writes=[("Pb", w)])

        def st_T(n):
            c = I(n)
            w = c["w"]
            src, res = (Pbm[c["w4"]], ("Pbm", c["w4"])) if c["moba"] else (Pb[w], ("Pb", w))
            for kt in range(c["nkt"]):
                tb = PTB + kt // 8
                S.op("tensor", lambda e, kt=kt: e.transpose(
                    out=psb[:, PTB * 1024 + kt * 128: PTB * 1024 + (kt + 1) * 128],
                    in_=src[:, kt * 128:(kt + 1) * 128], identity=ident_b[:]),
                    reads=[res], writes=[("ps", tb)])

        def st_PTcopy(n):
            c = I(n)
            w, nk = c["w"], c["nk"]
            S.op("vector", lambda e: e.tensor_copy(
                out=PT[w][:, 0:1024], in_=psb[:, PTB * 1024: PTB * 1024 + 1024]),
                reads=[("ps", PTB)], writes=[("PTa", w)])
            S.op("vector", lambda e: e.tensor_copy(
                out=PT[w][:, 1024:nk], in_=psb[:, PTB * 1024 + 1024: PTB * 1024 + nk]),
                reads=[("ps", PTB + 1)], writes=[("PTb", w)])

        def st_PV(n):
            c = I(n)
            w, hs, nkt = c["w"], c["hs"], c["nkt"]
            for kt in range(nkt):
                S.op("tensor", lambda e, kt=kt: e.matmul(
                    out=ps[:, 7 * 512:7 * 512 + 128], lhsT=PT[w][:, kt * 128:(kt + 1) * 128],
                    rhs=v_h[hs][:, kt, :], start=(kt == 0), stop=(kt == nkt - 1)),
                    reads=[("PTa", w), ("PTb", w), ("v", hs)], writes=[("ps", 7)])

        def st_out(n):
            c = I(n)
            h, i, w4 = c["h"], c["i"], c["w4"]
            if c["moba"]:
                S.op("scalar", lambda e: e.activation(
                    out=o_sb[:, i, h * 128:(h + 1) * 128], in_=ps[:, 7 * 512:7 * 512 + 128], func=AF.Copy,
                    scale=rinv4[:, w4:w4 + 1]), reads=[("ps", 7), ("rinv", w4)], writes=[("o", i)])
            else:
                S.op("scalar", lambda e: e.activation(
                    out=o_sb[:, i, h * 128:(h + 1) * 128], in_=ps[:, 7 * 512:7 * 512 + 128], func=AF.Copy),
                    reads=[("ps", 7)], writes=[("o", i)])

        ok = lambda n: 0 <= n < N and items[n] is not None
        for p in range(-2, N + 2):
            if ok(p + 2): st_scores(p + 2)
            if ok(p): st_expP(p)
            if ok(p + 2): st_pre(p + 2)
            if ok(p + 1): st_B(p + 1)
            if ok(p + 2): st_exp1(p + 2)
            if ok(p - 1): st_T(p - 1)
            if ok(p - 1): st_PTcopy(p - 1)
            if ok(p - 2): st_PV(p - 2)
            if ok(p - 2): st_out(p - 2)

    one_col = misc[:, 8:9]
    S.op("vector", lambda e: e.memset(one_col, 1.0), writes=["one"])
    attention()
    S.barrier()
    if debug:
        for t in range(T):
            S.op("sync", lambda e, t=t: e.dma_start(out=o_dbg[t * 128:(t + 1) * 128, :], in_=o_sb[:, t, :]),
                 dma="d_dbg")
        S.barrier()
    if stage == 5:
        return finish()

    oT = hT
    norm_T(lambda t: (o_sb[:, t, :], ("o", t)), oT, 32, 2, nR_xn, nR_junk, nR_gbc, "hT")
    S.barrier()

    def op_epilogue(n, t, b):
        S.op("scalar", lambda e, t=t, n=n, b=b: e.activation(
            out=big[:, t, n * 512:(n + 1) * 512], in_=bank(b), func=AF.Copy),
            reads=[("ps", b)], writes=[("f", t)])

    cnt["pp"] = 0

    def bank_fn2(n, t):
        b = cnt["pp"] % 8
        cnt["pp"] += 1
        return b

    tok_proj(lambda k, t: oT[:, k, t * 128:(t + 1) * 128], lambda k, t: ("hT", t), KC,
             win_sb, ["d_win0", "d_win1"], lambda n: wout[n], 4, op_epilogue, bank_fn2, "win")
    postnorm_residual(big, x1_own, x2_own, 1, 1.0, gpost2)
    S.barrier()

    ffn(1, x2_own, out_d, 48, 2)

    return finish()


_NC_CACHE = {}


def _rope_tables(pos0):
    half = 16
    inv_freq = (500000.0 ** (-np.arange(0, 32, 2, dtype=np.float32) / 32)).astype(np.float32)
    pos = (pos0 + np.arange(1024, dtype=np.float32))
    ang = pos[:, None] * inv_freq[None, :]
    cos = np.cos(ang).astype(np.float32)
    sin = np.sin(ang).astype(np.float32)
    cos2 = np.concatenate([cos, cos], axis=1)
    sinS = np.concatenate([-sin, sin], axis=1)
    tab = np.stack([cos2, sinS], axis=1)
    tab = np.broadcast_to(tab[:, :, None, :], (1024, 2, 4, 32))
    tab = tab.reshape(T, 128, 2, 4, 32).transpose(1, 0, 2, 3, 4)
    return np.ascontiguousarray(tab).reshape(128, T * 2 * 4 * 32)


def prepare_inputs(inputs):
    f = lambda a: np.ascontiguousarray(np.asarray(a, dtype=np.float32))
    x = f(inputs["x"])

    def gu(wg, wu):
        g = f(wg)[0].reshape(KC, 128, NCH, 128).transpose(2, 1, 0, 3)
        u = f(wu)[0].reshape(KC, 128, NCH, 128).transpose(2, 1, 0, 3)
        return np.ascontiguousarray(np.stack([g, u], axis=2))

    def dn(w):
        return np.ascontiguousarray(f(w)[0].reshape(NJ, CPJ, 128, 4, 512).transpose(0, 3, 2, 1, 4))

    def colgrp(w, ng):
        return np.ascontiguousarray(f(w)[0].reshape(KC, 128, ng, 512).transpose(2, 1, 0, 3))

    def gcol(g):
        return f(g).reshape(KC, 128).T

    shared = {
        "wgu1": gu(inputs["ffn1_w_gate"], inputs["ffn1_w_up"]), "wd1": dn(inputs["ffn1_w_down"]),
        "wgu2": gu(inputs["ffn2_w_gate"], inputs["ffn2_w_up"]), "wd2": dn(inputs["ffn2_w_down"]),
        "win": colgrp(inputs["w_in"], 12), "wout": colgrp(inputs["w_out"], 4),
        "gT": np.ascontiguousarray(np.concatenate([
            gcol(inputs["ffn1_pre_g"]), gcol(inputs["mix_pre_g"]),
            gcol(np.concatenate([f(inputs["moba_out_g"])[0], f(inputs["sb_out_g"])[0]])),
            gcol(inputs["ffn2_pre_g"])], axis=1)),
        "gpost": np.ascontiguousarray(np.stack([
            np.broadcast_to(f(inputs[k])[0][None, :], (128, D)) for k in
            ("ffn1_post_g", "mix_post_g", "ffn2_post_g")])),
        "ident": np.eye(128, dtype=np.float32),
    }
    qi = np.arange(128)[:, None]
    ki = np.arange(128)[None, :]
    m_le = np.where(ki <= qi, 0.0, 2 * NEG).astype(np.float32)
    m_lt = np.where(ki < qi, 0.0, 2 * NEG).astype(np.float32)
    shared["masks"] = np.ascontiguousarray(np.concatenate([m_le, m_lt], axis=1))
    ropes = [_rope_tables(0.0), _rope_tables(1024.0)]
    in_maps = []
    for c in range(8):
        b, r = divmod(c, 2)
        m = dict(shared)
        m["x_own"] = np.ascontiguousarray(x[b, r * 1024:(r + 1) * 1024])
        m["x_par"] = np.ascontiguousarray(x[b, (1 - r) * 1024:(2 - r) * 1024])
        m["rope"] = np.ascontiguousarray(np.stack([ropes[1 - r], ropes[r]]))
        m["pbias"] = np.full((128, 1), NEG if r == 0 else 0.0, dtype=np.float32)
        in_maps.append(m)
    return in_maps


def kernel(**inputs):
    in_maps = prepare_inputs(inputs)
    if "nc" not in _NC_CACHE:
        _NC_CACHE["nc"] = build_nc(False)
    res = run_bass_kernel_spmd(_NC_CACHE["nc"], in_maps, core_ids=list(range(8)))
    out = np.empty((4, 2048, D), dtype=np.float32)
    for c in range(8):
        b, r = divmod(c, 2)
        out[b, r * 1024:(r + 1) * 1024] = res.results[c]["out"]
    return out
```

```python
import numpy as np
import concourse.bass as bass
import concourse.mybir as mybir
from concourse.bass_utils import run_bass_kernel_spmd

F32 = mybir.dt.float32
BF16 = mybir.dt.bfloat16
AF = mybir.ActivationFunctionType
ALU = mybir.AluOpType
AX = mybir.AxisListType

D = 2048
DFF = 5632
NCH = DFF // 128
NJ = 4
CPJ = NCH // NJ
T = 8
KC = D // 128
NH = 16
EPS = 1e-6
SCALE = 128 ** -0.5
NEG = -30000.0
KB = 1024
SB_BASE = 16512

SAME_ENGINE_SYNC = {"vector": True, "scalar": True, "gpsimd": True, "tensor": False, "sync": False}


class Sched:
    def __init__(self, nc):
        self.nc = nc
        self.engines = ["sync", "scalar", "vector", "gpsimd", "tensor"]
        self.lists = {e: [] for e in self.engines}
        self.cnt = {}
        self.semh = {}
        self.seen = {e: {} for e in self.engines}
        self.lastw = {}
        self.readers = {}
        self._stack = []

    def add_sem(self, key):
        cm = self.nc.semaphore("s_" + key)
        h = cm.__enter__()
        self._stack.append(cm)
        self.semh[key] = h
        self.cnt[key] = 0

    def close(self):
        for cm in reversed(self._stack):
            cm.__exit__(None, None, None)

    def _waits(self, engine, deps):
        need = {}
        for (sk, v) in deps:
            if v > need.get(sk, 0):
                need[sk] = v
        out = []
        for sk, v in need.items():
            if sk == engine and not SAME_ENGINE_SYNC[engine]:
                continue
            if self.seen[engine].get(sk, 0) >= v:
                continue
            self.seen[engine][sk] = v
            out.append((sk, v))
        return out

    def op(self, engine, fn, reads=(), writes=(), dma=None):
        deps = []
        for r in reads:
            t = self.lastw.get(r)
            if t:
                deps.append(t)
        for w in writes:
            t = self.lastw.get(w)
            if t:
                deps.append(t)
            deps.extend(self.readers.get(w, ()))
        waits = self._waits(engine, deps)
        if dma is None:
            self.cnt[engine] += 1
            tok = (engine, self.cnt[engine])
            inc = (engine, 1)
        else:
            self.cnt[dma] += 16
            tok = (dma, self.cnt[dma])
            inc = (dma, 16)
        self.lists[engine].append((waits, fn, inc))
        for r in reads:
            self.readers.setdefault(r, []).append(tok)
        for w in writes:
            self.lastw[w] = tok
            self.readers[w] = []
        return tok

    def barrier(self):
        for e in self.engines:
            deps = [(k, v) for k, v in self.cnt.items() if v > 0]
            waits = self._waits(e, deps)
            if waits:
                self.lists[e].append((waits, None, None))
        self.lastw = {}
        self.readers = {}

    def emit(self, block):
        nc = self.nc

        def run(eng, items):
            for waits, fn, inc in items:
                for sk, v in waits:
                    eng.wait_ge(self.semh[sk], v)
                if fn is not None:
                    ins = fn(eng)
                    ins.then_inc(self.semh[inc[0]], inc[1])

        @block.sync
        def _(e):
            run(e, self.lists["sync"])

        @block.scalar
        def _(e):
            run(e, self.lists["scalar"])

        @block.vector
        def _(e):
            run(e, self.lists["vector"])

        @block.gpsimd
        def _(e):
            run(e, self.lists["gpsimd"])

        @block.tensor
        def _(e):
            run(e, self.lists["tensor"])


import os as _os
_DBG = set(_os.environ.get("KDBG", "").split(","))


def build_nc(debug=False, stage=99):
    nc = bass.Bass("TRN2", target_bir_lowering=False)
    S = Sched(nc)

    def din(name, shape, dt=F32):
        if stage <= 3 and name in ("wgu2", "wd2", "win", "wout", "rope"):
            return None
        if stage <= 5 and name in ("wgu2", "wd2", "wout"):
            return None
        return nc.dram_tensor(name, list(shape), dt, kind="ExternalInput").ap()

    x_own = din("x_own", [T * 128, D])
    x_par = din("x_par", [T * 128, D])
    wgu = [din("wgu1", [NCH, 128, 2, KC, 128]), din("wgu2", [NCH, 128, 2, KC, 128])]
    wd = [din("wd1", [NJ, 4, 128, CPJ, 512]), din("wd2", [NJ, 4, 128, CPJ, 512])]
    win = din("win", [12, 128, KC, 512])
    wout = din("wout", [4, 128, KC, 512])
    gT_d = din("gT", [128, 64])
    gpost_d = din("gpost", [3, 128, D])
    rope_d = din("rope", [2, 128, T * 2 * 4 * 32])
    ident_d = din("ident", [128, 128])
    masks_d = din("masks", [128, 2 * 128])
    pbias_d = din("pbias", [128, 1])
    out_d = nc.dram_tensor("out", [T * 128, D], F32, kind="ExternalOutput").ap()

    dk = "ExternalOutput" if debug else "Internal"
    x1_own = nc.dram_tensor("x1_own", [T * 128, D], F32, kind=dk).ap()
    x1_par = nc.dram_tensor("x1_par", [T * 128, D], F32, kind=dk).ap()
    x2_own = nc.dram_tensor("x2_own", [T * 128, D], F32, kind=dk).ap()
    kT_dram = nc.dram_tensor("kT_dram", [NH, 128, 2048], BF16, kind="Internal").ap()
    v_dram = nc.dram_tensor("v_dram", [NH, 128, 16, 128], BF16, kind="Internal").ap()
    o_dbg = nc.dram_tensor("o_dbg", [T * 128, D], F32, kind=dk).ap() if debug else None

    def sb(name, shape, dt, off):
        return nc.alloc_sbuf_tensor_at(name, list(shape), dt, offset=SB_BASE + off)

    o = 0
    ident_f = sb("ident_f", [128, 128], F32, o); o += 512
    ident_b = sb("ident_b", [128, 128], BF16, o); o += 256
    masks_b = sb("masks_b", [128, 256], BF16, o); o += 512
    gT = sb("gT", [128, 64], F32, o); o += 256
    pbias = sb("pbias", [128, 1], F32, o); o += 32
    small = sb("small", [128, 96], F32, o); o += 384
    ones_b = sb("ones_b", [128, 2048], BF16, o); o += 4096
    small2 = sb("small2", [128, 64], F32, o); o += 256
    CONST_END = 6 * KB + 512
    assert o <= CONST_END
    A0 = CONST_END
    B0 = A0 + 64 * KB
    C0 = B0 + 32 * KB
    R0 = C0 + 32 * KB
    big = sb("big", [128, T, D], F32, A0)
    hT = sb("hT", [128, KC, 1024], BF16, B0)
    qT = sb("qT", [128, NH, 1024], BF16, C0)
    aT = sb("aT", [128, CPJ, 1024], BF16, C0)
    sg = [sb("sg%d" % i, [128, 1024], F32, C0 + 22 * KB + i * 4 * KB) for i in range(2)]
    r = R0
    wgu_sb = [sb("wgu_sb%d" % i, [128, 2, KC, 128], BF16, r + i * 8 * KB) for i in range(3)]
    r += 24 * KB
    wd_sb = [sb("wd_sb%d" % i, [128, CPJ, 512], BF16, r + i * 11 * KB) for i in range(2)]
    r += 22 * KB
    xt = sb("xt", [128, D], F32, r); r += 8 * KB
    xn = sb("xn", [128, D], F32, r); r += 8 * KB
    gpost = sb("gpost", [128, D], F32, r); r += 8 * KB
    FFN_END = r
    r = R0
    win_sb = [sb("win_sb%d" % i, [128, KC, 512], BF16, r + i * 16 * KB) for i in range(2)]
    r += 32 * KB
    rope_sb = sb("rope_sb", [128, 2, T * 2 * 4 * 32], F32, r); r += 16 * KB
    xt2 = sb("xt2", [128, D], F32, r); r += 8 * KB
    xn2 = sb("xn2", [128, D], F32, r); r += 8 * KB
    gpost2 = sb("gpost2", [128, D], F32, R0 + 32 * KB)
    qk_sb = [sb("qk_sb%d" % i, [128, 512], BF16, r + i * KB) for i in range(2)]; r += 2 * KB
    kst = [sb("kst%d" % i, [128, 4, 128], BF16, r + i * KB) for i in range(2)]; r += 2 * KB
    rt1 = sb("rt1", [128, 4, 32], F32, r); r += 512
    rt2 = sb("rt2", [128, 4, 32], F32, r); r += 512
    QKV_END = r
    r = R0
    kT_h = [sb("kT_h%d" % i, [128, 2048], BF16, r + i * 4 * KB) for i in range(2)]; r += 8 * KB
    v_h = [sb("v_h%d" % i, [128, 16, 128], BF16, r + i * 4 * KB) for i in range(2)]; r += 8 * KB
    EL = [sb("EL%d" % i, [128, 2048], F32, r + i * 8 * KB) for i in range(2)]; r += 16 * KB
    Pbm = [sb("Pbm%d" % i, [128, 2048], BF16, r - 16 * KB + i * 4 * KB) for i in range(4)]
    zb = [sb("zb%d" % i, [128, 2048], F32, r + i * 8 * KB) for i in range(2)]; r += 16 * KB
    Cb1 = sb("Cb1", [128, 2048], F32, r); r += 8 * KB
    Pb = [sb("Pb%d" % i, [128, 2048], BF16, r + i * 4 * KB) for i in range(2)]; r += 8 * KB
    PT = [sb("PT%d" % i, [128, 2048], BF16, r + i * 4 * KB) for i in range(2)]; r += 8 * KB
    km_f = sb("km_f", [128, 8], F32, r); r += 32
    km_b = [sb("km_b%d" % i, [128, 8], BF16, r + 32 * i) for i in range(2)]; r += 64
    ATT_END = r
    LIMIT = 229376 - SB_BASE
    assert max(FFN_END, QKV_END, ATT_END) <= LIMIT, (FFN_END, QKV_END, ATT_END)

    ps = nc.alloc_psum_tensor("ps", [128, 4096], F32)
    psb = ps.bitcast(BF16)

    def bank(b, w=512, off=0):
        return ps[:, b * 512 + off: b * 512 + off + w]

    for k in ["sync", "scalar", "vector", "gpsimd", "tensor"]:
        S.add_sem(k)
    for k in ["d_const", "d_const_sw", "d_xt0", "d_xt1", "d_xt2", "d_out0", "d_out1", "d_gpost", "d_out", "d_wgu0", "d_wgu1", "d_wgu2", "d_wd0", "d_wd1",
              "d_win0", "d_win1", "d_rope", "d_kst0", "d_kst1", "d_vst0", "d_vst1",
              "d_kT0", "d_kT1", "d_v0", "d_v1", "d_dbg"]:
        S.add_sem(k)

    ss = small[:, 0:16]
    rs = small[:, 16:32]
    rstd = small[:, 32:48]
    g8 = small[:, 48:56]
    m8 = small[:, 56:64]
    selb = small[:, 64:72]
    rsum = small[:, 72:80]
    misc = small[:, 80:96]
    g8s = [small2[:, 0:8], small2[:, 8:16]]
    m8s = [small2[:, 16:24], small2[:, 24:32]]
    sels = [small2[:, 32:40], small2[:, 40:48]]
    rsums = [small[:, 64:72], small[:, 72:80]]
    rtot = small2[:, 48:50]
    rinv4 = small2[:, 50:54]
    negT = small2[:, 54:56]
    negTp = small2[:, 56:58]

    S.op("sync", lambda e: e.dma_start(out=ident_f[:], in_=ident_d[:, :]), writes=["ident_f"], dma="d_const")
    S.op("sync", lambda e: e.dma_start(out=gT[:], in_=gT_d[:, :]), writes=["gT"], dma="d_const")
    S.op("sync", lambda e: e.dma_start(out=pbias[:], in_=pbias_d[:, :]), writes=["pbias"], dma="d_const")
    S.op("gpsimd", lambda e: e.dma_start(out=ident_b[:], in_=ident_d[:, :]), writes=["ident_b"], dma="d_const_sw")
    S.op("gpsimd", lambda e: e.dma_start(out=masks_b[:], in_=masks_d[:, :]), writes=["masks_b"], dma="d_const_sw")
    S.op("vector", lambda e: e.memset(ones_b[:], 1.0), writes=["ones_b"])
    S.barrier()

    nA_xt = [sb("nA_xt%d" % i, [128, D], F32, A0 + i * 8 * KB) for i in range(3)]
    nA_xn = [sb("nA_xn%d" % i, [128, D], F32, A0 + 24 * KB + i * 8 * KB) for i in range(2)]
    nA_junk = sb("nA_junk", [128, D], BF16, A0 + 40 * KB)
    nA_gbc = sb("nA_gbc", [128, KC, 128], F32, A0 + 44 * KB)
    nR_xn = [sb("nR_xn%d" % i, [128, D], F32, R0 + 32 * KB + i * 8 * KB) for i in range(2)]
    nR_junk = sb("nR_junk", [128, D], BF16, R0 + 48 * KB)
    nR_gbc = sb("nR_gbc", [128, KC, 128], F32, R0 + 52 * KB)
    pB_xt = [sb("pB_xt%d" % i, [128, D], F32, B0 + i * 8 * KB) for i in range(2)]
    pB_out = [sb("pB_out%d" % i, [128, D], F32, B0 + 16 * KB + i * 8 * KB) for i in range(2)]
    pC_junk = sb("pC_junk", [128, D], BF16, C0)

    def norm_T(get_src, dst_T, gcol0, ngroups, xn_ring, junk, gbc, tag):
        gw = D // ngroups
        for kc in range(KC):
            S.op("vector", lambda e, kc=kc: e.tensor_scalar(
                out=gbc[:, kc, :], in0=ones_b[:, 0:128], scalar1=gT[:, gcol0 + kc:gcol0 + kc + 1], scalar2=None,
                op0=ALU.mult), writes=["gbc"])
        srcs = {}

        def S1(t):
            src_ap, src_res = get_src(t)
            k = t % len(xn_ring)
            xn_ = xn_ring[k]
            for g in range(ngroups):
                S.op("scalar", lambda e, g=g: e.activation(
                    out=junk[:, 0:gw], in_=src_ap[:, g * gw:(g + 1) * gw], func=AF.Square,
                    accum_out=ss[:, 2 * t + g: 2 * t + g + 1]), reads=[src_res], writes=["junk", ("ss", t)])
            S.op("scalar", lambda e: e.activation(
                out=rs[:, 2 * t:2 * t + ngroups], in_=ss[:, 2 * t:2 * t + ngroups], func=AF.Sqrt,
                scale=1.0 / gw, bias=eps_col[:, 0:1]), reads=[("ss", t)], writes=[("rs", t)])
            S.op("vector", lambda e: e.reciprocal(out=rstd[:, 2 * t:2 * t + ngroups], in_=rs[:, 2 * t:2 * t + ngroups]),
                 reads=[("rs", t)], writes=[("rstd", t)])
            for g in range(ngroups):
                S.op("scalar", lambda e, g=g: e.activation(
                    out=xn_[:, g * gw:(g + 1) * gw], in_=src_ap[:, g * gw:(g + 1) * gw], func=AF.Copy,
                    scale=rstd[:, 2 * t + g:2 * t + g + 1]), reads=[src_res, ("rstd", t)], writes=[("xn", k)])

        def S2(t):
            k = t % len(xn_ring)
            xn_ = xn_ring[k]
            b0 = 4 * (t % 2)
            for kc in range(KC):
                b = b0 + kc // 4
                S.op("tensor", lambda e, kc=kc, b=b: e.transpose(
                    out=bank(b, 128, (kc % 4) * 128), in_=xn_[:, kc * 128:(kc + 1) * 128], identity=ident_f[:]),
                    reads=[("xn", k)], writes=[("ps", b)])

        def S3(t):
            b0 = 4 * (t % 2)
            for q in range(4):
                b = b0 + q
                S.op("vector", lambda e, q=q, b=b: e.tensor_tensor(
                    out=dst_T[:, 4 * q:4 * q + 4, t * 128:(t + 1) * 128],
                    in0=bank(b).rearrange("p (k j) -> p k j", k=4), in1=gbc[:, 4 * q:4 * q + 4, :], op=ALU.mult),
                    reads=[("ps", b), "gbc"], writes=[(tag, t)])

        for p_ in range(-1, T):
            if p_ + 1 < T:
                S1(p_ + 1)
            if p_ >= 0:
                S2(p_)
                S3(p_)

    eps_col = misc[:, 0:1]
    S.op("vector", lambda e: e.memset(eps_col, EPS), writes=["eps"])
    S.barrier()

    def dram_src(src, ring):
        def get(t):
            k = t % len(ring)
            S.op("sync", lambda e: e.dma_start(out=ring[k][:], in_=src[t * 128:(t + 1) * 128, :]),
                 writes=[("xt", k)], dma="d_xt%d" % k)
            return ring[k], ("xt", k)
        return get

    def postnorm_residual(fsb, src, dst, gidx, res_scale, gp_):
        S.op("sync", lambda e: e.dma_start(out=gp_[:], in_=gpost_d[gidx, :, :]), writes=["gpost"], dma="d_gpost")

        alias_B = [("hT", tt) for tt in range(T)]
        alias_C = [("aT", cc) for cc in range(CPJ)] + [("sg", 0), ("sg", 1)]

        def P1(t):
            k = t % 2
            S.op("sync", lambda e: e.dma_start(out=pB_xt[k][:], in_=src[t * 128:(t + 1) * 128, :]),
                 writes=[("xt", k)] + (alias_B if t < 2 else []), dma="d_xt%d" % k)
            S.op("scalar", lambda e: e.activation(
                out=pC_junk[:], in_=fsb[:, t, :], func=AF.Square, accum_out=ss[:, t:t + 1]),
                reads=[("f", t)], writes=["junk", ("ss", t)] + (alias_C if t == 0 else []))
            S.op("scalar", lambda e: e.activation(
                out=rs[:, t:t + 1], in_=ss[:, t:t + 1], func=AF.Sqrt, scale=1.0 / D, bias=eps_col[:, 0:1]),
                reads=[("ss", t)], writes=[("rs", t)])
            S.op("vector", lambda e: e.reciprocal(out=rstd[:, t:t + 1], in_=rs[:, t:t + 1]),
                 reads=[("rs", t)], writes=[("rstd", t)])
            S.op("vector", lambda e: e.tensor_scalar(
                out=rstd[:, 8 + t:9 + t], in0=rstd[:, t:t + 1], scalar1=float(res_scale), scalar2=None, op0=ALU.mult),
                reads=[("rstd", t)], writes=[("rstds", t)])

        def P2(t):
            k = t % 2
            S.op("vector", lambda e: e.scalar_tensor_tensor(
                out=pB_out[k][:], in0=fsb[:, t, :], scalar=rstd[:, 8 + t:9 + t], in1=gp_[:], op0=ALU.mult, op1=ALU.mult),
                reads=[("f", t), ("rstds", t), "gpost"], writes=[("po", k), ("po2", k)] + (alias_B if t < 2 else []))
            S.op("gpsimd", lambda e: e.tensor_tensor(
                out=pB_out[k][:, 0:1024], in0=pB_out[k][:, 0:1024], in1=pB_xt[k][:, 0:1024], op=ALU.add),
                reads=[("po", k), ("xt", k)], writes=[("po", k)])
            S.op("vector", lambda e: e.tensor_tensor(
                out=pB_out[k][:, 1024:2048], in0=pB_out[k][:, 1024:2048], in1=pB_xt[k][:, 1024:2048], op=ALU.add),
                reads=[("po2", k), ("xt", k)], writes=[("po2", k)])
            S.op("sync", lambda e: e.dma_start(out=dst[t * 128:(t + 1) * 128, :], in_=pB_out[k][:]),
                 reads=[("po", k), ("po2", k)], writes=[("dst", t)], dma="d_out%d" % k)

        for p_ in range(-1, T):
            if p_ + 1 < T:
                P1(p_ + 1)
            if p_ >= 0:
                P2(p_)


    def tok_proj(lhsT_fn, lhs_res_fn, nk, w_slots, w_sems, w_src_fn, ngrp, epilogue, bank_fn, w_tag):
        for n in range(ngrp):
            sl = n % len(w_slots)
            S.op("gpsimd", lambda e, n=n, sl=sl: e.dma_start(out=w_slots[sl][:], in_=w_src_fn(n)),
                 writes=[(w_tag, sl)], dma=w_sems[sl])
            for t in range(T):
                b = bank_fn(n, t)
                for k in range(nk):
                    S.op("tensor", lambda e, k=k, t=t, b=b, sl=sl: e.matmul(
                        out=bank(b), lhsT=lhsT_fn(k, t), rhs=w_slots[sl][:, k, :], start=(k == 0), stop=(k == nk - 1)),
                        reads=[(w_tag, sl), lhs_res_fn(k, t)], writes=[("ps", b)])
                epilogue(n, t, b)

    def ffn(idx, src, dst, gcol0, gidx, sub=99):
        Wgu, Wd = wgu[idx], wd[idx]
        norm_T(dram_src(src, nA_xt), hT, gcol0, 1, nA_xn, nA_junk, nA_gbc, "hT")
        if sub < 1:
            S.barrier()
            return
        for j in range(NJ):
            for cc in range(CPJ):
                c = j * CPJ + cc
                sl = c % 3
                par = c % 2
                S.op("gpsimd", lambda e, c=c, sl=sl: e.dma_start(out=wgu_sb[sl][:], in_=Wgu[c]),
                     writes=[("wgu", sl)], dma="d_wgu%d" % sl)
                for gu in range(2):
                    for kc in range(KC):
                        for half in range(2):
                            b = par * 4 + gu * 2 + half
                            S.op("tensor", lambda e, gu=gu, kc=kc, half=half, b=b, sl=sl: e.matmul(
                                out=bank(b), lhsT=wgu_sb[sl][:, gu, kc, :], rhs=hT[:, kc, half * 512:(half + 1) * 512],
                                start=(kc == 0), stop=(kc == KC - 1)),
                                reads=[("wgu", sl)] + [("hT", tt) for tt in range(half * 4, half * 4 + 4)],
                                writes=[("ps", b)])
                bg = par * 4
                S.op("scalar", lambda e, bg=bg, par=par: e.activation(
                    out=sg[par][:], in_=ps[:, bg * 512: bg * 512 + 1024], func=AF.Silu),
                    reads=[("ps", bg), ("ps", bg + 1)], writes=[("sg", par)])
                S.op("vector", lambda e, bg=bg, par=par, cc=cc: e.tensor_tensor(
                    out=aT[:, cc, :], in0=sg[par][:], in1=ps[:, (bg + 2) * 512:(bg + 2) * 512 + 1024], op=ALU.mult),
                    reads=[("sg", par), ("ps", bg + 2), ("ps", bg + 3)], writes=[("aT", cc)])
            for n in range(4):
                sl = (j * 4 + n) % 2
                S.op("gpsimd", lambda e, j=j, n=n, sl=sl: e.dma_start(out=wd_sb[sl][:], in_=Wd[j, n]),
                     writes=[("wd", sl)], dma="d_wd%d" % sl)
                for t in range(T):
                    for cc in range(CPJ):
                        S.op("tensor", lambda e, t=t, cc=cc, sl=sl: e.matmul(
                            out=bank(t), lhsT=aT[:, cc, t * 128:(t + 1) * 128], rhs=wd_sb[sl][:, cc, :],
                            start=(cc == 0), stop=(cc == CPJ - 1)),
                            reads=[("wd", sl), ("aT", cc)], writes=[("ps", t)])
                    if j == 0:
                        S.op("scalar", lambda e, t=t, n=n: e.activation(
                            out=big[:, t, n * 512:(n + 1) * 512], in_=bank(t), func=AF.Copy),
                            reads=[("ps", t)], writes=[("f", t)])
                    else:
                        S.op("vector", lambda e, t=t, n=n: e.tensor_tensor(
                            out=big[:, t, n * 512:(n + 1) * 512], in0=big[:, t, n * 512:(n + 1) * 512],
                            in1=bank(t), op=ALU.add),
                            reads=[("ps", t), ("f", t)], writes=[("f", t)])
        postnorm_residual(big, src, dst, gidx, 0.5, gpost)
        S.barrier()

    def finish():
        with nc.Block() as block:
            S.emit(block)
        S.close()
        return nc

    if stage == 0:
        return finish()
    if stage == 1:
        ffn(0, x_par, x1_par, 0, 0, sub=0)
        return finish()
    ffn(0, x_par, x1_par, 0, 0)
    if stage == 2:
        return finish()
    ffn(0, x_own, x1_own, 0, 0)
    if stage == 3:
        return finish()

    S.op("sync", lambda e: e.dma_start(out=rope_sb[:], in_=rope_d.rearrange("w p f -> p w f")),
         writes=["rope"], dma="d_rope")
    rope_v = [rope_sb[:, w, :].rearrange("p (t c h f) -> p t c h f", t=T, c=2, h=4) for w in range(2)]
    cnt = {"pp": 0, "tp": 0, "st": 0}

    def qkv_for(which, src, groups):
        norm_T(dram_src(src, nA_xt), hT, 16, 1, nA_xn, nA_junk, nA_gbc, "hT")

        pending = []

        def epilogue(n_idx, t, b):
            prev = pending.pop() if pending else None
            epilogue_main(n_idx, t, b)
            if prev is not None:
                prev()

        def epilogue_main(n_idx, t, b):
            grp = groups[n_idx]
            typ = grp // 2
            hh0 = (grp % 2) * 4
            h0 = (0 if typ < 3 else 8) + hh0
            kt = t if which == 0 else 8 + t
            st = cnt["st"] % 2
            cnt["st"] += 1
            S.op("scalar", lambda e, b=b, st=st: e.activation(out=qk_sb[st][:], in_=bank(b), func=AF.Copy),
                 reads=[("ps", b)], writes=[("qk", st)])
            if typ in (2, 5):
                if "nov" in _DBG:
                    return
                S.op("sync", lambda e, st=st, h0=h0, kt=kt: e.dma_start(
                    out=v_dram[h0:h0 + 4, :, kt, :].rearrange("h p d -> p h d"),
                    in_=qk_sb[st][:].rearrange("p (h d) -> p h d", h=4)),
                    reads=[("qk", st)], writes=[("vd", h0, kt)], dma="d_vst%d" % st)
                return
            if typ in (0, 1) and "norope" not in _DBG:
                ps4 = bank(b).rearrange("p (h d) -> p h d", h=4)
                cosv = rope_v[which][:, t, 0, :, :]
                sinv = rope_v[which][:, t, 1, :, :]
                S.op("vector", lambda e, ps4=ps4, cosv=cosv: e.tensor_tensor(
                    out=rt1[:], in0=ps4[:, :, 0:32], in1=cosv, op=ALU.mult),
                    reads=[("ps", b), "rope", ("qk", st)], writes=["rt1"])
                if "ropeA" not in _DBG:
                  S.op("vector", lambda e, ps4=ps4, sinv=sinv: e.tensor_tensor(
                    out=rt2[:, :, 0:16], in0=ps4[:, :, 16:32], in1=sinv[:, :, 0:16], op=ALU.mult),
                    reads=[("ps", b), "rope", ("qk", st)], writes=["rt2"])
                if "ropeA" not in _DBG:
                  S.op("vector", lambda e, ps4=ps4, sinv=sinv: e.tensor_tensor(
                    out=rt2[:, :, 16:32], in0=ps4[:, :, 0:16], in1=sinv[:, :, 16:32], op=ALU.mult),
                    reads=[("ps", b), "rope", ("qk", st)], writes=["rt2"])
                if "ropeA" not in _DBG and "ropeB" not in _DBG:
                  S.op("vector", lambda e, st=st: e.tensor_tensor(
                    out=qk_sb[st][:].rearrange("p (h d) -> p h d", h=4)[:, :, 0:32], in0=rt1[:], in1=rt2[:],
                    op=ALU.add), reads=["rt1", "rt2", ("qk", st)], writes=[("qk", st)])
            if "notr" in _DBG:
                return
            pending.append(lambda: epilogue_tail(typ, st, h0, t, kt))

        def epilogue_tail(typ, st, h0, t, kt):
            tb = 4 + cnt["tp"] % 4
            cnt["tp"] += 1
            for jh in range(4):
                S.op("tensor", lambda e, jh=jh, st=st, tb=tb: e.transpose(
                    out=psb[:, tb * 1024 + jh * 128: tb * 1024 + (jh + 1) * 128],
                    in_=qk_sb[st][:, jh * 128:(jh + 1) * 128], identity=ident_b[:]),
                    reads=[("qk", st)], writes=[("ps", tb)])
            tsrc = psb[:, tb * 1024: tb * 1024 + 512].rearrange("p (h j) -> p h j", h=4)
            if typ in (0, 3):
                S.op("vector", lambda e, tsrc=tsrc, h0=h0, t=t: e.tensor_copy(
                    out=qT[:, h0:h0 + 4, t * 128:(t + 1) * 128], in_=tsrc),
                    reads=[("ps", tb)], writes=[("qT", h0, t)])
            else:
                S.op("vector", lambda e, tsrc=tsrc, st=st: e.tensor_copy(out=kst[st][:], in_=tsrc),
                     reads=[("ps", tb)], writes=[("kst", st)])
                if "nok" not in _DBG:
                  S.op("sync", lambda e, st=st, h0=h0, kt=kt: e.dma_start(
                    out=kT_dram[h0:h0 + 4, :, kt * 128:(kt + 1) * 128].rearrange("h d j -> d h j"),
                    in_=kst[st][:]), reads=[("kst", st)], writes=[("kd", h0, kt)], dma="d_kst%d" % st)

        def bank_fn(n, t):
            b = cnt["pp"] % 4
            cnt["pp"] += 1
            return b

        tok_proj(lambda k, t: hT[:, k, t * 128:(t + 1) * 128], lambda k, t: ("hT", t), KC,
                 win_sb, ["d_win0", "d_win1"], lambda n: win[groups[n]], len(groups), epilogue, bank_fn, "win")
        if pending:
            pending.pop()()
        S.barrier()

    qkv_for(0, x1_par, [2, 3, 4, 5, 8, 9, 10, 11])
    qkv_for(1, x1_own, list(range(12)))
    if stage == 4:
        return finish()

    o_sb = big
    PTB = 4

    def attention():
        items = [(h, i) for h in range(8) for i in range(T)] + [None, None] + \
                [(h, i) for h in range(8, NH) for i in range(T)]
        N = len(items)

        def I(n):
            h, i = items[n]
            nk = 1024 + 128 * (i + 1)
            return dict(h=h, i=i, w=n % 2, w4=n % 4, hs=h % 2, moba=(h < 8), nk=nk, nkt=nk // 128,
                        npast=4 + i // 2, sb=[("ps", bb) for bb in range((nk + 511) // 512)])

        def st_scores(n):
            c = I(n)
            h, i, hs, nk = c["h"], c["i"], c["hs"], c["nk"]
            if i == 0:
                S.op("sync", lambda e: e.dma_start(out=kT_h[hs][:], in_=kT_dram[h]),
                     writes=[("kT", hs)], dma="d_kT%d" % hs)
                S.op("sync", lambda e: e.dma_start(out=v_h[hs][:], in_=v_dram[h]),
                     writes=[("v", hs)], dma="d_v%d" % hs)
                if c["moba"]:
                    S.op("vector", lambda e: e.tensor_reduce(
                        out=km_f[:], in_=kT_h[hs][:].rearrange("p (n k) -> p n k", k=256), axis=AX.X, op=ALU.add),
                        reads=[("kT", hs)], writes=["km_f"])
                    S.op("vector", lambda e: e.tensor_copy(out=km_b[hs][:], in_=km_f[:]),
                         reads=["km_f"], writes=[("km_b", hs)])
                    for ww in range(2):
                        S.op("vector", lambda e, ww=ww: e.memset(g8s[ww], -1e30), writes=[("g8", ww)])
            qTi = qT[:, h, i * 128:(i + 1) * 128]
            mask = masks_b[:, 0:128] if c["moba"] else masks_b[:, 128:256]
            for c0 in range(0, nk, 512):
                wd_ = min(512, nk - c0)
                last = (c0 + wd_ == nk)
                S.op("tensor", lambda e, c0=c0, wd_=wd_, last=last: e.matmul(
                    out=ps[:, c0:c0 + wd_], lhsT=qTi, rhs=kT_h[hs][:, c0:c0 + wd_], start=True, stop=(not last)),
                    reads=[("kT", hs)], writes=[("ps", c0 // 512)])
            S.op("tensor", lambda e: e.matmul(
                out=ps[:, nk - 128:nk], lhsT=ident_b[:], rhs=mask, start=False, stop=True),
                writes=[("ps", (nk - 128) // 512)])
            if c["moba"]:
                S.op("tensor", lambda e: e.matmul(
                    out=ps[:, 6 * 512:6 * 512 + 8], lhsT=qTi, rhs=km_b[hs][:], start=True, stop=True),
                    reads=[("km_b", hs)], writes=[("ps", 6)])

        def st_pre(n):
            c = I(n)
            w, nk, npast = c["w"], c["nk"], c["npast"]
            if c["moba"]:
                g8w, m8w, selw = g8s[w], m8s[w], sels[w]
                S.op("vector", lambda e: e.tensor_scalar(
                    out=g8w[:, 0:4], in0=ps[:, 6 * 512:6 * 512 + 4], scalar1=pbias[:, 0:1], scalar2=None,
                    op0=ALU.add), reads=[("ps", 6)], writes=[("g8", w)])
                if npast > 4:
                    S.op("vector", lambda e: e.tensor_copy(
                        out=g8w[:, 4:npast], in_=ps[:, 6 * 512 + 4:6 * 512 + npast]),
                        reads=[("ps", 6)], writes=[("g8", w)])
                S.op("vector", lambda e: e.max(out=m8w, in_=g8w), reads=[("g8", w)], writes=[("m8", w)])
                S.op("vector", lambda e: e.tensor_scalar(
                    out=selw, in0=g8w, scalar1=m8w[:, 2:3], scalar2=None, op0=ALU.is_ge),
                    reads=[("g8", w), ("m8", w)], writes=[("sel", w)])
                S.op("vector", lambda e: e.tensor_scalar(
                    out=selw, in0=selw, scalar1=-1.0, scalar2=-NEG, op0=ALU.add, op1=ALU.mult),
                    reads=[("sel", w)], writes=[("sel", w)])
                S.op("vector", lambda e: e.tensor_scalar(
                    out=selw[:, 0:4], in0=selw[:, 0:4], scalar1=pbias[:, 0:1], scalar2=None, op0=ALU.add),
                    reads=[("sel", w)], writes=[("sel", w)])
            else:
                S.op("scalar", lambda e: e.activation(
                    out=zb[w][:, 0:nk], in_=ps[:, 0:nk], func=AF.Copy, scale=SCALE),
                    reads=c["sb"], writes=[("zb", w), ("zbh", w)])

        def st_exp1(n):
            c = I(n)
            w, nk, npast = c["w"], c["nk"], c["npast"]
            if c["moba"]:
                selw, rsw, w4 = sels[w], rsums[w], c["w4"]
                for nb in range(npast):
                    S.op("scalar", lambda e, nb=nb: e.activation(
                        out=Pbm[w4][:, nb * 256:(nb + 1) * 256], in_=ps[:, nb * 256:(nb + 1) * 256], func=AF.Exp,
                        scale=SCALE, bias=selw[:, nb:nb + 1], accum_out=rsw[:, nb:nb + 1]),
                        reads=[("ps", nb // 2), ("sel", w)], writes=[("Pbm", w4), ("rsum", w)])
                S.op("scalar", lambda e: e.activation(
                    out=Pbm[w4][:, npast * 256:nk], in_=ps[:, npast * 256:nk], func=AF.Exp, scale=SCALE,
                    accum_out=rsw[:, npast:npast + 1]),
                    reads=c["sb"], writes=[("Pbm", w4), ("rsum", w)])
            else:
                S.op("scalar", lambda e: e.activation(
                    out=EL[w][:, 0:1024], in_=zb[w][:, 0:1024], func=AF.Exp, bias=pbias[:, 0:1]),
                    reads=[("zb", w)], writes=[("EL", w), ("Pbm", 2 * w), ("Pbm", 2 * w + 1)])
                S.op("scalar", lambda e: e.activation(
                    out=EL[w][:, 1024:nk], in_=zb[w][:, 1024:nk], func=AF.Exp),
                    reads=[("zb", w)], writes=[("EL", w)])
                S.op("scalar", lambda e: e.activation(
                    out=EL[w][:, 0:nk], in_=EL[w][:, 0:nk], func=AF.Ln, bias=one_col[:, 0:1]),
                    reads=[("EL", w)], writes=[("EL", w)])

        def st_B(n):
            c = I(n)
            w, w4, nk, npast = c["w"], c["w4"], c["nk"], c["npast"]
            if c["moba"]:
                S.op("vector", lambda e: e.reduce_sum(
                    out=rtot[:, w:w + 1], in_=rsums[w][:, 0:npast + 1], axis=AX.X),
                    reads=[("rsum", w)], writes=[("rtot", w)])
                S.op("vector", lambda e: e.reciprocal(out=rinv4[:, w4:w4 + 1], in_=rtot[:, w:w + 1]),
                     reads=[("rtot", w)], writes=[("rinv", w4)])
            else:
                S.op("vector", lambda e: e.tensor_tensor_scan(
                    out=Cb1[:, 0:nk], data0=ones_b[:, 0:nk], data1=EL[w][:, 0:nk], initial=0.0,
                    op0=ALU.mult, op1=ALU.add), reads=[("EL", w)], writes=["Cb"])
                S.op("vector", lambda e: e.tensor_scalar(
                    out=negT[:, w:w + 1], in0=Cb1[:, nk - 1:nk], scalar1=-1.0, scalar2=None, op0=ALU.mult),
                    reads=["Cb"], writes=[("negT", w)])
                S.op("vector", lambda e: e.tensor_scalar(
                    out=negTp[:, w:w + 1], in0=negT[:, w:w + 1], scalar1=pbias[:, 0:1], scalar2=None, op0=ALU.add),
                    reads=[("negT", w)], writes=[("negTp", w)])
                S.op("gpsimd", lambda e: e.tensor_tensor(
                    out=zb[w][:, 1:nk], in0=zb[w][:, 1:nk], in1=Cb1[:, 0:nk - 1], op=ALU.add),
                    reads=["Cb", ("zb", w)], writes=[("zb", w)])

        def st_expP(n):
            c = I(n)
            w, nk = c["w"], c["nk"]
            if c["moba"]:
                return
            S.op("scalar", lambda e: e.activation(
                out=Pb[w][:, 0:1024], in_=zb[w][:, 0:1024], func=AF.Exp, bias=negTp[:, w:w + 1]),
                reads=[("zb", w), ("zbh", w), ("negTp", w)], writes=[("Pb", w)])
            S.op("scalar", lambda e: e.activation(
                out=Pb[w][:, 1024:nk], in_=zb[w][:, 1024:nk], func=AF.Exp, bias=negT[:, w:w + 1]),
                reads=[("zb", w), ("zbh", w), ("negT", w)], writes=[("Pb", w)])

        def st_T(n):
            c = I(n)
            w = c["w"]
            src, res = (Pbm[c["w4"]], ("Pbm", c["w4"])) if c["moba"] else (Pb[w], ("Pb", w))
            for kt in range(c["nkt"]):
                tb = PTB + kt // 8
                S.op("tensor", lambda e, kt=kt: e.transpose(
                    out=psb[:, PTB * 1024 + kt * 128: PTB * 1024 + (kt + 1) * 128],
                    in_=src[:, kt * 128:(kt + 1) * 128], identity=ident_b[:]),
                    reads=[res], writes=[("ps", tb)])

        def st_PTcopy(n):
            c = I(n)
            w, nk = c["w"], c["nk"]
            S.op("vector", lambda e: e.tensor_copy(
                out=PT[w][:, 0:1024], in_=psb[:, PTB * 1024: PTB * 1024 + 1024]),
                reads=[("ps", PTB)], writes=[("PTa", w)])
            S.op("vector", lambda e: e.tensor_copy(
                out=PT[w][:, 1024:nk], in_=psb[:, PTB * 1024 + 1024: PTB * 1024 + nk]),
                reads=[("ps", PTB + 1)], writes=[("PTb", w)])

        def st_PV(n):
            c = I(n)
            w, hs, nkt = c["w"], c["hs"], c["nkt"]
            for kt in range(nkt):
                S.op("tensor", lambda e, kt=kt: e.matmul(
                    out=ps[:, 7 * 512:7 * 512 + 128], lhsT=PT[w][:, kt * 128:(kt + 1) * 128],
                    rhs=v_h[hs][:, kt, :], start=(kt == 0), stop=(kt == nkt - 1)),
                    reads=[("PTa", w), ("PTb", w), ("v", hs)], writes=[("ps", 7)])

        def st_out(n):
            c = I(n)
            h, i, w4 = c["h"], c["i"], c["w4"]
            if c["moba"]:
                S.op("scalar", lambda e: e.activation(
                    out=o_sb[:, i, h * 128:(h + 1) * 128], in_=ps[:, 7 * 512:7 * 512 + 128], func=AF.Copy,
                    scale=rinv4[:, w4:w4 + 1]), reads=[("ps", 7), ("rinv", w4)], writes=[("o", i)])
            else:
                S.op("scalar", lambda e: e.activation(
                    out=o_sb[:, i, h * 128:(h + 1) * 128], in_=ps[:, 7 * 512:7 * 512 + 128], func=AF.Copy),
                    reads=[("ps", 7)], writes=[("o", i)])

        ok = lambda n: 0 <= n < N and items[n] is not None
        for p in range(-2, N + 2):
            if ok(p + 2): st_scores(p + 2)
            if ok(p): st_expP(p)
            if ok(p + 2): st_pre(p + 2)
            if ok(p + 1): st_B(p + 1)
            if ok(p + 2): st_exp1(p + 2)
            if ok(p - 1): st_T(p - 1)
            if ok(p - 1): st_PTcopy(p - 1)
            if ok(p - 2): st_PV(p - 2)
            if ok(p - 2): st_out(p - 2)

    one_col = misc[:, 8:9]
    S.op("vector", lambda e: e.memset(one_col, 1.0), writes=["one"])
    attention()
    S.barrier()
    if debug:
        for t in range(T):
            S.op("sync", lambda e, t=t: e.dma_start(out=o_dbg[t * 128:(t + 1) * 128, :], in_=o_sb[:, t, :]),
                 dma="d_dbg")
        S.barrier()
    if stage == 5:
        return finish()

    oT = hT
    norm_T(lambda t: (o_sb[:, t, :], ("o", t)), oT, 32, 2, nR_xn, nR_junk, nR_gbc, "hT")
    S.barrier()

    def op_epilogue(n, t, b):
        S.op("scalar", lambda e, t=t, n=n, b=b: e.activation(
            out=big[:, t, n * 512:(n + 1) * 512], in_=bank(b), func=AF.Copy),
            reads=[("ps", b)], writes=[("f", t)])

    cnt["pp"] = 0

    def bank_fn2(n, t):
        b = cnt["pp"] % 8
        cnt["pp"] += 1
        return b

    tok_proj(lambda k, t: oT[:, k, t * 128:(t + 1) * 128], lambda k, t: ("hT", t), KC,
             win_sb, ["d_win0", "d_win1"], lambda n: wout[n], 4, op_epilogue, bank_fn2, "win")
    postnorm_residual(big, x1_own, x2_own, 1, 1.0, gpost2)
    S.barrier()

    ffn(1, x2_own, out_d, 48, 2)

    return finish()


_NC_CACHE = {}


def _rope_tables(pos0):
    half = 16
    inv_freq = (500000.0 ** (-np.arange(0, 32, 2, dtype=np.float32) / 32)).astype(np.float32)
    pos = (pos0 + np.arange(1024, dtype=np.float32))
    ang = pos[:, None] * inv_freq[None, :]
    cos = np.cos(ang).astype(np.float32)
    sin = np.sin(ang).astype(np.float32)
    cos2 = np.concatenate([cos, cos], axis=1)
    sinS = np.concatenate([-sin, sin], axis=1)
    tab = np.stack([cos2, sinS], axis=1)
    tab = np.broadcast_to(tab[:, :, None, :], (1024, 2, 4, 32))
    tab = tab.reshape(T, 128, 2, 4, 32).transpose(1, 0, 2, 3, 4)
    return np.ascontiguousarray(tab).reshape(128, T * 2 * 4 * 32)


def prepare_inputs(inputs):
    f = lambda a: np.ascontiguousarray(np.asarray(a, dtype=np.float32))
    x = f(inputs["x"])

    def gu(wg, wu):
        g = f(wg)[0].reshape(KC, 128, NCH, 128).transpose(2, 1, 0, 3)
        u = f(wu)[0].reshape(KC, 128, NCH, 128).transpose(2, 1, 0, 3)
        return np.ascontiguousarray(np.stack([g, u], axis=2))

    def dn(w):
        return np.ascontiguousarray(f(w)[0].reshape(NJ, CPJ, 128, 4, 512).transpose(0, 3, 2, 1, 4))

    def colgrp(w, ng):
        return np.ascontiguousarray(f(w)[0].reshape(KC, 128, ng, 512).transpose(2, 1, 0, 3))

    def gcol(g):
        return f(g).reshape(KC, 128).T

    shared = {
        "wgu1": gu(inputs["ffn1_w_gate"], inputs["ffn1_w_up"]), "wd1": dn(inputs["ffn1_w_down"]),
        "wgu2": gu(inputs["ffn2_w_gate"], inputs["ffn2_w_up"]), "wd2": dn(inputs["ffn2_w_down"]),
        "win": colgrp(inputs["w_in"], 12), "wout": colgrp(inputs["w_out"], 4),
        "gT": np.ascontiguousarray(np.concatenate([
            gcol(inputs["ffn1_pre_g"]), gcol(inputs["mix_pre_g"]),
            gcol(np.concatenate([f(inputs["moba_out_g"])[0], f(inputs["sb_out_g"])[0]])),
            gcol(inputs["ffn2_pre_g"])], axis=1)),
        "gpost": np.ascontiguousarray(np.stack([
            np.broadcast_to(f(inputs[k])[0][None, :], (128, D)) for k in
            ("ffn1_post_g", "mix_post_g", "ffn2_post_g")])),
        "ident": np.eye(128, dtype=np.float32),
    }
    qi = np.arange(128)[:, None]
    ki = np.arange(128)[None, :]
    m_le = np.where(ki <= qi, 0.0, 2 * NEG).astype(np.float32)
    m_lt = np.where(ki < qi, 0.0, 2 * NEG).astype(np.float32)
    shared["masks"] = np.ascontiguousarray(np.concatenate([m_le, m_lt], axis=1))
    ropes = [_rope_tables(0.0), _rope_tables(1024.0)]
    in_maps = []
    for c in range(8):
        b, r = divmod(c, 2)
        m = dict(shared)
        m["x_own"] = np.ascontiguousarray(x[b, r * 1024:(r + 1) * 1024])
        m["x_par"] = np.ascontiguousarray(x[b, (1 - r) * 1024:(2 - r) * 1024])
        m["rope"] = np.ascontiguousarray(np.stack([ropes[1 - r], ropes[r]]))
        m["pbias"] = np.full((128, 1), NEG if r == 0 else 0.0, dtype=np.float32)
        in_maps.append(m)
    return in_maps


def kernel(**inputs):
    in_maps = prepare_inputs(inputs)
    if "nc" not in _NC_CACHE:
        _NC_CACHE["nc"] = build_nc(False)
    res = run_bass_kernel_spmd(_NC_CACHE["nc"], in_maps, core_ids=list(range(8)))
    out = np.empty((4, 2048, D), dtype=np.float32)
    for c in range(8):
        b, r = divmod(c, 2)
        out[b, r * 1024:(r + 1) * 1024] = res.results[c]["out"]
    return out
```

```python
import numpy as np
import concourse.bass as bass
import concourse.mybir as mybir
from concourse.bass_utils import run_bass_kernel_spmd

F32 = mybir.dt.float32
BF16 = mybir.dt.bfloat16
AF = mybir.ActivationFunctionType
ALU = mybir.AluOpType
AX = mybir.AxisListType

D = 2048
DFF = 5632
NCH = DFF // 128
NJ = 4
CPJ = NCH // NJ
T = 8
KC = D // 128
NH = 16
EPS = 1e-6
SCALE = 128 ** -0.5
NEG = -30000.0
KB = 1024
SB_BASE = 16512

SAME_ENGINE_SYNC = {"vector": True, "scalar": True, "gpsimd": True, "tensor": False, "sync": False}


class Sched:
    def __init__(self, nc):
        self.nc = nc
        self.engines = ["sync", "scalar", "vector", "gpsimd", "tensor"]
        self.lists = {e: [] for e in self.engines}
        self.cnt = {}
        self.semh = {}
        self.seen = {e: {} for e in self.engines}
        self.lastw = {}
        self.readers = {}
        self._stack = []

    def add_sem(self, key):
        cm = self.nc.semaphore("s_" + key)
        h = cm.__enter__()
        self._stack.append(cm)
        self.semh[key] = h
        self.cnt[key] = 0

    def close(self):
        for cm in reversed(self._stack):
            cm.__exit__(None, None, None)

    def _waits(self, engine, deps):
        need = {}
        for (sk, v) in deps:
            if v > need.get(sk, 0):
                need[sk] = v
        out = []
        for sk, v in need.items():
            if sk == engine and not SAME_ENGINE_SYNC[engine]:
                continue
            if self.seen[engine].get(sk, 0) >= v:
                continue
            self.seen[engine][sk] = v
            out.append((sk, v))
        return out

    def op(self, engine, fn, reads=(), writes=(), dma=None):
        deps = []
        for r in reads:
            t = self.lastw.get(r)
            if t:
                deps.append(t)
        for w in writes:
            t = self.lastw.get(w)
            if t:
                deps.append(t)
            deps.extend(self.readers.get(w, ()))
        waits = self._waits(engine, deps)
        if dma is None:
            self.cnt[engine] += 1
            tok = (engine, self.cnt[engine])
            inc = (engine, 1)
        else:
            self.cnt[dma] += 16
            tok = (dma, self.cnt[dma])
            inc = (dma, 16)
        self.lists[engine].append((waits, fn, inc))
        for r in reads:
            self.readers.setdefault(r, []).append(tok)
        for w in writes:
            self.lastw[w] = tok
            self.readers[w] = []
        return tok

    def barrier(self):
        for e in self.engines:
            deps = [(k, v) for k, v in self.cnt.items() if v > 0]
            waits = self._waits(e, deps)
            if waits:
                self.lists[e].append((waits, None, None))
        self.lastw = {}
        self.readers = {}

    def emit(self, block):
        nc = self.nc

        def run(eng, items):
            for waits, fn, inc in items:
                for sk, v in waits:
                    eng.wait_ge(self.semh[sk], v)
                if fn is not None:
                    ins = fn(eng)
                    ins.then_inc(self.semh[inc[0]], inc[1])

        @block.sync
        def _(e):
            run(e, self.lists["sync"])

        @block.scalar
        def _(e):
            run(e, self.lists["scalar"])

        @block.vector
        def _(e):
            run(e, self.lists["vector"])

        @block.gpsimd
        def _(e):
            run(e, self.lists["gpsimd"])

        @block.tensor
        def _(e):
            run(e, self.lists["tensor"])


import os as _os
_DBG = set(_os.environ.get("KDBG", "").split(","))


def build_nc(debug=False, stage=99):
    nc = bass.Bass("TRN2", target_bir_lowering=False)
    S = Sched(nc)

    def din(name, shape, dt=F32):
        if stage <= 3 and name in ("wgu2", "wd2", "win", "wout", "rope"):
            return None
        if stage <= 5 and name in ("wgu2", "wd2", "wout"):
            return None
        return nc.dram_tensor(name, list(shape), dt, kind="ExternalInput").ap()

    x_own = din("x_own", [T * 128, D])
    x_par = din("x_par", [T * 128, D])
    wgu = [din("wgu1", [NCH, 128, 2, KC, 128]), din("wgu2", [NCH, 128, 2, KC, 128])]
    wd = [din("wd1", [NJ, 4, 128, CPJ, 512]), din("wd2", [NJ, 4, 128, CPJ, 512])]
    win = din("win", [12, 128, KC, 512])
    wout = din("wout", [4, 128, KC, 512])
    gT_d = din("gT", [128, 64])
    gpost_d = din("gpost", [3, 128, D])
    rope_d = din("rope", [2, 128, T * 2 * 4 * 32])
    ident_d = din("ident", [128, 128])
    masks_d = din("masks", [128, 2 * 128])
    pbias_d = din("pbias", [128, 1])
    out_d = nc.dram_tensor("out", [T * 128, D], F32, kind="ExternalOutput").ap()

    dk = "ExternalOutput" if debug else "Internal"
    x1_own = nc.dram_tensor("x1_own", [T * 128, D], F32, kind=dk).ap()
    x1_par = nc.dram_tensor("x1_par", [T * 128, D], F32, kind=dk).ap()
    x2_own = nc.dram_tensor("x2_own", [T * 128, D], F32, kind=dk).ap()
    kT_dram = nc.dram_tensor("kT_dram", [NH, 128, 2048], BF16, kind="Internal").ap()
    v_dram = nc.dram_tensor("v_dram", [NH, 128, 16, 128], BF16, kind="Internal").ap()
    o_dbg = nc.dram_tensor("o_dbg", [T * 128, D], F32, kind=dk).ap() if debug else None

    def sb(name, shape, dt, off):
        return nc.alloc_sbuf_tensor_at(name, list(shape), dt, offset=SB_BASE + off)

    o = 0
    ident_f = sb("ident_f", [128, 128], F32, o); o += 512
    ident_b = sb("ident_b", [128, 128], BF16, o); o += 256
    masks_b = sb("masks_b", [128, 256], BF16, o); o += 512
    gT = sb("gT", [128, 64], F32, o); o += 256
    pbias = sb("pbias", [128, 1], F32, o); o += 32
    small = sb("small", [128, 96], F32, o); o += 384
    ones_b = sb("ones_b", [128, 2048], BF16, o); o += 4096
    small2 = sb("small2", [128, 64], F32, o); o += 256
    CONST_END = 6 * KB + 512
    assert o <= CONST_END
    A0 = CONST_END
    B0 = A0 + 64 * KB
    C0 = B0 + 32 * KB
    R0 = C0 + 32 * KB
    big = sb("big", [128, T, D], F32, A0)
    hT = sb("hT", [128, KC, 1024], BF16, B0)
    qT = sb("qT", [128, NH, 1024], BF16, C0)
    aT = sb("aT", [128, CPJ, 1024], BF16, C0)
    sg = [sb("sg%d" % i, [128, 1024], F32, C0 + 22 * KB + i * 4 * KB) for i in range(2)]
    r = R0
    wgu_sb = [sb("wgu_sb%d" % i, [128, 2, KC, 128], BF16, r + i * 8 * KB) for i in range(3)]
    r += 24 * KB
    wd_sb = [sb("wd_sb%d" % i, [128, CPJ, 512], BF16, r + i * 11 * KB) for i in range(2)]
    r += 22 * KB
    xt = sb("xt", [128, D], F32, r); r += 8 * KB
    xn = sb("xn", [128, D], F32, r); r += 8 * KB
    gpost = sb("gpost", [128, D], F32, r); r += 8 * KB
    FFN_END = r
    r = R0
    win_sb = [sb("win_sb%d" % i, [128, KC, 512], BF16, r + i * 16 * KB) for i in range(2)]
    r += 32 * KB
    rope_sb = sb("rope_sb", [128, 2, T * 2 * 4 * 32], F32, r); r += 16 * KB
    xt2 = sb("xt2", [128, D], F32, r); r += 8 * KB
    xn2 = sb("xn2", [128, D], F32, r); r += 8 * KB
    gpost2 = sb("gpost2", [128, D], F32, R0 + 32 * KB)
    qk_sb = [sb("qk_sb%d" % i, [128, 512], BF16, r + i * KB) for i in range(2)]; r += 2 * KB
    kst = [sb("kst%d" % i, [128, 4, 128], BF16, r + i * KB) for i in range(2)]; r += 2 * KB
    rt1 = sb("rt1", [128, 4, 32], F32, r); r += 512
    rt2 = sb("rt2", [128, 4, 32], F32, r); r += 512
    QKV_END = r
    r = R0
    kT_h = [sb("kT_h%d" % i, [128, 2048], BF16, r + i * 4 * KB) for i in range(2)]; r += 8 * KB
    v_h = [sb("v_h%d" % i, [128, 16, 130], BF16, r + i * 4352) for i in range(2)]; r += 2 * 4352
    EL = [sb("EL%d" % i, [128, 2048], F32, r + i * 8 * KB) for i in range(2)]; r += 16 * KB
    Pbm = [sb("Pbm%d" % i, [128, 2048], BF16, r - 16 * KB + i * 4 * KB) for i in range(4)]
    zb = [sb("zb%d" % i, [128, 2048], F32, r + i * 8 * KB) for i in range(2)]; r += 16 * KB
    Cb1 = sb("Cb1", [128, 2048], F32, r); r += 8 * KB
    Pb = [sb("Pb%d" % i, [128, 2048], BF16, r + i * 4 * KB) for i in range(2)]; r += 8 * KB
    PT = [sb("PT%d" % i, [128, 2048], BF16, r + i * 4 * KB) for i in range(2)]; r += 8 * KB
    km_f = sb("km_f", [128, 8], F32, r); r += 32
    km_b = [sb("km_b%d" % i, [128, 8], BF16, r + 32 * i) for i in range(2)]; r += 64
    ATT_END = r
    LIMIT = 229376 - SB_BASE
    assert max(FFN_END, QKV_END, ATT_END) <= LIMIT, (FFN_END, QKV_END, ATT_END)

    ps = nc.alloc_psum_tensor("ps", [128, 4096], F32)
    psb = ps.bitcast(BF16)

    def bank(b, w=512, off=0):
        return ps[:, b * 512 + off: b * 512 + off + w]

    for k in ["sync", "scalar", "vector", "gpsimd", "tensor"]:
        S.add_sem(k)
    for k in ["d_const", "d_const_sw", "d_xt0", "d_xt1", "d_xt2", "d_out0", "d_out1", "d_gpost", "d_out", "d_wgu0", "d_wgu1", "d_wgu2", "d_wd0", "d_wd1",
              "d_win0", "d_win1", "d_rope", "d_kst0", "d_kst1", "d_vst0", "d_vst1",
              "d_kT0", "d_kT1", "d_v0", "d_v1", "d_dbg"]:
        S.add_sem(k)

    ss = small[:, 0:16]
    rs = small[:, 16:32]
    rstd = small[:, 32:48]
    g8 = small[:, 48:56]
    m8 = small[:, 56:64]
    selb = small[:, 64:72]
    rsum = small[:, 72:80]
    misc = small[:, 80:96]
    g8s = [small2[:, 0:8], small2[:, 8:16]]
    m8s = [small2[:, 16:24], small2[:, 24:32]]
    sels = [small2[:, 32:40], small2[:, 40:48]]
    rsums = [small[:, 64:72], small[:, 72:80]]
    rtot = small2[:, 48:50]
    rinv4 = small2[:, 50:54]
    negT = small2[:, 54:56]
    negTp = small2[:, 56:58]

    S.op("sync", lambda e: e.dma_start(out=ident_f[:], in_=ident_d[:, :]), writes=["ident_f"], dma="d_const")
    S.op("sync", lambda e: e.dma_start(out=gT[:], in_=gT_d[:, :]), writes=["gT"], dma="d_const")
    S.op("sync", lambda e: e.dma_start(out=pbias[:], in_=pbias_d[:, :]), writes=["pbias"], dma="d_const")
    S.op("gpsimd", lambda e: e.dma_start(out=ident_b[:], in_=ident_d[:, :]), writes=["ident_b"], dma="d_const_sw")
    S.op("gpsimd", lambda e: e.dma_start(out=masks_b[:], in_=masks_d[:, :]), writes=["masks_b"], dma="d_const_sw")
    S.op("vector", lambda e: e.memset(ones_b[:], 1.0), writes=["ones_b"])
    S.barrier()

    nA_xt = [sb("nA_xt%d" % i, [128, D], F32, A0 + i * 8 * KB) for i in range(3)]
    nA_xn = [sb("nA_xn%d" % i, [128, D], F32, A0 + 24 * KB + i * 8 * KB) for i in range(2)]
    nA_junk = sb("nA_junk", [128, D], BF16, A0 + 40 * KB)
    nA_gbc = sb("nA_gbc", [128, KC, 128], F32, A0 + 44 * KB)
    nR_xn = [sb("nR_xn%d" % i, [128, D], F32, R0 + 32 * KB + i * 8 * KB) for i in range(2)]
    nR_junk = sb("nR_junk", [128, D], BF16, R0 + 48 * KB)
    nR_gbc = sb("nR_gbc", [128, KC, 128], F32, R0 + 52 * KB)
    pB_xt = [sb("pB_xt%d" % i, [128, D], F32, B0 + i * 8 * KB) for i in range(2)]
    pB_out = [sb("pB_out%d" % i, [128, D], F32, B0 + 16 * KB + i * 8 * KB) for i in range(2)]
    pC_junk = sb("pC_junk", [128, D], BF16, C0)

    def norm_T(get_src, dst_T, gcol0, ngroups, xn_ring, junk, gbc, tag):
        gw = D // ngroups
        for kc in range(KC):
            S.op("vector", lambda e, kc=kc: e.tensor_scalar(
                out=gbc[:, kc, :], in0=ones_b[:, 0:128], scalar1=gT[:, gcol0 + kc:gcol0 + kc + 1], scalar2=None,
                op0=ALU.mult), writes=["gbc"])
        srcs = {}

        def S1(t):
            src_ap, src_res = get_src(t)
            k = t % len(xn_ring)
            xn_ = xn_ring[k]
            for g in range(ngroups):
                S.op("scalar", lambda e, g=g: e.activation(
                    out=junk[:, 0:gw], in_=src_ap[:, g * gw:(g + 1) * gw], func=AF.Square,
                    accum_out=ss[:, 2 * t + g: 2 * t + g + 1]), reads=[src_res], writes=["junk", ("ss", t)])
            S.op("scalar", lambda e: e.activation(
                out=rs[:, 2 * t:2 * t + ngroups], in_=ss[:, 2 * t:2 * t + ngroups], func=AF.Sqrt,
                scale=1.0 / gw, bias=eps_col[:, 0:1]), reads=[("ss", t)], writes=[("rs", t)])
            S.op("vector", lambda e: e.reciprocal(out=rstd[:, 2 * t:2 * t + ngroups], in_=rs[:, 2 * t:2 * t + ngroups]),
                 reads=[("rs", t)], writes=[("rstd", t)])
            for g in range(ngroups):
                S.op("scalar", lambda e, g=g: e.activation(
                    out=xn_[:, g * gw:(g + 1) * gw], in_=src_ap[:, g * gw:(g + 1) * gw], func=AF.Copy,
                    scale=rstd[:, 2 * t + g:2 * t + g + 1]), reads=[src_res, ("rstd", t)], writes=[("xn", k)])

        def S2(t):
            k = t % len(xn_ring)
            xn_ = xn_ring[k]
            b0 = 4 * (t % 2)
            for kc in range(KC):
                b = b0 + kc // 4
                S.op("tensor", lambda e, kc=kc, b=b: e.transpose(
                    out=bank(b, 128, (kc % 4) * 128), in_=xn_[:, kc * 128:(kc + 1) * 128], identity=ident_f[:]),
                    reads=[("xn", k)], writes=[("ps", b)])

        def S3(t):
            b0 = 4 * (t % 2)
            for q in range(4):
                b = b0 + q
                S.op("vector", lambda e, q=q, b=b: e.tensor_tensor(
                    out=dst_T[:, 4 * q:4 * q + 4, t * 128:(t + 1) * 128],
                    in0=bank(b).rearrange("p (k j) -> p k j", k=4), in1=gbc[:, 4 * q:4 * q + 4, :], op=ALU.mult),
                    reads=[("ps", b), "gbc"], writes=[(tag, t)])

        for p_ in range(-1, T):
            if p_ + 1 < T:
                S1(p_ + 1)
            if p_ >= 0:
                S2(p_)
                S3(p_)

    eps_col = misc[:, 0:1]
    S.op("vector", lambda e: e.memset(eps_col, EPS), writes=["eps"])
    S.barrier()

    def dram_src(src, ring):
        def get(t):
            k = t % len(ring)
            S.op("sync", lambda e: e.dma_start(out=ring[k][:], in_=src[t * 128:(t + 1) * 128, :]),
                 writes=[("xt", k)], dma="d_xt%d" % k)
            return ring[k], ("xt", k)
        return get

    def postnorm_residual(fsb, src, dst, gidx, res_scale, gp_):
        S.op("sync", lambda e: e.dma_start(out=gp_[:], in_=gpost_d[gidx, :, :]), writes=["gpost"], dma="d_gpost")

        alias_B = [("hT", tt) for tt in range(T)]
        alias_C = [("aT", cc) for cc in range(CPJ)] + [("sg", 0), ("sg", 1)]

        def P1(t):
            k = t % 2
            S.op("sync", lambda e: e.dma_start(out=pB_xt[k][:], in_=src[t * 128:(t + 1) * 128, :]),
                 writes=[("xt", k)] + (alias_B if t < 2 else []), dma="d_xt%d" % k)
            S.op("scalar", lambda e: e.activation(
                out=pC_junk[:], in_=fsb[:, t, :], func=AF.Square, accum_out=ss[:, t:t + 1]),
                reads=[("f", t)], writes=["junk", ("ss", t)] + (alias_C if t == 0 else []))
            S.op("scalar", lambda e: e.activation(
                out=rs[:, t:t + 1], in_=ss[:, t:t + 1], func=AF.Sqrt, scale=1.0 / D, bias=eps_col[:, 0:1]),
                reads=[("ss", t)], writes=[("rs", t)])
            S.op("vector", lambda e: e.reciprocal(out=rstd[:, t:t + 1], in_=rs[:, t:t + 1]),
                 reads=[("rs", t)], writes=[("rstd", t)])
            S.op("vector", lambda e: e.tensor_scalar(
                out=rstd[:, 8 + t:9 + t], in0=rstd[:, t:t + 1], scalar1=float(res_scale), scalar2=None, op0=ALU.mult),
                reads=[("rstd", t)], writes=[("rstds", t)])

        def P2(t):
            k = t % 2
            S.op("vector", lambda e: e.scalar_tensor_tensor(
                out=pB_out[k][:], in0=fsb[:, t, :], scalar=rstd[:, 8 + t:9 + t], in1=gp_[:], op0=ALU.mult, op1=ALU.mult),
                reads=[("f", t), ("rstds", t), "gpost"], writes=[("po", k), ("po2", k)] + (alias_B if t < 2 else []))
            S.op("gpsimd", lambda e: e.tensor_tensor(
                out=pB_out[k][:, 0:1024], in0=pB_out[k][:, 0:1024], in1=pB_xt[k][:, 0:1024], op=ALU.add),
                reads=[("po", k), ("xt", k)], writes=[("po", k)])
            S.op("vector", lambda e: e.tensor_tensor(
                out=pB_out[k][:, 1024:2048], in0=pB_out[k][:, 1024:2048], in1=pB_xt[k][:, 1024:2048], op=ALU.add),
                reads=[("po2", k), ("xt", k)], writes=[("po2", k)])
            S.op("sync", lambda e: e.dma_start(out=dst[t * 128:(t + 1) * 128, :], in_=pB_out[k][:]),
                 reads=[("po", k), ("po2", k)], writes=[("dst", t)], dma="d_out%d" % k)

        for p_ in range(-1, T):
            if p_ + 1 < T:
                P1(p_ + 1)
            if p_ >= 0:
                P2(p_)


    def tok_proj(lhsT_fn, lhs_res_fn, nk, w_slots, w_sems, w_src_fn, ngrp, epilogue, bank_fn, w_tag):
        for n in range(ngrp):
            sl = n % len(w_slots)
            S.op("gpsimd", lambda e, n=n, sl=sl: e.dma_start(out=w_slots[sl][:], in_=w_src_fn(n)),
                 writes=[(w_tag, sl)], dma=w_sems[sl])
            for t in range(T):
                b = bank_fn(n, t)
                for k in range(nk):
                    S.op("tensor", lambda e, k=k, t=t, b=b, sl=sl: e.matmul(
                        out=bank(b), lhsT=lhsT_fn(k, t), rhs=w_slots[sl][:, k, :], start=(k == 0), stop=(k == nk - 1)),
                        reads=[(w_tag, sl), lhs_res_fn(k, t)], writes=[("ps", b)])
                epilogue(n, t, b)

    def ffn(idx, src, dst, gcol0, gidx, sub=99):
        Wgu, Wd = wgu[idx], wd[idx]
        norm_T(dram_src(src, nA_xt), hT, gcol0, 1, nA_xn, nA_junk, nA_gbc, "hT")
        if sub < 1:
            S.barrier()
            return
        for j in range(NJ):
            for cc in range(CPJ):
                c = j * CPJ + cc
                sl = c % 3
                par = c % 2
                S.op("gpsimd", lambda e, c=c, sl=sl: e.dma_start(out=wgu_sb[sl][:], in_=Wgu[c]),
                     writes=[("wgu", sl)], dma="d_wgu%d" % sl)
                for gu in range(2):
                    for kc in range(KC):
                        for half in range(2):
                            b = par * 4 + gu * 2 + half
                            S.op("tensor", lambda e, gu=gu, kc=kc, half=half, b=b, sl=sl: e.matmul(
                                out=bank(b), lhsT=wgu_sb[sl][:, gu, kc, :], rhs=hT[:, kc, half * 512:(half + 1) * 512],
                                start=(kc == 0), stop=(kc == KC - 1)),
                                reads=[("wgu", sl)] + [("hT", tt) for tt in range(half * 4, half * 4 + 4)],
                                writes=[("ps", b)])
                bg = par * 4
                S.op("scalar", lambda e, bg=bg, par=par: e.activation(
                    out=sg[par][:], in_=ps[:, bg * 512: bg * 512 + 1024], func=AF.Silu),
                    reads=[("ps", bg), ("ps", bg + 1)], writes=[("sg", par)])
                S.op("vector", lambda e, bg=bg, par=par, cc=cc: e.tensor_tensor(
                    out=aT[:, cc, :], in0=sg[par][:], in1=ps[:, (bg + 2) * 512:(bg + 2) * 512 + 1024], op=ALU.mult),
                    reads=[("sg", par), ("ps", bg + 2), ("ps", bg + 3)], writes=[("aT", cc)])
            for n in range(4):
                sl = (j * 4 + n) % 2
                S.op("gpsimd", lambda e, j=j, n=n, sl=sl: e.dma_start(out=wd_sb[sl][:], in_=Wd[j, n]),
                     writes=[("wd", sl)], dma="d_wd%d" % sl)
                for t in range(T):
                    for cc in range(CPJ):
                        S.op("tensor", lambda e, t=t, cc=cc, sl=sl: e.matmul(
                            out=bank(t), lhsT=aT[:, cc, t * 128:(t + 1) * 128], rhs=wd_sb[sl][:, cc, :],
                            start=(cc == 0), stop=(cc == CPJ - 1)),
                            reads=[("wd", sl), ("aT", cc)], writes=[("ps", t)])
                    if j == 0:
                        S.op("scalar", lambda e, t=t, n=n: e.activation(
                            out=big[:, t, n * 512:(n + 1) * 512], in_=bank(t), func=AF.Copy),
                            reads=[("ps", t)], writes=[("f", t)])
                    else:
                        S.op("vector", lambda e, t=t, n=n: e.tensor_tensor(
                            out=big[:, t, n * 512:(n + 1) * 512], in0=big[:, t, n * 512:(n + 1) * 512],
                            in1=bank(t), op=ALU.add),
                            reads=[("ps", t), ("f", t)], writes=[("f", t)])
        postnorm_residual(big, src, dst, gidx, 0.5, gpost)
        S.barrier()

    def finish():
        with nc.Block() as block:
            S.emit(block)
        S.close()
        return nc

    if stage == 0:
        return finish()
    if stage == 1:
        ffn(0, x_par, x1_par, 0, 0, sub=0)
        return finish()
    ffn(0, x_par, x1_par, 0, 0)
    if stage == 2:
        return finish()
    ffn(0, x_own, x1_own, 0, 0)
    if stage == 3:
        return finish()

    S.op("sync", lambda e: e.dma_start(out=rope_sb[:], in_=rope_d.rearrange("w p f -> p w f")),
         writes=["rope"], dma="d_rope")
    rope_v = [rope_sb[:, w, :].rearrange("p (t c h f) -> p t c h f", t=T, c=2, h=4) for w in range(2)]
    cnt = {"pp": 0, "tp": 0, "st": 0}

    def qkv_for(which, src, groups):
        norm_T(dram_src(src, nA_xt), hT, 16, 1, nA_xn, nA_junk, nA_gbc, "hT")

        pending = []

        def epilogue(n_idx, t, b):
            prev = pending.pop() if pending else None
            epilogue_main(n_idx, t, b)
            if prev is not None:
                prev()

        def epilogue_main(n_idx, t, b):
            grp = groups[n_idx]
            typ = grp // 2
            hh0 = (grp % 2) * 4
            h0 = (0 if typ < 3 else 8) + hh0
            kt = t if which == 0 else 8 + t
            st = cnt["st"] % 2
            cnt["st"] += 1
            S.op("scalar", lambda e, b=b, st=st: e.activation(out=qk_sb[st][:], in_=bank(b), func=AF.Copy),
                 reads=[("ps", b)], writes=[("qk", st)])
            if typ in (2, 5):
                if "nov" in _DBG:
                    return
                S.op("sync", lambda e, st=st, h0=h0, kt=kt: e.dma_start(
                    out=v_dram[h0:h0 + 4, :, kt, :].rearrange("h p d -> p h d"),
                    in_=qk_sb[st][:].rearrange("p (h d) -> p h d", h=4)),
                    reads=[("qk", st)], writes=[("vd", h0, kt)], dma="d_vst%d" % st)
                return
            if typ in (0, 1) and "norope" not in _DBG:
                ps4 = bank(b).rearrange("p (h d) -> p h d", h=4)
                cosv = rope_v[which][:, t, 0, :, :]
                sinv = rope_v[which][:, t, 1, :, :]
                S.op("vector", lambda e, ps4=ps4, cosv=cosv: e.tensor_tensor(
                    out=rt1[:], in0=ps4[:, :, 0:32], in1=cosv, op=ALU.mult),
                    reads=[("ps", b), "rope", ("qk", st)], writes=["rt1"])
                if "ropeA" not in _DBG:
                  S.op("vector", lambda e, ps4=ps4, sinv=sinv: e.tensor_tensor(
                    out=rt2[:, :, 0:16], in0=ps4[:, :, 16:32], in1=sinv[:, :, 0:16], op=ALU.mult),
                    reads=[("ps", b), "rope", ("qk", st)], writes=["rt2"])
                if "ropeA" not in _DBG:
                  S.op("vector", lambda e, ps4=ps4, sinv=sinv: e.tensor_tensor(
                    out=rt2[:, :, 16:32], in0=ps4[:, :, 0:16], in1=sinv[:, :, 16:32], op=ALU.mult),
                    reads=[("ps", b), "rope", ("qk", st)], writes=["rt2"])
                if "ropeA" not in _DBG and "ropeB" not in _DBG:
                  S.op("vector", lambda e, st=st: e.tensor_tensor(
                    out=qk_sb[st][:].rearrange("p (h d) -> p h d", h=4)[:, :, 0:32], in0=rt1[:], in1=rt2[:],
                    op=ALU.add), reads=["rt1", "rt2", ("qk", st)], writes=[("qk", st)])
            if "notr" in _DBG:
                return
            pending.append(lambda: epilogue_tail(typ, st, h0, t, kt))

        def epilogue_tail(typ, st, h0, t, kt):
            tb = 4 + cnt["tp"] % 4
            cnt["tp"] += 1
            for jh in range(4):
                S.op("tensor", lambda e, jh=jh, st=st, tb=tb: e.transpose(
                    out=psb[:, tb * 1024 + jh * 128: tb * 1024 + (jh + 1) * 128],
                    in_=qk_sb[st][:, jh * 128:(jh + 1) * 128], identity=ident_b[:]),
                    reads=[("qk", st)], writes=[("ps", tb)])
            tsrc = psb[:, tb * 1024: tb * 1024 + 512].rearrange("p (h j) -> p h j", h=4)
            if typ in (0, 3):
                S.op("vector", lambda e, tsrc=tsrc, h0=h0, t=t: e.tensor_copy(
                    out=qT[:, h0:h0 + 4, t * 128:(t + 1) * 128], in_=tsrc),
                    reads=[("ps", tb)], writes=[("qT", h0, t)])
            else:
                S.op("vector", lambda e, tsrc=tsrc, st=st: e.tensor_copy(out=kst[st][:], in_=tsrc),
                     reads=[("ps", tb)], writes=[("kst", st)])
                if "nok" not in _DBG:
                  S.op("sync", lambda e, st=st, h0=h0, kt=kt: e.dma_start(
                    out=kT_dram[h0:h0 + 4, :, kt * 128:(kt + 1) * 128].rearrange("h d j -> d h j"),
                    in_=kst[st][:]), reads=[("kst", st)], writes=[("kd", h0, kt)], dma="d_kst%d" % st)

        def bank_fn(n, t):
            b = cnt["pp"] % 4
            cnt["pp"] += 1
            return b

        tok_proj(lambda k, t: hT[:, k, t * 128:(t + 1) * 128], lambda k, t: ("hT", t), KC,
                 win_sb, ["d_win0", "d_win1"], lambda n: win[groups[n]], len(groups), epilogue, bank_fn, "win")
        if pending:
            pending.pop()()
        S.barrier()

    qkv_for(0, x1_par, [2, 3, 4, 5, 8, 9, 10, 11])
    qkv_for(1, x1_own, list(range(12)))
    if stage == 4:
        return finish()

    o_sb = big
    PTB = 4

    def attention():
        items = [(h, i) for h in range(8) for i in range(T)] + [None, None] + \
                [(h, i) for h in range(8, NH) for i in range(T)]
        N = len(items)

        def I(n):
            h, i = items[n]
            nk = 1024 + 128 * (i + 1)
            return dict(h=h, i=i, w=n % 2, w4=n % 4, hs=h % 2, moba=(h < 8), nk=nk, nkt=nk // 128,
                        npast=4 + i // 2, sb=[("ps", bb) for bb in range((nk + 511) // 512)])

        def st_scores(n):
            c = I(n)
            h, i, hs, nk = c["h"], c["i"], c["hs"], c["nk"]
            if i == 0:
                S.op("sync", lambda e: e.dma_start(out=kT_h[hs][:], in_=kT_dram[h]),
                     writes=[("kT", hs)], dma="d_kT%d" % hs)
                S.op("sync", lambda e: e.dma_start(out=v_h[hs][:, :, 0:128], in_=v_dram[h]),
                     writes=[("v", hs)], dma="d_v%d" % hs)
                if c["moba"]:
                    S.op("vector", lambda e: e.tensor_reduce(
                        out=km_f[:], in_=kT_h[hs][:].rearrange("p (n k) -> p n k", k=256), axis=AX.X, op=ALU.add),
                        reads=[("kT", hs)], writes=["km_f"])
                    S.op("vector", lambda e: e.tensor_copy(out=km_b[hs][:], in_=km_f[:]),
                         reads=["km_f"], writes=[("km_b", hs)])
                    for ww in range(2):
                        S.op("vector", lambda e, ww=ww: e.memset(g8s[ww], -1e30), writes=[("g8", ww)])
            qTi = qT[:, h, i * 128:(i + 1) * 128]
            mask = masks_b[:, 0:128] if c["moba"] else masks_b[:, 128:256]
            for c0 in range(0, nk, 512):
                wd_ = min(512, nk - c0)
                last = (c0 + wd_ == nk)
                S.op("tensor", lambda e, c0=c0, wd_=wd_, last=last: e.matmul(
                    out=ps[:, c0:c0 + wd_], lhsT=qTi, rhs=kT_h[hs][:, c0:c0 + wd_], start=True, stop=(not last)),
                    reads=[("kT", hs)], writes=[("ps", c0 // 512)])
            S.op("tensor", lambda e: e.matmul(
                out=ps[:, nk - 128:nk], lhsT=ident_b[:], rhs=mask, start=False, stop=True),
                writes=[("ps", (nk - 128) // 512)])
            if c["moba"]:
                S.op("tensor", lambda e: e.matmul(
                    out=ps[:, 6 * 512:6 * 512 + 8], lhsT=qTi, rhs=km_b[hs][:], start=True, stop=True),
                    reads=[("km_b", hs)], writes=[("ps", 6)])

        def st_pre(n):
            c = I(n)
            w, nk, npast = c["w"], c["nk"], c["npast"]
            if c["moba"]:
                g8w, m8w, selw = g8s[w], m8s[w], sels[w]
                S.op("vector", lambda e: e.tensor_scalar(
                    out=g8w[:, 0:4], in0=ps[:, 6 * 512:6 * 512 + 4], scalar1=pbias[:, 0:1], scalar2=None,
                    op0=ALU.add), reads=[("ps", 6)], writes=[("g8", w)])
                if npast > 4:
                    S.op("vector", lambda e: e.tensor_copy(
                        out=g8w[:, 4:npast], in_=ps[:, 6 * 512 + 4:6 * 512 + npast]),
                        reads=[("ps", 6)], writes=[("g8", w)])
                S.op("vector", lambda e: e.max(out=m8w, in_=g8w), reads=[("g8", w)], writes=[("m8", w)])
                S.op("vector", lambda e: e.tensor_scalar(
                    out=selw, in0=g8w, scalar1=m8w[:, 2:3], scalar2=None, op0=ALU.is_ge),
                    reads=[("g8", w), ("m8", w)], writes=[("sel", w)])
                S.op("vector", lambda e: e.tensor_scalar(
                    out=selw, in0=selw, scalar1=-1.0, scalar2=-NEG, op0=ALU.add, op1=ALU.mult),
                    reads=[("sel", w)], writes=[("sel", w)])
                S.op("vector", lambda e: e.tensor_scalar(
                    out=selw[:, 0:4], in0=selw[:, 0:4], scalar1=pbias[:, 0:1], scalar2=None, op0=ALU.add),
                    reads=[("sel", w)], writes=[("sel", w)])
            else:
                S.op("scalar", lambda e: e.activation(
                    out=zb[w][:, 0:nk], in_=ps[:, 0:nk], func=AF.Copy, scale=SCALE),
                    reads=c["sb"], writes=[("zb", w), ("zbh", w)])

        def st_exp1(n):
            c = I(n)
            w, nk, npast = c["w"], c["nk"], c["npast"]
            if c["moba"]:
                selw, rsw, w4 = sels[w], rsums[w], c["w4"]
                for nb in range(npast):
                    S.op("scalar", lambda e, nb=nb: e.activation(
                        out=Pbm[w4][:, nb * 256:(nb + 1) * 256], in_=ps[:, nb * 256:(nb + 1) * 256], func=AF.Exp,
                        scale=SCALE, bias=selw[:, nb:nb + 1]),
                        reads=[("ps", nb // 2), ("sel", w)], writes=[("Pbm", w4)])
                S.op("scalar", lambda e: e.activation(
                    out=Pbm[w4][:, npast * 256:nk], in_=ps[:, npast * 256:nk], func=AF.Exp, scale=SCALE),
                    reads=c["sb"], writes=[("Pbm", w4)])
            else:
                S.op("scalar", lambda e: e.activation(
                    out=EL[w][:, 0:1024], in_=zb[w][:, 0:1024], func=AF.Exp, bias=pbias[:, 0:1]),
                    reads=[("zb", w)], writes=[("EL", w), ("Pbm", 2 * w), ("Pbm", 2 * w + 1)])
                S.op("scalar", lambda e: e.activation(
                    out=EL[w][:, 1024:nk], in_=zb[w][:, 1024:nk], func=AF.Exp),
                    reads=[("zb", w)], writes=[("EL", w)])
                S.op("scalar", lambda e: e.activation(
                    out=EL[w][:, 0:nk], in_=EL[w][:, 0:nk], func=AF.Ln, bias=one_col[:, 0:1]),
                    reads=[("EL", w)], writes=[("EL", w)])

        def st_B(n):
            c = I(n)
            w, w4, nk, npast = c["w"], c["w4"], c["nk"], c["npast"]
            if c["moba"]:
                return
            else:
                S.op("vector", lambda e: e.tensor_tensor_scan(
                    out=Cb1[:, 0:nk], data0=ones_b[:, 0:nk], data1=EL[w][:, 0:nk], initial=0.0,
                    op0=ALU.mult, op1=ALU.add), reads=[("EL", w)], writes=["Cb"])
                S.op("vector", lambda e: e.tensor_scalar(
                    out=negT[:, w:w + 1], in0=Cb1[:, nk - 1:nk], scalar1=-1.0, scalar2=None, op0=ALU.mult),
                    reads=["Cb"], writes=[("negT", w)])
                S.op("vector", lambda e: e.tensor_scalar(
                    out=negTp[:, w:w + 1], in0=negT[:, w:w + 1], scalar1=pbias[:, 0:1], scalar2=None, op0=ALU.add),
                    reads=[("negT", w)], writes=[("negTp", w)])
                S.op("gpsimd", lambda e: e.tensor_tensor(
                    out=zb[w][:, 1:nk], in0=zb[w][:, 1:nk], in1=Cb1[:, 0:nk - 1], op=ALU.add),
                    reads=["Cb", ("zb", w)], writes=[("zb", w)])

        def st_expP(n):
            c = I(n)
            w, nk = c["w"], c["nk"]
            if c["moba"]:
                return
            S.op("scalar", lambda e: e.activation(
                out=Pb[w][:, 0:1024], in_=zb[w][:, 0:1024], func=AF.Exp, bias=negTp[:, w:w + 1]),
                reads=[("zb", w), ("zbh", w), ("negTp", w)], writes=[("Pb", w)])
            S.op("scalar", lambda e: e.activation(
                out=Pb[w][:, 1024:nk], in_=zb[w][:, 1024:nk], func=AF.Exp, bias=negT[:, w:w + 1]),
                reads=[("zb", w), ("zbh", w), ("negT", w)], writes=[("Pb", w)])

        def st_T(n):
            c = I(n)
            w = c["w"]
            src, res = (Pbm[c["w4"]], ("Pbm", c["w4"])) if c["moba"] else (Pb[w], ("Pb", w))
            for kt in range(c["nkt"]):
                tb = PTB + kt // 8
                S.op("tensor", lambda e, kt=kt: e.transpose(
                    out=psb[:, PTB * 1024 + kt * 128: PTB * 1024 + (kt + 1) * 128],
                    in_=src[:, kt * 128:(kt + 1) * 128], identity=ident_b[:]),
                    reads=[res], writes=[("ps", tb)])

        def st_PTcopy(n):
            c = I(n)
            w, nk = c["w"], c["nk"]
            S.op("vector", lambda e: e.tensor_copy(
                out=PT[w][:, 0:1024], in_=psb[:, PTB * 1024: PTB * 1024 + 1024]),
                reads=[("ps", PTB)], writes=[("PTa", w)])
            S.op("vector", lambda e: e.tensor_copy(
                out=PT[w][:, 1024:nk], in_=psb[:, PTB * 1024 + 1024: PTB * 1024 + nk]),
                reads=[("ps", PTB + 1)], writes=[("PTb", w)])

        def st_PV(n):
            c = I(n)
            w, hs, nkt = c["w"], c["hs"], c["nkt"]
            for kt in range(nkt):
                S.op("tensor", lambda e, kt=kt: e.matmul(
                    out=ps[:, 7 * 512:7 * 512 + 129], lhsT=PT[w][:, kt * 128:(kt + 1) * 128],
                    rhs=v_h[hs][:, kt, 0:129], start=(kt == 0), stop=(kt == nkt - 1)),
                    reads=[("PTa", w), ("PTb", w), ("v", hs)], writes=[("ps", 7)])

        def st_out(n):
            c = I(n)
            h, i, w4 = c["h"], c["i"], c["w4"]
            if c["moba"]:
                S.op("vector", lambda e: e.reciprocal(
                    out=rinv4[:, w4:w4 + 1], in_=ps[:, 7 * 512 + 128:7 * 512 + 129]),
                    reads=[("ps", 7)], writes=[("rinv", w4)])
                S.op("scalar", lambda e: e.activation(
                    out=o_sb[:, i, h * 128:(h + 1) * 128], in_=ps[:, 7 * 512:7 * 512 + 128], func=AF.Copy,
                    scale=rinv4[:, w4:w4 + 1]), reads=[("ps", 7), ("rinv", w4)], writes=[("o", i)])
            else:
                S.op("scalar", lambda e: e.activation(
                    out=o_sb[:, i, h * 128:(h + 1) * 128], in_=ps[:, 7 * 512:7 * 512 + 128], func=AF.Copy),
                    reads=[("ps", 7)], writes=[("o", i)])

        ok = lambda n: 0 <= n < N and items[n] is not None
        for p in range(-2, N + 2):
            if ok(p + 2): st_scores(p + 2)
            if ok(p): st_expP(p)
            if ok(p + 2): st_pre(p + 2)
            if ok(p + 1): st_B(p + 1)
            if ok(p + 2): st_exp1(p + 2)
            if ok(p - 1): st_T(p - 1)
            if ok(p - 1): st_PTcopy(p - 1)
            if ok(p - 2): st_PV(p - 2)
            if ok(p - 2): st_out(p - 2)

    one_col = misc[:, 8:9]
    S.op("vector", lambda e: e.memset(one_col, 1.0), writes=["one"])
    for _i in range(2):
        S.op("vector", lambda e, _i=_i: e.memset(v_h[_i][:, :, 128:130], 1.0), writes=[("vones", _i)])
    attention()
    S.barrier()
    if debug:
        for t in range(T):
            S.op("sync", lambda e, t=t: e.dma_start(out=o_dbg[t * 128:(t + 1) * 128, :], in_=o_sb[:, t, :]),
                 dma="d_dbg")
        S.barrier()
    if stage == 5:
        return finish()

    oT = hT
    norm_T(lambda t: (o_sb[:, t, :], ("o", t)), oT, 32, 2, nR_xn, nR_junk, nR_gbc, "hT")
    S.barrier()

    def op_epilogue(n, t, b):
        S.op("scalar", lambda e, t=t, n=n, b=b: e.activation(
            out=big[:, t, n * 512:(n + 1) * 512], in_=bank(b), func=AF.Copy),
            reads=[("ps", b)], writes=[("f", t)])

    cnt["pp"] = 0

    def bank_fn2(n, t):
        b = cnt["pp"] % 8
        cnt["pp"] += 1
        return b

    tok_proj(lambda k, t: oT[:, k, t * 128:(t + 1) * 128], lambda k, t: ("hT", t), KC,
             win_sb, ["d_win0", "d_win1"], lambda n: wout[n], 4, op_epilogue, bank_fn2, "win")
    postnorm_residual(big, x1_own, x2_own, 1, 1.0, gpost2)
    S.barrier()

    ffn(1, x2_own, out_d, 48, 2)

    return finish()


_NC_CACHE = {}


def _rope_tables(pos0):
    half = 16
    inv_freq = (500000.0 ** (-np.arange(0, 32, 2, dtype=np.float32) / 32)).astype(np.float32)
    pos = (pos0 + np.arange(1024, dtype=np.float32))
    ang = pos[:, None] * inv_freq[None, :]
    cos = np.cos(ang).astype(np.float32)
    sin = np.sin(ang).astype(np.float32)
    cos2 = np.concatenate([cos, cos], axis=1)
    sinS = np.concatenate([-sin, sin], axis=1)
    tab = np.stack([cos2, sinS], axis=1)
    tab = np.broadcast_to(tab[:, :, None, :], (1024, 2, 4, 32))
    tab = tab.reshape(T, 128, 2, 4, 32).transpose(1, 0, 2, 3, 4)
    return np.ascontiguousarray(tab).reshape(128, T * 2 * 4 * 32)


def prepare_inputs(inputs):
    f = lambda a: np.ascontiguousarray(np.asarray(a, dtype=np.float32))
    x = f(inputs["x"])

    def gu(wg, wu):
        g = f(wg)[0].reshape(KC, 128, NCH, 128).transpose(2, 1, 0, 3)
        u = f(wu)[0].reshape(KC, 128, NCH, 128).transpose(2, 1, 0, 3)
        return np.ascontiguousarray(np.stack([g, u], axis=2))

    def dn(w):
        return np.ascontiguousarray(f(w)[0].reshape(NJ, CPJ, 128, 4, 512).transpose(0, 3, 2, 1, 4))

    def colgrp(w, ng):
        return np.ascontiguousarray(f(w)[0].reshape(KC, 128, ng, 512).transpose(2, 1, 0, 3))

    def gcol(g):
        return f(g).reshape(KC, 128).T

    shared = {
        "wgu1": gu(inputs["ffn1_w_gate"], inputs["ffn1_w_up"]), "wd1": dn(inputs["ffn1_w_down"]),
        "wgu2": gu(inputs["ffn2_w_gate"], inputs["ffn2_w_up"]), "wd2": dn(inputs["ffn2_w_down"]),
        "win": colgrp(inputs["w_in"], 12), "wout": colgrp(inputs["w_out"], 4),
        "gT": np.ascontiguousarray(np.concatenate([
            gcol(inputs["ffn1_pre_g"]), gcol(inputs["mix_pre_g"]),
            gcol(np.concatenate([f(inputs["moba_out_g"])[0], f(inputs["sb_out_g"])[0]])),
            gcol(inputs["ffn2_pre_g"])], axis=1)),
        "gpost": np.ascontiguousarray(np.stack([
            np.broadcast_to(f(inputs[k])[0][None, :], (128, D)) for k in
            ("ffn1_post_g", "mix_post_g", "ffn2_post_g")])),
        "ident": np.eye(128, dtype=np.float32),
    }
    qi = np.arange(128)[:, None]
    ki = np.arange(128)[None, :]
    m_le = np.where(ki <= qi, 0.0, 2 * NEG).astype(np.float32)
    m_lt = np.where(ki < qi, 0.0, 2 * NEG).astype(np.float32)
    shared["masks"] = np.ascontiguousarray(np.concatenate([m_le, m_lt], axis=1))
    ropes = [_rope_tables(0.0), _rope_tables(1024.0)]
    in_maps = []
    for c in range(8):
        b, r = divmod(c, 2)
        m = dict(shared)
        m["x_own"] = np.ascontiguousarray(x[b, r * 1024:(r + 1) * 1024])
        m["x_par"] = np.ascontiguousarray(x[b, (1 - r) * 1024:(2 - r) * 1024])
        m["rope"] = np.ascontiguousarray(np.stack([ropes[1 - r], ropes[r]]))
        m["pbias"] = np.full((128, 1), NEG if r == 0 else 0.0, dtype=np.float32)
        in_maps.append(m)
    return in_maps


def kernel(**inputs):
    in_maps = prepare_inputs(inputs)
    if "nc" not in _NC_CACHE:
        _NC_CACHE["nc"] = build_nc(False)
    res = run_bass_kernel_spmd(_NC_CACHE["nc"], in_maps, core_ids=list(range(8)))
    out = np.empty((4, 2048, D), dtype=np.float32)
    for c in range(8):
        b, r = divmod(c, 2)
        out[b, r * 1024:(r + 1) * 1024] = res.results[c]["out"]
    return out
```

```python
import numpy as np
import concourse.bass as bass
import concourse.mybir as mybir
from concourse.bass_utils import run_bass_kernel_spmd

F32 = mybir.dt.float32
BF16 = mybir.dt.bfloat16
AF = mybir.ActivationFunctionType
ALU = mybir.AluOpType
AX = mybir.AxisListType

D = 2048
DFF = 5632
NCH = DFF // 128
NJ = 4
CPJ = NCH // NJ
T = 8
KC = D // 128
NH = 16
EPS = 1e-6
SCALE = 128 ** -0.5
NEG = -30000.0
KB = 1024
SB_BASE = 16512

SAME_ENGINE_SYNC = {"vector": True, "scalar": True, "gpsimd": True, "tensor": False, "sync": False}


class Sched:
    def __init__(self, nc):
        self.nc = nc
        self.engines = ["sync", "scalar", "vector", "gpsimd", "tensor"]
        self.lists = {e: [] for e in self.engines}
        self.cnt = {}
        self.semh = {}
        self.seen = {e: {} for e in self.engines}
        self.lastw = {}
        self.readers = {}
        self._stack = []

    def add_sem(self, key):
        cm = self.nc.semaphore("s_" + key)
        h = cm.__enter__()
        self._stack.append(cm)
        self.semh[key] = h
        self.cnt[key] = 0

    def close(self):
        for cm in reversed(self._stack):
            cm.__exit__(None, None, None)

    def _waits(self, engine, deps):
        need = {}
        for (sk, v) in deps:
            if v > need.get(sk, 0):
                need[sk] = v
        out = []
        for sk, v in need.items():
            if sk == engine and not SAME_ENGINE_SYNC[engine]:
                continue
            if self.seen[engine].get(sk, 0) >= v:
                continue
            self.seen[engine][sk] = v
            out.append((sk, v))
        return out

    def op(self, engine, fn, reads=(), writes=(), dma=None):
        deps = []
        for r in reads:
            t = self.lastw.get(r)
            if t:
                deps.append(t)
        for w in writes:
            t = self.lastw.get(w)
            if t:
                deps.append(t)
            deps.extend(self.readers.get(w, ()))
        waits = self._waits(engine, deps)
        if dma is None:
            self.cnt[engine] += 1
            tok = (engine, self.cnt[engine])
            inc = (engine, 1)
        else:
            self.cnt[dma] += 16
            tok = (dma, self.cnt[dma])
            inc = (dma, 16)
        self.lists[engine].append((waits, fn, inc))
        for r in reads:
            self.readers.setdefault(r, []).append(tok)
        for w in writes:
            self.lastw[w] = tok
            self.readers[w] = []
        return tok

    def barrier(self):
        for e in self.engines:
            deps = [(k, v) for k, v in self.cnt.items() if v > 0]
            waits = self._waits(e, deps)
            if waits:
                self.lists[e].append((waits, None, None))
        self.lastw = {}
        self.readers = {}

    def emit(self, block):
        nc = self.nc

        def run(eng, items):
            for waits, fn, inc in items:
                for sk, v in waits:
                    eng.wait_ge(self.semh[sk], v)
                if fn is not None:
                    ins = fn(eng)
                    ins.then_inc(self.semh[inc[0]], inc[1])

        @block.sync
        def _(e):
            run(e, self.lists["sync"])

        @block.scalar
        def _(e):
            run(e, self.lists["scalar"])

        @block.vector
        def _(e):
            run(e, self.lists["vector"])

        @block.gpsimd
        def _(e):
            run(e, self.lists["gpsimd"])

        @block.tensor
        def _(e):
            run(e, self.lists["tensor"])


import os as _os
_DBG = set(_os.environ.get("KDBG", "").split(","))


def build_nc(debug=False, stage=99):
    nc = bass.Bass("TRN2", target_bir_lowering=False)
    S = Sched(nc)

    def din(name, shape, dt=F32):
        if stage <= 3 and name in ("wgu2", "wd2", "win", "wout", "rope"):
            return None
        if stage <= 5 and name in ("wgu2", "wd2", "wout"):
            return None
        return nc.dram_tensor(name, list(shape), dt, kind="ExternalInput").ap()

    x_own = din("x_own", [T * 128, D])
    x_par = din("x_par", [T * 128, D])
    wgu = [din("wgu1", [NCH, 128, 2, KC, 128]), din("wgu2", [NCH, 128, 2, KC, 128])]
    wd = [din("wd1", [NJ, 4, 128, CPJ, 512]), din("wd2", [NJ, 4, 128, CPJ, 512])]
    win = din("win", [12, 128, KC, 512])
    wout = din("wout", [4, 128, KC, 512])
    gT_d = din("gT", [128, 64])
    gpost_d = din("gpost", [3, 128, D])
    rope_d = din("rope", [2, 128, T * 2 * 4 * 32])
    ident_d = din("ident", [128, 128])
    masks_d = din("masks", [128, 2 * 128])
    pbias_d = din("pbias", [128, 1])
    out_d = nc.dram_tensor("out", [T * 128, D], F32, kind="ExternalOutput").ap()

    dk = "ExternalOutput" if debug else "Internal"
    x1_own = nc.dram_tensor("x1_own", [T * 128, D], F32, kind=dk).ap()
    x1_par = nc.dram_tensor("x1_par", [T * 128, D], F32, kind=dk).ap()
    x2_own = nc.dram_tensor("x2_own", [T * 128, D], F32, kind=dk).ap()
    kT_dram = nc.dram_tensor("kT_dram", [NH, 128, 2048], BF16, kind="Internal").ap()
    v_dram = nc.dram_tensor("v_dram", [NH, 128, 16, 128], BF16, kind="Internal").ap()
    o_dbg = nc.dram_tensor("o_dbg", [T * 128, D], F32, kind=dk).ap() if debug else None

    def sb(name, shape, dt, off):
        return nc.alloc_sbuf_tensor_at(name, list(shape), dt, offset=SB_BASE + off)

    o = 0
    ident_f = sb("ident_f", [128, 128], F32, o); o += 512
    ident_b = sb("ident_b", [128, 128], BF16, o); o += 256
    masks_b = sb("masks_b", [128, 256], BF16, o); o += 512
    gT = sb("gT", [128, 64], F32, o); o += 256
    pbias = sb("pbias", [128, 1], F32, o); o += 32
    small = sb("small", [128, 96], F32, o); o += 384
    ones_b = sb("ones_b", [128, 2048], BF16, o); o += 4096
    small2 = sb("small2", [128, 64], F32, o); o += 256
    CONST_END = 6 * KB + 512
    assert o <= CONST_END
    A0 = CONST_END
    B0 = A0 + 64 * KB
    C0 = B0 + 32 * KB
    R0 = C0 + 32 * KB
    big = sb("big", [128, T, D], F32, A0)
    hT = sb("hT", [128, KC, 1024], BF16, B0)
    qT = sb("qT", [128, NH, 1024], BF16, C0)
    aT = sb("aT", [128, CPJ, 1024], BF16, C0)
    sg = [sb("sg%d" % i, [128, 1024], F32, C0 + 22 * KB + i * 4 * KB) for i in range(2)]
    r = R0
    wgu_sb = [sb("wgu_sb%d" % i, [128, 2, KC, 128], BF16, r + i * 8 * KB) for i in range(3)]
    r += 24 * KB
    wd_sb = [sb("wd_sb%d" % i, [128, CPJ, 512], BF16, r + i * 11 * KB) for i in range(2)]
    r += 22 * KB
    xt = sb("xt", [128, D], F32, r); r += 8 * KB
    xn = sb("xn", [128, D], F32, r); r += 8 * KB
    gpost = sb("gpost", [128, D], F32, r); r += 8 * KB
    FFN_END = r
    r = R0
    win_sb = [sb("win_sb%d" % i, [128, KC, 512], BF16, r + i * 16 * KB) for i in range(2)]
    r += 32 * KB
    rope_sb = sb("rope_sb", [128, 2, T * 2 * 4 * 32], F32, r); r += 16 * KB
    xt2 = sb("xt2", [128, D], F32, r); r += 8 * KB
    xn2 = sb("xn2", [128, D], F32, r); r += 8 * KB
    gpost2 = sb("gpost2", [128, D], F32, R0 + 32 * KB)
    qk_sb = [sb("qk_sb%d" % i, [128, 512], BF16, r + i * KB) for i in range(2)]; r += 2 * KB
    kst = [sb("kst%d" % i, [128, 4, 128], BF16, r + i * KB) for i in range(2)]; r += 2 * KB
    rt1 = sb("rt1", [128, 4, 32], F32, r); r += 512
    rt2 = sb("rt2", [128, 4, 32], F32, r); r += 512
    QKV_END = r
    r = R0
    kT_h = [sb("kT_h%d" % i, [128, 2048], BF16, r + i * 4 * KB) for i in range(2)]; r += 8 * KB
    v_h = [sb("v_h%d" % i, [128, 16, 128], BF16, r + i * 4 * KB) for i in range(2)]; r += 8 * KB
    EL = [sb("EL%d" % i, [128, 2048], F32, r + i * 8 * KB) for i in range(2)]; r += 16 * KB
    Pbm = [sb("Pbm%d" % i, [128, 2048], BF16, r - 16 * KB + i * 4 * KB) for i in range(4)]
    zb = [sb("zb%d" % i, [128, 2048], F32, r + i * 8 * KB) for i in range(2)]; r += 16 * KB
    Cb1 = sb("Cb1", [128, 2048], F32, r); r += 8 * KB
    Pb = [sb("Pb%d" % i, [128, 2048], BF16, r + i * 4 * KB) for i in range(2)]; r += 8 * KB
    PT = [sb("PT%d" % i, [128, 2048], BF16, r + i * 4 * KB) for i in range(2)]; r += 8 * KB
    km_f = sb("km_f", [128, 8], F32, r); r += 32
    km_b = [sb("km_b%d" % i, [128, 8], BF16, r + 32 * i) for i in range(2)]; r += 64
    ATT_END = r
    LIMIT = 229376 - SB_BASE
    assert max(FFN_END, QKV_END, ATT_END) <= LIMIT, (FFN_END, QKV_END, ATT_END)

    ps = nc.alloc_psum_tensor("ps", [128, 4096], F32)
    psb = ps.bitcast(BF16)

    def bank(b, w=512, off=0):
        return ps[:, b * 512 + off: b * 512 + off + w]

    for k in ["sync", "scalar", "vector", "gpsimd", "tensor"]:
        S.add_sem(k)
    for k in ["d_const", "d_const_sw", "d_xt0", "d_xt1", "d_xt2", "d_out0", "d_out1", "d_gpost", "d_out", "d_wgu0", "d_wgu1", "d_wgu2", "d_wd0", "d_wd1",
              "d_win0", "d_win1", "d_rope", "d_kst0", "d_kst1", "d_vst0", "d_vst1",
              "d_kT0", "d_kT1", "d_v0", "d_v1", "d_dbg"]:
        S.add_sem(k)

    ss = small[:, 0:16]
    rs = small[:, 16:32]
    rstd = small[:, 32:48]
    g8 = small[:, 48:56]
    m8 = small[:, 56:64]
    selb = small[:, 64:72]
    rsum = small[:, 72:80]
    misc = small[:, 80:96]
    g8s = [small2[:, 0:8], small2[:, 8:16]]
    m8s = [small2[:, 16:24], small2[:, 24:32]]
    sels = [small2[:, 32:40], small2[:, 40:48]]
    rsums = [small[:, 64:72], small[:, 72:80]]
    rtot = small2[:, 48:50]
    rinv4 = small2[:, 50:54]
    negT = small2[:, 54:56]
    negTp = small2[:, 56:58]

    S.op("sync", lambda e: e.dma_start(out=ident_f[:], in_=ident_d[:, :]), writes=["ident_f"], dma="d_const")
    S.op("sync", lambda e: e.dma_start(out=gT[:], in_=gT_d[:, :]), writes=["gT"], dma="d_const")
    S.op("sync", lambda e: e.dma_start(out=pbias[:], in_=pbias_d[:, :]), writes=["pbias"], dma="d_const")
    S.op("gpsimd", lambda e: e.dma_start(out=ident_b[:], in_=ident_d[:, :]), writes=["ident_b"], dma="d_const_sw")
    S.op("gpsimd", lambda e: e.dma_start(out=masks_b[:], in_=masks_d[:, :]), writes=["masks_b"], dma="d_const_sw")
    S.op("vector", lambda e: e.memset(ones_b[:], 1.0), writes=["ones_b"])
    S.barrier()

    nA_xt = [sb("nA_xt%d" % i, [128, D], F32, A0 + i * 8 * KB) for i in range(3)]
    nA_xn = [sb("nA_xn%d" % i, [128, D], F32, A0 + 24 * KB + i * 8 * KB) for i in range(2)]
    nA_junk = sb("nA_junk", [128, D], BF16, A0 + 40 * KB)
    nA_gbc = sb("nA_gbc", [128, KC, 128], F32, A0 + 44 * KB)
    nR_xn = [sb("nR_xn%d" % i, [128, D], F32, R0 + 32 * KB + i * 8 * KB) for i in range(2)]
    nR_junk = sb("nR_junk", [128, D], BF16, R0 + 48 * KB)
    nR_gbc = sb("nR_gbc", [128, KC, 128], F32, R0 + 52 * KB)
    pB_xt = [sb("pB_xt%d" % i, [128, D], F32, B0 + i * 8 * KB) for i in range(2)]
    pB_out = [sb("pB_out%d" % i, [128, D], F32, B0 + 16 * KB + i * 8 * KB) for i in range(2)]
    pC_junk = sb("pC_junk", [128, D], BF16, C0)

    def norm_T(get_src, dst_T, gcol0, ngroups, xn_ring, junk, gbc, tag):
        gw = D // ngroups
        for kc in range(KC):
            S.op("vector", lambda e, kc=kc: e.tensor_scalar(
                out=gbc[:, kc, :], in0=ones_b[:, 0:128], scalar1=gT[:, gcol0 + kc:gcol0 + kc + 1], scalar2=None,
                op0=ALU.mult), writes=["gbc"])
        srcs = {}

        def S1(t):
            src_ap, src_res = get_src(t)
            k = t % len(xn_ring)
            xn_ = xn_ring[k]
            for g in range(ngroups):
                S.op("scalar", lambda e, g=g: e.activation(
                    out=junk[:, 0:gw], in_=src_ap[:, g * gw:(g + 1) * gw], func=AF.Square,
                    accum_out=ss[:, 2 * t + g: 2 * t + g + 1]), reads=[src_res], writes=["junk", ("ss", t)])
            S.op("scalar", lambda e: e.activation(
                out=rs[:, 2 * t:2 * t + ngroups], in_=ss[:, 2 * t:2 * t + ngroups], func=AF.Sqrt,
                scale=1.0 / gw, bias=eps_col[:, 0:1]), reads=[("ss", t)], writes=[("rs", t)])
            S.op("vector", lambda e: e.reciprocal(out=rstd[:, 2 * t:2 * t + ngroups], in_=rs[:, 2 * t:2 * t + ngroups]),
                 reads=[("rs", t)], writes=[("rstd", t)])
            for g in range(ngroups):
                S.op("scalar", lambda e, g=g: e.activation(
                    out=xn_[:, g * gw:(g + 1) * gw], in_=src_ap[:, g * gw:(g + 1) * gw], func=AF.Copy,
                    scale=rstd[:, 2 * t + g:2 * t + g + 1]), reads=[src_res, ("rstd", t)], writes=[("xn", k)])

        def S2(t):
            k = t % len(xn_ring)
            xn_ = xn_ring[k]
            b0 = 4 * (t % 2)
            for kc in range(KC):
                b = b0 + kc // 4
                S.op("tensor", lambda e, kc=kc, b=b: e.transpose(
                    out=bank(b, 128, (kc % 4) * 128), in_=xn_[:, kc * 128:(kc + 1) * 128], identity=ident_f[:]),
                    reads=[("xn", k)], writes=[("ps", b)])

        def S3(t):
            b0 = 4 * (t % 2)
            for q in range(4):
                b = b0 + q
                S.op("vector", lambda e, q=q, b=b: e.tensor_tensor(
                    out=dst_T[:, 4 * q:4 * q + 4, t * 128:(t + 1) * 128],
                    in0=bank(b).rearrange("p (k j) -> p k j", k=4), in1=gbc[:, 4 * q:4 * q + 4, :], op=ALU.mult),
                    reads=[("ps", b), "gbc"], writes=[(tag, t)])

        for p_ in range(-1, T):
            if p_ + 1 < T:
                S1(p_ + 1)
            if p_ >= 0:
                S2(p_)
                S3(p_)

    eps_col = misc[:, 0:1]
    S.op("vector", lambda e: e.memset(eps_col, EPS), writes=["eps"])
    S.barrier()

    def dram_src(src, ring):
        def get(t):
            k = t % len(ring)
            S.op("sync", lambda e: e.dma_start(out=ring[k][:], in_=src[t * 128:(t + 1) * 128, :]),
                 writes=[("xt", k)], dma="d_xt%d" % k)
            return ring[k], ("xt", k)
        return get

    def postnorm_residual(fsb, src, dst, gidx, res_scale, gp_):
        S.op("sync", lambda e: e.dma_start(out=gp_[:], in_=gpost_d[gidx, :, :]), writes=["gpost"], dma="d_gpost")

        alias_B = [("hT", tt) for tt in range(T)]
        alias_C = [("aT", cc) for cc in range(CPJ)] + [("sg", 0), ("sg", 1)]

        def P1(t):
            k = t % 2
            S.op("sync", lambda e: e.dma_start(out=pB_xt[k][:], in_=src[t * 128:(t + 1) * 128, :]),
                 writes=[("xt", k)] + (alias_B if t < 2 else []), dma="d_xt%d" % k)
            S.op("scalar", lambda e: e.activation(
                out=pC_junk[:], in_=fsb[:, t, :], func=AF.Square, accum_out=ss[:, t:t + 1]),
                reads=[("f", t)], writes=["junk", ("ss", t)] + (alias_C if t == 0 else []))
            S.op("scalar", lambda e: e.activation(
                out=rs[:, t:t + 1], in_=ss[:, t:t + 1], func=AF.Sqrt, scale=1.0 / D, bias=eps_col[:, 0:1]),
                reads=[("ss", t)], writes=[("rs", t)])
            S.op("vector", lambda e: e.reciprocal(out=rstd[:, t:t + 1], in_=rs[:, t:t + 1]),
                 reads=[("rs", t)], writes=[("rstd", t)])
            S.op("vector", lambda e: e.tensor_scalar(
                out=rstd[:, 8 + t:9 + t], in0=rstd[:, t:t + 1], scalar1=float(res_scale), scalar2=None, op0=ALU.mult),
                reads=[("rstd", t)], writes=[("rstds", t)])

        def P2(t):
            k = t % 2
            S.op("vector", lambda e: e.scalar_tensor_tensor(
                out=pB_out[k][:], in0=fsb[:, t, :], scalar=rstd[:, 8 + t:9 + t], in1=gp_[:], op0=ALU.mult, op1=ALU.mult),
                reads=[("f", t), ("rstds", t), "gpost"], writes=[("po", k), ("po2", k)] + (alias_B if t < 2 else []))
            S.op("gpsimd", lambda e: e.tensor_tensor(
                out=pB_out[k][:, 0:1024], in0=pB_out[k][:, 0:1024], in1=pB_xt[k][:, 0:1024], op=ALU.add),
                reads=[("po", k), ("xt", k)], writes=[("po", k)])
            S.op("vector", lambda e: e.tensor_tensor(
                out=pB_out[k][:, 1024:2048], in0=pB_out[k][:, 1024:2048], in1=pB_xt[k][:, 1024:2048], op=ALU.add),
                reads=[("po2", k), ("xt", k)], writes=[("po2", k)])
            S.op("sync", lambda e: e.dma_start(out=dst[t * 128:(t + 1) * 128, :], in_=pB_out[k][:]),
                 reads=[("po", k), ("po2", k)], writes=[("dst", t)], dma="d_out%d" % k)

        for p_ in range(-1, T):
            if p_ + 1 < T:
                P1(p_ + 1)
            if p_ >= 0:
                P2(p_)


    def tok_proj(lhsT_fn, lhs_res_fn, nk, w_slots, w_sems, w_src_fn, ngrp, epilogue, bank_fn, w_tag):
        for n in range(ngrp):
            sl = n % len(w_slots)
            S.op("gpsimd", lambda e, n=n, sl=sl: e.dma_start(out=w_slots[sl][:], in_=w_src_fn(n)),
                 reads=([("hT", 3 + 4 * n)] if n < 2 else []),
                 writes=[(w_tag, sl)], dma=w_sems[sl])
            for t in range(T):
                b = bank_fn(n, t)
                for k in range(nk):
                    S.op("tensor", lambda e, k=k, t=t, b=b, sl=sl: e.matmul(
                        out=bank(b), lhsT=lhsT_fn(k, t), rhs=w_slots[sl][:, k, :], start=(k == 0), stop=(k == nk - 1)),
                        reads=[(w_tag, sl), lhs_res_fn(k, t)], writes=[("ps", b)])
                epilogue(n, t, b)

    def ffn(idx, src, dst, gcol0, gidx, sub=99):
        Wgu, Wd = wgu[idx], wd[idx]
        norm_T(dram_src(src, nA_xt), hT, gcol0, 1, nA_xn, nA_junk, nA_gbc, "hT")
        if sub < 1:
            S.barrier()
            return
        for j in range(NJ):
            for cc in range(CPJ):
                c = j * CPJ + cc
                sl = c % 3
                par = c % 2
                S.op("gpsimd", lambda e, c=c, sl=sl: e.dma_start(out=wgu_sb[sl][:], in_=Wgu[c]),
                     reads=([("hT", 3 + 2 * c)] if c < 3 else []),
                     writes=[("wgu", sl)], dma="d_wgu%d" % sl)
                for gu in range(2):
                    for kc in range(KC):
                        for half in range(2):
                            b = par * 4 + gu * 2 + half
                            S.op("tensor", lambda e, gu=gu, kc=kc, half=half, b=b, sl=sl: e.matmul(
                                out=bank(b), lhsT=wgu_sb[sl][:, gu, kc, :], rhs=hT[:, kc, half * 512:(half + 1) * 512],
                                start=(kc == 0), stop=(kc == KC - 1)),
                                reads=[("wgu", sl)] + [("hT", tt) for tt in range(half * 4, half * 4 + 4)],
                                writes=[("ps", b)])
                bg = par * 4
                S.op("scalar", lambda e, bg=bg, par=par: e.activation(
                    out=sg[par][:], in_=ps[:, bg * 512: bg * 512 + 1024], func=AF.Silu),
                    reads=[("ps", bg), ("ps", bg + 1)], writes=[("sg", par)])
                S.op("vector", lambda e, bg=bg, par=par, cc=cc: e.tensor_tensor(
                    out=aT[:, cc, :], in0=sg[par][:], in1=ps[:, (bg + 2) * 512:(bg + 2) * 512 + 1024], op=ALU.mult),
                    reads=[("sg", par), ("ps", bg + 2), ("ps", bg + 3)], writes=[("aT", cc)])
            for n in range(4):
                sl = (j * 4 + n) % 2
                S.op("gpsimd", lambda e, j=j, n=n, sl=sl: e.dma_start(out=wd_sb[sl][:], in_=Wd[j, n]),
                     writes=[("wd", sl)], dma="d_wd%d" % sl)
                for t in range(T):
                    for cc in range(CPJ):
                        S.op("tensor", lambda e, t=t, cc=cc, sl=sl: e.matmul(
                            out=bank(t), lhsT=aT[:, cc, t * 128:(t + 1) * 128], rhs=wd_sb[sl][:, cc, :],
                            start=(cc == 0), stop=(cc == CPJ - 1)),
                            reads=[("wd", sl), ("aT", cc)], writes=[("ps", t)])
                    if j == 0:
                        S.op("scalar", lambda e, t=t, n=n: e.activation(
                            out=big[:, t, n * 512:(n + 1) * 512], in_=bank(t), func=AF.Copy),
                            reads=[("ps", t)], writes=[("f", t)])
                    else:
                        S.op("vector", lambda e, t=t, n=n: e.tensor_tensor(
                            out=big[:, t, n * 512:(n + 1) * 512], in0=big[:, t, n * 512:(n + 1) * 512],
                            in1=bank(t), op=ALU.add),
                            reads=[("ps", t), ("f", t)], writes=[("f", t)])
        postnorm_residual(big, src, dst, gidx, 0.5, gpost)
        S.barrier()

    def finish():
        with nc.Block() as block:
            S.emit(block)
        S.close()
        return nc

    if stage == 0:
        return finish()
    if stage == 1:
        ffn(0, x_par, x1_par, 0, 0, sub=0)
        return finish()
    ffn(0, x_par, x1_par, 0, 0)
    if stage == 2:
        return finish()
    ffn(0, x_own, x1_own, 0, 0)
    if stage == 3:
        return finish()

    S.op("sync", lambda e: e.dma_start(out=rope_sb[:], in_=rope_d.rearrange("w p f -> p w f")),
         writes=["rope"], dma="d_rope")
    rope_v = [rope_sb[:, w, :].rearrange("p (t c h f) -> p t c h f", t=T, c=2, h=4) for w in range(2)]
    cnt = {"pp": 0, "tp": 0, "st": 0}

    def qkv_for(which, src, groups):
        norm_T(dram_src(src, nA_xt), hT, 16, 1, nA_xn, nA_junk, nA_gbc, "hT")

        pending = []

        def epilogue(n_idx, t, b):
            prev = pending.pop() if pending else None
            epilogue_main(n_idx, t, b)
            if prev is not None:
                prev()

        def epilogue_main(n_idx, t, b):
            grp = groups[n_idx]
            typ = grp // 2
            hh0 = (grp % 2) * 4
            h0 = (0 if typ < 3 else 8) + hh0
            kt = t if which == 0 else 8 + t
            st = cnt["st"] % 2
            cnt["st"] += 1
            S.op("scalar", lambda e, b=b, st=st: e.activation(out=qk_sb[st][:], in_=bank(b), func=AF.Copy),
                 reads=[("ps", b)], writes=[("qk", st)])
            if typ in (2, 5):
                if "nov" in _DBG:
                    return
                S.op("sync", lambda e, st=st, h0=h0, kt=kt: e.dma_start(
                    out=v_dram[h0:h0 + 4, :, kt, :].rearrange("h p d -> p h d"),
                    in_=qk_sb[st][:].rearrange("p (h d) -> p h d", h=4)),
                    reads=[("qk", st)], writes=[("vd", h0, kt)], dma="d_vst%d" % st)
                return
            if typ in (0, 1) and "norope" not in _DBG:
                ps4 = bank(b).rearrange("p (h d) -> p h d", h=4)
                cosv = rope_v[which][:, t, 0, :, :]
                sinv = rope_v[which][:, t, 1, :, :]
                S.op("vector", lambda e, ps4=ps4, cosv=cosv: e.tensor_tensor(
                    out=rt1[:], in0=ps4[:, :, 0:32], in1=cosv, op=ALU.mult),
                    reads=[("ps", b), "rope", ("qk", st)], writes=["rt1"])
                if "ropeA" not in _DBG:
                  S.op("vector", lambda e, ps4=ps4, sinv=sinv: e.tensor_tensor(
                    out=rt2[:, :, 0:16], in0=ps4[:, :, 16:32], in1=sinv[:, :, 0:16], op=ALU.mult),
                    reads=[("ps", b), "rope", ("qk", st)], writes=["rt2"])
                if "ropeA" not in _DBG:
                  S.op("vector", lambda e, ps4=ps4, sinv=sinv: e.tensor_tensor(
                    out=rt2[:, :, 16:32], in0=ps4[:, :, 0:16], in1=sinv[:, :, 16:32], op=ALU.mult),
                    reads=[("ps", b), "rope", ("qk", st)], writes=["rt2"])
                if "ropeA" not in _DBG and "ropeB" not in _DBG:
                  S.op("vector", lambda e, st=st: e.tensor_tensor(
                    out=qk_sb[st][:].rearrange("p (h d) -> p h d", h=4)[:, :, 0:32], in0=rt1[:], in1=rt2[:],
                    op=ALU.add), reads=["rt1", "rt2", ("qk", st)], writes=[("qk", st)])
            if "notr" in _DBG:
                return
            pending.append(lambda: epilogue_tail(typ, st, h0, t, kt))

        def epilogue_tail(typ, st, h0, t, kt):
            tb = 4 + cnt["tp"] % 4
            cnt["tp"] += 1
            for jh in range(4):
                S.op("tensor", lambda e, jh=jh, st=st, tb=tb: e.transpose(
                    out=psb[:, tb * 1024 + jh * 128: tb * 1024 + (jh + 1) * 128],
                    in_=qk_sb[st][:, jh * 128:(jh + 1) * 128], identity=ident_b[:]),
                    reads=[("qk", st)], writes=[("ps", tb)])
            tsrc = psb[:, tb * 1024: tb * 1024 + 512].rearrange("p (h j) -> p h j", h=4)
            if typ in (0, 3):
                S.op("vector", lambda e, tsrc=tsrc, h0=h0, t=t: e.tensor_copy(
                    out=qT[:, h0:h0 + 4, t * 128:(t + 1) * 128], in_=tsrc),
                    reads=[("ps", tb)], writes=[("qT", h0, t)])
            else:
                S.op("vector", lambda e, tsrc=tsrc, st=st: e.tensor_copy(out=kst[st][:], in_=tsrc),
                     reads=[("ps", tb)], writes=[("kst", st)])
                if "nok" not in _DBG:
                  S.op("sync", lambda e, st=st, h0=h0, kt=kt: e.dma_start(
                    out=kT_dram[h0:h0 + 4, :, kt * 128:(kt + 1) * 128].rearrange("h d j -> d h j"),
                    in_=kst[st][:]), reads=[("kst", st)], writes=[("kd", h0, kt)], dma="d_kst%d" % st)

        def bank_fn(n, t):
            b = cnt["pp"] % 4
            cnt["pp"] += 1
            return b

        tok_proj(lambda k, t: hT[:, k, t * 128:(t + 1) * 128], lambda k, t: ("hT", t), KC,
                 win_sb, ["d_win0", "d_win1"], lambda n: win[groups[n]], len(groups), epilogue, bank_fn, "win")
        if pending:
            pending.pop()()
        S.barrier()

    qkv_for(0, x1_par, [2, 3, 4, 5, 8, 9, 10, 11])
    qkv_for(1, x1_own, list(range(12)))
    if stage == 4:
        return finish()

    o_sb = big
    PTB = 4

    def attention():
        items = [(h, i) for h in range(8) for i in range(T)] + [None, None] + \
                [(h, i) for h in range(8, NH) for i in range(T)]
        N = len(items)

        def I(n):
            h, i = items[n]
            nk = 1024 + 128 * (i + 1)
            return dict(h=h, i=i, w=n % 2, w4=n % 4, hs=h % 2, moba=(h < 8), nk=nk, nkt=nk // 128,
                        npast=4 + i // 2, sb=[("ps", bb) for bb in range((nk + 511) // 512)])

        def st_scores(n):
            c = I(n)
            h, i, hs, nk = c["h"], c["i"], c["hs"], c["nk"]
            if i == 0:
                S.op("sync", lambda e: e.dma_start(out=kT_h[hs][:], in_=kT_dram[h]),
                     writes=[("kT", hs)], dma="d_kT%d" % hs)
                S.op("sync", lambda e: e.dma_start(out=v_h[hs][:], in_=v_dram[h]),
                     writes=[("v", hs)], dma="d_v%d" % hs)
                if c["moba"]:
                    S.op("vector", lambda e: e.tensor_reduce(
                        out=km_f[:], in_=kT_h[hs][:].rearrange("p (n k) -> p n k", k=256), axis=AX.X, op=ALU.add),
                        reads=[("kT", hs)], writes=["km_f"])
                    S.op("vector", lambda e: e.tensor_copy(out=km_b[hs][:], in_=km_f[:]),
                         reads=["km_f"], writes=[("km_b", hs)])
                    for ww in range(2):
                        S.op("vector", lambda e, ww=ww: e.memset(g8s[ww], -1e30), writes=[("g8", ww)])
            qTi = qT[:, h, i * 128:(i + 1) * 128]
            mask = masks_b[:, 0:128] if c["moba"] else masks_b[:, 128:256]
            for c0 in range(0, nk, 512):
                wd_ = min(512, nk - c0)
                last = (c0 + wd_ == nk)
                S.op("tensor", lambda e, c0=c0, wd_=wd_, last=last: e.matmul(
                    out=ps[:, c0:c0 + wd_], lhsT=qTi, rhs=kT_h[hs][:, c0:c0 + wd_], start=True, stop=(not last)),
                    reads=[("kT", hs)], writes=[("ps", c0 // 512)])
            S.op("tensor", lambda e: e.matmul(
                out=ps[:, nk - 128:nk], lhsT=ident_b[:], rhs=mask, start=False, stop=True),
                writes=[("ps", (nk - 128) // 512)])
            if c["moba"]:
                S.op("tensor", lambda e: e.matmul(
                    out=ps[:, 6 * 512:6 * 512 + 8], lhsT=qTi, rhs=km_b[hs][:], start=True, stop=True),
                    reads=[("km_b", hs)], writes=[("ps", 6)])

        def st_pre(n):
            c = I(n)
            w, nk, npast = c["w"], c["nk"], c["npast"]
            if c["moba"]:
                g8w, m8w, selw = g8s[w], m8s[w], sels[w]
                S.op("vector", lambda e: e.tensor_scalar(
                    out=g8w[:, 0:4], in0=ps[:, 6 * 512:6 * 512 + 4], scalar1=pbias[:, 0:1], scalar2=None,
                    op0=ALU.add), reads=[("ps", 6)], writes=[("g8", w)])
                if npast > 4:
                    S.op("vector", lambda e: e.tensor_copy(
                        out=g8w[:, 4:npast], in_=ps[:, 6 * 512 + 4:6 * 512 + npast]),
                        reads=[("ps", 6)], writes=[("g8", w)])
                S.op("vector", lambda e: e.max(out=m8w, in_=g8w), reads=[("g8", w)], writes=[("m8", w)])
                S.op("vector", lambda e: e.tensor_scalar(
                    out=selw, in0=g8w, scalar1=m8w[:, 2:3], scalar2=None, op0=ALU.is_ge),
                    reads=[("g8", w), ("m8", w)], writes=[("sel", w)])
                S.op("vector", lambda e: e.tensor_scalar(
                    out=selw, in0=selw, scalar1=-1.0, scalar2=-NEG, op0=ALU.add, op1=ALU.mult),
                    reads=[("sel", w)], writes=[("sel", w)])
                S.op("vector", lambda e: e.tensor_scalar(
                    out=selw[:, 0:4], in0=selw[:, 0:4], scalar1=pbias[:, 0:1], scalar2=None, op0=ALU.add),
                    reads=[("sel", w)], writes=[("sel", w)])
            else:
                S.op("scalar", lambda e: e.activation(
                    out=zb[w][:, 0:nk], in_=ps[:, 0:nk], func=AF.Copy, scale=SCALE),
                    reads=c["sb"], writes=[("zb", w), ("zbh", w)])

        def st_exp1(n):
            c = I(n)
            w, nk, npast = c["w"], c["nk"], c["npast"]
            if c["moba"]:
                selw, rsw, w4 = sels[w], rsums[w], c["w4"]
                for nb in range(npast):
                    S.op("scalar", lambda e, nb=nb: e.activation(
                        out=Pbm[w4][:, nb * 256:(nb + 1) * 256], in_=ps[:, nb * 256:(nb + 1) * 256], func=AF.Exp,
                        scale=SCALE, bias=selw[:, nb:nb + 1], accum_out=rsw[:, nb:nb + 1]),
                        reads=[("ps", nb // 2), ("sel", w)], writes=[("Pbm", w4), ("rsum", w)])
                S.op("scalar", lambda e: e.activation(
                    out=Pbm[w4][:, npast * 256:nk], in_=ps[:, npast * 256:nk], func=AF.Exp, scale=SCALE,
                    accum_out=rsw[:, npast:npast + 1]),
                    reads=c["sb"], writes=[("Pbm", w4), ("rsum", w)])
            else:
                S.op("scalar", lambda e: e.activation(
                    out=EL[w][:, 0:1024], in_=zb[w][:, 0:1024], func=AF.Exp, bias=pbias[:, 0:1]),
                    reads=[("zb", w)], writes=[("EL", w), ("Pbm", 2 * w), ("Pbm", 2 * w + 1)])
                S.op("scalar", lambda e: e.activation(
                    out=EL[w][:, 1024:nk], in_=zb[w][:, 1024:nk], func=AF.Exp),
                    reads=[("zb", w)], writes=[("EL", w)])
                S.op("scalar", lambda e: e.activation(
                    out=EL[w][:, 0:nk], in_=EL[w][:, 0:nk], func=AF.Ln, bias=one_col[:, 0:1]),
                    reads=[("EL", w)], writes=[("EL", w)])

        def st_B(n):
            c = I(n)
            w, w4, nk, npast = c["w"], c["w4"], c["nk"], c["npast"]
            if c["moba"]:
                S.op("vector", lambda e: e.reduce_sum(
                    out=rtot[:, w:w + 1], in_=rsums[w][:, 0:npast + 1], axis=AX.X),
                    reads=[("rsum", w)], writes=[("rtot", w)])
                S.op("vector", lambda e: e.reciprocal(out=rinv4[:, w4:w4 + 1], in_=rtot[:, w:w + 1]),
                     reads=[("rtot", w)], writes=[("rinv", w4)])
            else:
                S.op("vector", lambda e: e.tensor_tensor_scan(
                    out=Cb1[:, 0:nk], data0=ones_b[:, 0:nk], data1=EL[w][:, 0:nk], initial=0.0,
                    op0=ALU.mult, op1=ALU.add), reads=[("EL", w)], writes=["Cb"])
                S.op("vector", lambda e: e.tensor_scalar(
                    out=negT[:, w:w + 1], in0=Cb1[:, nk - 1:nk], scalar1=-1.0, scalar2=None, op0=ALU.mult),
                    reads=["Cb"], writes=[("negT", w)])
                S.op("vector", lambda e: e.tensor_scalar(
                    out=negTp[:, w:w + 1], in0=negT[:, w:w + 1], scalar1=pbias[:, 0:1], scalar2=None, op0=ALU.add),
                    reads=[("negT", w)], writes=[("negTp", w)])
                S.op("gpsimd", lambda e: e.tensor_tensor(
                    out=zb[w][:, 1:nk], in0=zb[w][:, 1:nk], in1=Cb1[:, 0:nk - 1], op=ALU.add),
                    reads=["Cb", ("zb", w)], writes=[("zb", w)])

        def st_expP(n):
            c = I(n)
            w, nk = c["w"], c["nk"]
            if c["moba"]:
                return
            S.op("scalar", lambda e: e.activation(
                out=Pb[w][:, 0:1024], in_=zb[w][:, 0:1024], func=AF.Exp, bias=negTp[:, w:w + 1]),
                reads=[("zb", w), ("zbh", w), ("negTp", w)], writes=[("Pb", w)])
            S.op("scalar", lambda e: e.activation(
                out=Pb[w][:, 1024:nk], in_=zb[w][:, 1024:nk], func=AF.Exp, bias=negT[:, w:w + 1]),
                reads=[("zb", w), ("zbh", w), ("negT", w)], writes=[("Pb", w)])

        def st_T(n):
            c = I(n)
            w = c["w"]
            src, res = (Pbm[c["w4"]], ("Pbm", c["w4"])) if c["moba"] else (Pb[w], ("Pb", w))
            for kt in range(c["nkt"]):
                tb = PTB + kt // 8
                S.op("tensor", lambda e, kt=kt: e.transpose(
                    out=psb[:, PTB * 1024 + kt * 128: PTB * 1024 + (kt + 1) * 128],
                    in_=src[:, kt * 128:(kt + 1) * 128], identity=ident_b[:]),
                    reads=[res], writes=[("ps", tb)])

        def st_PTcopy(n):
            c = I(n)
            w, nk = c["w"], c["nk"]
            S.op("vector", lambda e: e.tensor_copy(
                out=PT[w][:, 0:1024], in_=psb[:, PTB * 1024: PTB * 1024 + 1024]),
                reads=[("ps", PTB)], writes=[("PTa", w)])
            S.op("vector", lambda e: e.tensor_copy(
                out=PT[w][:, 1024:nk], in_=psb[:, PTB * 1024 + 1024: PTB * 1024 + nk]),
                reads=[("ps", PTB + 1)], writes=[("PTb", w)])

        def st_PV(n):
            c = I(n)
            w, hs, nkt = c["w"], c["hs"], c["nkt"]
            for kt in range(nkt):
                S.op("tensor", lambda e, kt=kt: e.matmul(
                    out=ps[:, 7 * 512:7 * 512 + 128], lhsT=PT[w][:, kt * 128:(kt + 1) * 128],
                    rhs=v_h[hs][:, kt, :], start=(kt == 0), stop=(kt == nkt - 1)),
                    reads=[("PTa", w), ("PTb", w), ("v", hs)], writes=[("ps", 7)])

        def st_out(n):
            c = I(n)
            h, i, w4 = c["h"], c["i"], c["w4"]
            if c["moba"]:
                S.op("scalar", lambda e: e.activation(
                    out=o_sb[:, i, h * 128:(h + 1) * 128], in_=ps[:, 7 * 512:7 * 512 + 128], func=AF.Copy,
                    scale=rinv4[:, w4:w4 + 1]), reads=[("ps", 7), ("rinv", w4)], writes=[("o", i)])
            else:
                S.op("scalar", lambda e: e.activation(
                    out=o_sb[:, i, h * 128:(h + 1) * 128], in_=ps[:, 7 * 512:7 * 512 + 128], func=AF.Copy),
                    reads=[("ps", 7)], writes=[("o", i)])

        ok = lambda n: 0 <= n < N and items[n] is not None
        for p in range(-2, N + 2):
            if ok(p + 2): st_scores(p + 2)
            if ok(p): st_expP(p)
            if ok(p + 2): st_pre(p + 2)
            if ok(p + 1): st_B(p + 1)
            if ok(p + 2): st_exp1(p + 2)
            if ok(p - 1): st_T(p - 1)
            if ok(p - 1): st_PTcopy(p - 1)
            if ok(p - 2): st_PV(p - 2)
            if ok(p - 2): st_out(p - 2)

    one_col = misc[:, 8:9]
    S.op("vector", lambda e: e.memset(one_col, 1.0), writes=["one"])
    attention()
    S.barrier()
    if debug:
        for t in range(T):
            S.op("sync", lambda e, t=t: e.dma_start(out=o_dbg[t * 128:(t + 1) * 128, :], in_=o_sb[:, t, :]),
                 dma="d_dbg")
        S.barrier()
    if stage == 5:
        return finish()

    oT = hT
    norm_T(lambda t: (o_sb[:, t, :], ("o", t)), oT, 32, 2, nR_xn, nR_junk, nR_gbc, "hT")
    S.barrier()

    def op_epilogue(n, t, b):
        S.op("scalar", lambda e, t=t, n=n, b=b: e.activation(
            out=big[:, t, n * 512:(n + 1) * 512], in_=bank(b), func=AF.Copy),
            reads=[("ps", b)], writes=[("f", t)])

    cnt["pp"] = 0

    def bank_fn2(n, t):
        b = cnt["pp"] % 8
        cnt["pp"] += 1
        return b

    tok_proj(lambda k, t: oT[:, k, t * 128:(t + 1) * 128], lambda k, t: ("hT", t), KC,
             win_sb, ["d_win0", "d_win1"], lambda n: wout[n], 4, op_epilogue, bank_fn2, "win")
    postnorm_residual(big, x1_own, x2_own, 1, 1.0, gpost2)
    S.barrier()

    ffn(1, x2_own, out_d, 48, 2)

    return finish()


_NC_CACHE = {}


def _rope_tables(pos0):
    half = 16
    inv_freq = (500000.0 ** (-np.arange(0, 32, 2, dtype=np.float32) / 32)).astype(np.float32)
    pos = (pos0 + np.arange(1024, dtype=np.float32))
    ang = pos[:, None] * inv_freq[None, :]
    cos = np.cos(ang).astype(np.float32)
    sin = np.sin(ang).astype(np.float32)
    cos2 = np.concatenate([cos, cos], axis=1)
    sinS = np.concatenate([-sin, sin], axis=1)
    tab = np.stack([cos2, sinS], axis=1)
    tab = np.broadcast_to(tab[:, :, None, :], (1024, 2, 4, 32))
    tab = tab.reshape(T, 128, 2, 4, 32).transpose(1, 0, 2, 3, 4)
    return np.ascontiguousarray(tab).reshape(128, T * 2 * 4 * 32)


def prepare_inputs(inputs):
    f = lambda a: np.ascontiguousarray(np.asarray(a, dtype=np.float32))
    x = f(inputs["x"])

    def gu(wg, wu):
        g = f(wg)[0].reshape(KC, 128, NCH, 128).transpose(2, 1, 0, 3)
        u = f(wu)[0].reshape(KC, 128, NCH, 128).transpose(2, 1, 0, 3)
        return np.ascontiguousarray(np.stack([g, u], axis=2))

    def dn(w):
        return np.ascontiguousarray(f(w)[0].reshape(NJ, CPJ, 128, 4, 512).transpose(0, 3, 2, 1, 4))

    def colgrp(w, ng):
        return np.ascontiguousarray(f(w)[0].reshape(KC, 128, ng, 512).transpose(2, 1, 0, 3))

    def gcol(g):
        return f(g).reshape(KC, 128).T

    shared = {
        "wgu1": gu(inputs["ffn1_w_gate"], inputs["ffn1_w_up"]), "wd1": dn(inputs["ffn1_w_down"]),
        "wgu2": gu(inputs["ffn2_w_gate"], inputs["ffn2_w_up"]), "wd2": dn(inputs["ffn2_w_down"]),
        "win": colgrp(inputs["w_in"], 12), "wout": colgrp(inputs["w_out"], 4),
        "gT": np.ascontiguousarray(np.concatenate([
            gcol(inputs["ffn1_pre_g"]), gcol(inputs["mix_pre_g"]),
            gcol(np.concatenate([f(inputs["moba_out_g"])[0], f(inputs["sb_out_g"])[0]])),
            gcol(inputs["ffn2_pre_g"])], axis=1)),
        "gpost": np.ascontiguousarray(np.stack([
            np.broadcast_to(f(inputs[k])[0][None, :], (128, D)) for k in
            ("ffn1_post_g", "mix_post_g", "ffn2_post_g")])),
        "ident": np.eye(128, dtype=np.float32),
    }
    qi = np.arange(128)[:, None]
    ki = np.arange(128)[None, :]
    m_le = np.where(ki <= qi, 0.0, 2 * NEG).astype(np.float32)
    m_lt = np.where(ki < qi, 0.0, 2 * NEG).astype(np.float32)
    shared["masks"] = np.ascontiguousarray(np.concatenate([m_le, m_lt], axis=1))
    ropes = [_rope_tables(0.0), _rope_tables(1024.0)]
    in_maps = []
    for c in range(8):
        b, r = divmod(c, 2)
        m = dict(shared)
        m["x_own"] = np.ascontiguousarray(x[b, r * 1024:(r + 1) * 1024])
        m["x_par"] = np.ascontiguousarray(x[b, (1 - r) * 1024:(2 - r) * 1024])
        m["rope"] = np.ascontiguousarray(np.stack([ropes[1 - r], ropes[r]]))
        m["pbias"] = np.full((128, 1), NEG if r == 0 else 0.0, dtype=np.float32)
        in_maps.append(m)
    return in_maps


def kernel(**inputs):
    in_maps = prepare_inputs(inputs)
    if "nc" not in _NC_CACHE:
        _NC_CACHE["nc"] = build_nc(False)
    res = run_bass_kernel_spmd(_NC_CACHE["nc"], in_maps, core_ids=list(range(8)))
    out = np.empty((4, 2048, D), dtype=np.float32)
    for c in range(8):
        b, r = divmod(c, 2)
        out[b, r * 1024:(r + 1) * 1024] = res.results[c]["out"]
    return out
```
